# Optimizing a Trainium2 kernel written in Bass

```python
import math
import jax, jax.numpy as jnp
from jax import lax
import numpy as np

D_MODEL = 1024
BATCH = 1
SEQ = 16384
DEPTH = 1
DEC_BATCH = 32
DEC_SEQ = 1
PAST_LEN = 16384
PAGE_SIZE = 128

A_HEADS = 8
A_KV_HEADS = 2
A_GROUP = A_HEADS // A_KV_HEADS
HEAD_DIM = 64
CMP_STRIDE = 16
CMP_BLOCK = 2 * CMP_STRIDE
SLC_BLOCK = 64
RATIO = SLC_BLOCK // CMP_STRIDE
SLC_TOPN = 16
WINDOW = 512
Q_BLOCK = 128
N_KV_SLOTS = 4
B_HEADS = 4
B_DK = 128
B_DV = 128
CONV_W = 4
DELTA_CHUNK = 64
N_EXPERTS = 64
TOP_K = 6
D_EXPERT = 128
D_SHARED = 128
ROUTED_SCALE = 2.5
MOE_BLOCK = 128
EPS = 1e-6

A_WIDTH = A_HEADS * HEAD_DIM
B_WIDTH = B_HEADS * B_DV
CONV_CH = 2 * B_HEADS * B_DK + B_HEADS * B_DV
IN_SPLITS = (A_WIDTH, 6 * A_KV_HEADS * HEAD_DIM, 3 * A_HEADS, CONV_CH, B_HEADS, B_HEADS, B_WIDTH, 2 * D_MODEL)
IN_WIDTH = sum(IN_SPLITS)
IN_OFFSETS = tuple(int(v) for v in np.cumsum(IN_SPLITS)[:-1])
F32 = jnp.float32
NEG = -1e30
BIG = 1e30

kernel_name = 'hybrid_nsa_gdn_moe_adaln_step'


def rmsnorm(x, g):
    xf = x.astype(F32)
    y = xf * lax.rsqrt(jnp.mean(xf * xf, axis=-1, keepdims=True) + EPS)
    return (y * g.astype(F32)).astype(x.dtype)


def l2norm(x):
    return x * lax.rsqrt(jnp.sum(x * x, axis=-1, keepdims=True) + EPS)


def alibi_slopes():
    h = jnp.arange(1, A_HEADS + 1, dtype=F32)
    return jnp.exp2(-8.0 * h / A_HEADS)


def adaln(c, w_ada, b_ada):
    mod = jnp.einsum('bd,de->be', jax.nn.silu(c), w_ada) + b_ada
    return jnp.split(mod[:, None, :], 6, axis=-1)


def modulate(x, g, shift, scale):
    return rmsnorm(x, g) * (1 + scale) + shift


def project(h, w_in):
    z = jnp.einsum('btd,de->bte', h, w_in)
    return jnp.split(z, IN_OFFSETS, axis=-1)


def nsa_query(q_a, q_norm_g):
    B, T = q_a.shape[:2]
    return rmsnorm(q_a.reshape(B, T, A_HEADS, HEAD_DIM), q_norm_g) * HEAD_DIM ** -0.5


def compress(rows, w, pe):
    B, L, K, D = rows.shape
    r = rows.reshape(B, L // CMP_STRIDE, CMP_STRIDE, K, D)
    first = jnp.einsum('bnjkd,jde->bnke', r, w[:CMP_STRIDE])
    second = jnp.einsum('bnjkd,jde->bnke', r, w[CMP_STRIDE:])
    bias = jnp.einsum('jd,jde->e', pe, w)
    return first[:, :-1] + second[:, 1:] + bias


def nsa_keys(rows, k_norm_g, w_cmp, pe_cmp):
    B, L = rows.shape[:2]
    nsb = L // SLC_BLOCK
    kc = rmsnorm(compress(rows[:, :, 0], w_cmp[0], pe_cmp[0]), k_norm_g[0])
    vc = compress(rows[:, :, 1], w_cmp[1], pe_cmp[1])
    cpos = jnp.arange(kc.shape[1], dtype=jnp.int32) * CMP_STRIDE + (CMP_BLOCK - 1)

    def blocks(r):
        return r.reshape(B, nsb, SLC_BLOCK, A_KV_HEADS, HEAD_DIM).transpose(0, 3, 1, 2, 4)

    ks = blocks(rmsnorm(rows[:, :, 2], k_norm_g[1]))
    vs = blocks(rows[:, :, 3])
    return kc, vc, cpos, ks, vs


def nsa_attend(q, qpos, kc, vc, cpos, ks, vs, kw, vw, wpos, gates):
    B, Q = q.shape[:2]
    qg = q.reshape(B, Q, A_KV_HEADS, A_GROUP, HEAD_DIM)
    sl = alibi_slopes().reshape(A_KV_HEADS, A_GROUP)[None, :, :, None, None]
    dist_c = qpos[:, None] - cpos[None, :]
    valid_c = dist_c >= 0
    s_c = jnp.einsum('bqkgd,bnkd->bkgqn', qg, kc).astype(F32)
    s_c = jnp.where(valid_c, s_c - sl * dist_c.astype(F32), NEG)
    p_c = jax.nn.softmax(s_c, axis=-1) * jnp.any(valid_c, axis=-1)[:, None].astype(F32)
    o_c = jnp.einsum('bkgqn,bnkd->bqkgd', p_c.astype(vc.dtype), vc)
    nsb = ks.shape[2]
    nc = kc.shape[1]
    pg = jnp.pad(p_c.sum(axis=2), ((0, 0), (0, 0), (0, 0), (0, RATIO * nsb - nc)))
    pg = pg.reshape(B, A_KV_HEADS, Q, nsb, RATIO)
    imp = pg.sum(-1) + jnp.pad(pg[..., :-1, RATIO - 1], ((0, 0), (0, 0), (0, 0), (1, 0)))
    jidx = jnp.arange(nsb, dtype=jnp.int32)
    cur = qpos // SLC_BLOCK
    forced = (jidx[None, :] == cur[:, None]) | (jidx[None, :] == 0)
    avail = jidx[None, :] <= cur[:, None]
    imp = jnp.where(forced, BIG, jnp.where(avail, imp, NEG))
    _, sel = lax.top_k(imp, min(SLC_TOPN, nsb))
    bi = jnp.arange(B)[:, None, None, None]
    ki = jnp.arange(A_KV_HEADS)[None, :, None, None]
    kg = ks[bi, ki, sel]
    vg = vs[bi, ki, sel]
    spos = sel[..., None] * SLC_BLOCK + jnp.arange(SLC_BLOCK, dtype=jnp.int32)
    dist_s = (qpos[:, None, None] - spos)[:, :, None]
    s_s = jnp.einsum('bqkgd,bkqnsd->bkgqns', qg, kg).astype(F32)
    s_s = jnp.where(dist_s >= 0, s_s - sl[..., None] * dist_s.astype(F32), NEG)
    shp = s_s.shape
    p_s = jax.nn.softmax(s_s.reshape(shp[:4] + (-1,)), axis=-1).reshape(shp)
    o_s = jnp.einsum('bkgqns,bkqnsd->bqkgd', p_s.astype(vg.dtype), vg)
    dist_w = qpos[:, None] - wpos[None, :]
    valid_w = (dist_w >= 0) & (dist_w <= WINDOW) & (wpos >= 0)[None, :]
    s_w = jnp.einsum('bqkgd,bnkd->bkgqn', qg, kw).astype(F32)
    s_w = jnp.where(valid_w, s_w - sl * dist_w.astype(F32), NEG)
    p_w = jax.nn.softmax(s_w, axis=-1)
    o_w = jnp.einsum('bkgqn,bnkd->bqkgd', p_w.astype(vw.dtype), vw)
    gr = gates.reshape(B, Q, A_KV_HEADS, A_GROUP, 3)
    o = gr[..., 0:1] * o_c + gr[..., 1:2] * o_s + gr[..., 2:3] * o_w
    return o.reshape(B, Q, A_WIDTH)


def nsa_prompt(q, gates, kv_rows, win_rows, lp):
    B, T = q.shape[:2]
    kc, vc, cpos, ks, vs = nsa_keys(kv_rows, lp['k_norm_g'], lp['w_cmp'], lp['pe_cmp'])
    kw = rmsnorm(win_rows[:, :, 0], lp['k_norm_g'][2])
    pad = ((0, 0), (WINDOW, 0), (0, 0), (0, 0))
    kw_pad = jnp.pad(kw, pad)
    vw_pad = jnp.pad(win_rows[:, :, 1], pad)

    def block(i):
        q0 = i * Q_BLOCK
        qb = lax.dynamic_slice_in_dim(q, q0, Q_BLOCK, axis=1)
        gb = lax.dynamic_slice_in_dim(gates, q0, Q_BLOCK, axis=1)
        qpos = q0 + jnp.arange(Q_BLOCK, dtype=jnp.int32)
        kwb = lax.dynamic_slice_in_dim(kw_pad, q0, WINDOW + Q_BLOCK, axis=1)
        vwb = lax.dynamic_slice_in_dim(vw_pad, q0, WINDOW + Q_BLOCK, axis=1)
        wpos = q0 - WINDOW + jnp.arange(WINDOW + Q_BLOCK, dtype=jnp.int32)
        return nsa_attend(qb, qpos, kc, vc, cpos, ks, vs, kwb, vwb, wpos, gb)

    o = lax.map(block, jnp.arange(T // Q_BLOCK, dtype=jnp.int32))
    return o.transpose(1, 0, 2, 3).reshape(B, T, A_WIDTH)


def nsa_sample(q, gates, kv_new, win_new, cache_kv, page_table, cache_win, lp):
    DB, S = q.shape[:2]
    P = page_table.shape[1] * cache_kv.shape[1]
    past = cache_kv[page_table].reshape(DB, P, N_KV_SLOTS, A_KV_HEADS, HEAD_DIM)
    rows = jnp.concatenate([past.astype(kv_new.dtype), kv_new], axis=1)
    L = P + S
    Lp = -(-L // SLC_BLOCK) * SLC_BLOCK
    rows = jnp.pad(rows, ((0, 0), (0, Lp - L), (0, 0), (0, 0), (0, 0)))
    kc, vc, cpos, ks, vs = nsa_keys(rows, lp['k_norm_g'], lp['w_cmp'], lp['pe_cmp'])
    nwb = cache_win.shape[1]
    win = jnp.concatenate([cache_win.astype(win_new.dtype), win_new], axis=1)
    wpos = P - nwb + jnp.arange(nwb + S, dtype=jnp.int32)
    kw = rmsnorm(win[:, :, 0], lp['k_norm_g'][2])
    qpos = P + jnp.arange(S, dtype=jnp.int32)
    o = nsa_attend(q, qpos, kc, vc, cpos, ks, vs, kw, win[:, :, 1], wpos, gates)
    return o, win[:, S:]


def causal_conv(xpad, w):
    T = xpad.shape[1] - (CONV_W - 1)
    acc = xpad[:, 0:T] * w[0]
    for j in range(1, CONV_W):
        acc = acc + xpad[:, j:j + T] * w[j]
    return jax.nn.silu(acc)


def delta_inputs(qkv, a_b, b_b, a_log, dt_bias):
    B, T, _ = qkv.shape
    qkv = qkv.astype(F32)
    nk = B_HEADS * B_DK
    q = l2norm(qkv[..., :nk].reshape(B, T, B_HEADS, B_DK)) * B_DK ** -0.5
    k = l2norm(qkv[..., nk:2 * nk].reshape(B, T, B_HEADS, B_DK))
    v = qkv[..., 2 * nk:].reshape(B, T, B_HEADS, B_DV)
    g = -jnp.exp(a_log.astype(F32)) * jax.nn.softplus(a_b.astype(F32) + dt_bias.astype(F32))
    beta = jax.nn.sigmoid(b_b.astype(F32))
    return q, k, v, g, beta


def delta_chunked(q, k, v, g, beta):
    B, T, H, _ = q.shape
    C = DELTA_CHUNK
    N = T // C

    def chunks(x):
        return jnp.moveaxis(x.reshape((B, N, C) + x.shape[2:]), 2, 3)

    qc, kc, vc, gc, bc = chunks(q), chunks(k), chunks(v), chunks(g), chunks(beta)
    G = jnp.cumsum(gc, axis=-1)
    i = jnp.arange(C)
    incl = i[:, None] >= i[None, :]
    strict = i[:, None] > i[None, :]
    decay = jnp.exp(jnp.where(incl, G[..., :, None] - G[..., None, :], -jnp.inf))
    kk = jnp.einsum('bnhid,bnhjd->bnhij', kc, kc)
    a_mat = jnp.where(strict, kk * decay * bc[..., :, None], 0.0) + jnp.eye(C, dtype=F32)
    eg = jnp.exp(G)[..., None]
    rhs = jnp.concatenate([vc * bc[..., None], kc * bc[..., None] * eg], axis=-1)
    sol = lax.linalg.triangular_solve(a_mat, rhs, left_side=True, lower=True, unit_diagonal=True)
    u_c, w_c = sol[..., :B_DV], sol[..., B_DV:]
    qk = jnp.einsum('bnhid,bnhjd->bnhij', qc, kc) * decay
    q_dec = qc * eg
    k_dec = kc * jnp.exp(G[..., -1:] - G)[..., None]
    g_last = jnp.exp(G[..., -1])

    def step(s, inp):
        u_i, w_i, qk_i, qd_i, kd_i, gl_i = inp
        v_new = u_i - jnp.einsum('bhck,bhkv->bhcv', w_i, s)
        o = jnp.einsum('bhck,bhkv->bhcv', qd_i, s) + jnp.einsum('bhij,bhjv->bhiv', qk_i, v_new)
        s = s * gl_i[..., None, None] + jnp.einsum('bhck,bhcv->bhkv', kd_i, v_new)
        return s, o

    xs = (jnp.moveaxis(u_c, 1, 0), jnp.moveaxis(w_c, 1, 0), jnp.moveaxis(qk, 1, 0),
          jnp.moveaxis(q_dec, 1, 0), jnp.moveaxis(k_dec, 1, 0), jnp.moveaxis(g_last, 1, 0))
    s0 = jnp.zeros((B, H, B_DK, B_DV), F32)
    s_fin, o = lax.scan(step, s0, xs)
    o = jnp.moveaxis(jnp.moveaxis(o, 0, 1), 3, 2).reshape(B, T, H, B_DV)
    return o, s_fin


def delta_recurrent(s0, q, k, v, g, beta):
    def step(s, inp):
        q_t, k_t, v_t, g_t, b_t = inp
        s = s * jnp.exp(g_t)[..., None, None]
        u = b_t[..., None] * (v_t - jnp.einsum('bhk,bhkv->bhv', k_t, s))
        s = s + jnp.einsum('bhk,bhv->bhkv', k_t, u)
        return s, jnp.einsum('bhk,bhkv->bhv', q_t, s)

    xs = (jnp.moveaxis(q, 1, 0), jnp.moveaxis(k, 1, 0), jnp.moveaxis(v, 1, 0),
          jnp.moveaxis(g, 1, 0), jnp.moveaxis(beta, 1, 0))
    s_fin, o = lax.scan(step, s0.astype(F32), xs)
    return jnp.moveaxis(o, 0, 1), s_fin


def delta_output(o, gate_b, o_norm_g):
    B, T = o.shape[:2]
    gt = gate_b.reshape(B, T, B_HEADS, B_DV).astype(F32)
    y = rmsnorm(o, o_norm_g) * jax.nn.silu(gt)
    return y.reshape(B, T, B_WIDTH).astype(gate_b.dtype)


def merge_out(o_a, o_b, merge, lp):
    m_a, m_b = jnp.split(merge, 2, axis=-1)
    m = jax.nn.sigmoid(m_a) * (o_a @ lp['w_proj_a']) + jax.nn.sigmoid(m_b) * (o_b @ lp['w_proj_b'])
    return m @ lp['w_out']


def moe_block(h, lp):
    scores = jax.nn.sigmoid(jnp.einsum('nd,de->ne', h, lp['w_router']).astype(F32))
    _, idx = lax.top_k(scores + lp['b_router'].astype(F32), TOP_K)
    sel = jnp.take_along_axis(scores, idx, axis=-1)
    wts = sel / jnp.sum(sel, axis=-1, keepdims=True) * ROUTED_SCALE
    gate = jnp.sum(jax.nn.one_hot(idx, N_EXPERTS, dtype=F32) * wts[..., None], axis=-2)
    a, u = jnp.split(jnp.einsum('nd,edf->nef', h, lp['w_exp_gu']), 2, axis=-1)
    act = jax.nn.silu(a) * u * gate[..., None].astype(h.dtype)
    y = jnp.einsum('nef,efd->nd', act, lp['w_exp_down'])
    sa, su = jnp.split(h @ lp['w_sh_gu'], 2, axis=-1)
    return y + (jax.nn.silu(sa) * su) @ lp['w_sh_down']


def moe_apply(h, lp):
    B, T, D = h.shape
    n = B * T
    flat = h.reshape(n, D)
    if n % MOE_BLOCK == 0 and n > MOE_BLOCK:
        y = lax.map(lambda hb: moe_block(hb, lp), flat.reshape(n // MOE_BLOCK, MOE_BLOCK, D)).reshape(n, D)
    else:
        y = moe_block(flat, lp)
    return y.reshape(B, T, D)


def split_mixer_inputs(h, lp):
    B, T, _ = h.shape
    q_a, kv_a, g_a, qkv_b, a_b, b_b, gate_b, merge = project(h, lp['w_in'])
    q = nsa_query(q_a, lp['q_norm_g'])
    gates = jax.nn.sigmoid(g_a).reshape(B, T, A_HEADS, 3)
    kv = kv_a.reshape(B, T, 6, A_KV_HEADS, HEAD_DIM)
    return q, gates, kv[:, :, :N_KV_SLOTS], kv[:, :, N_KV_SLOTS:], qkv_b, a_b, b_b, gate_b, merge


def layer_prompt(x, c, lp):
    sh1, sc1, gt1, sh2, sc2, gt2 = adaln(c, lp['w_ada'], lp['b_ada'])
    h = modulate(x, lp['norm1_g'], sh1, sc1)
    T = h.shape[1]
    q, gates, kv_rows, win_rows, qkv_b, a_b, b_b, gate_b, merge = split_mixer_inputs(h, lp)
    o_a = nsa_prompt(q, gates, kv_rows, win_rows, lp)
    qkv_pad = jnp.pad(qkv_b, ((0, 0), (CONV_W - 1, 0), (0, 0)))
    qd, kd, vd, g, beta = delta_inputs(causal_conv(qkv_pad, lp['conv_w']), a_b, b_b, lp['a_log'], lp['dt_bias'])
    o_d, s_fin = delta_chunked(qd, kd, vd, g, beta)
    o_b = delta_output(o_d, gate_b, lp['o_norm_g'])
    x = x + gt1 * merge_out(o_a, o_b, merge, lp)
    x = x + gt2 * moe_apply(modulate(x, lp['norm2_g'], sh2, sc2), lp)
    return x, kv_rows, win_rows[:, -min(WINDOW, T):], qkv_pad[:, T:], s_fin.astype(x.dtype)


def layer_sample(x, c, cache_kv, page_table, cache_win, conv_state, delta_state, lp):
    sh1, sc1, gt1, sh2, sc2, gt2 = adaln(c, lp['w_ada'], lp['b_ada'])
    h = modulate(x, lp['norm1_g'], sh1, sc1)
    S = h.shape[1]
    q, gates, kv_rows, win_rows, qkv_b, a_b, b_b, gate_b, merge = split_mixer_inputs(h, lp)
    o_a, new_win = nsa_sample(q, gates, kv_rows, win_rows, cache_kv, page_table, cache_win, lp)
    qkv_pad = jnp.concatenate([conv_state.astype(qkv_b.dtype), qkv_b], axis=1)
    qd, kd, vd, g, beta = delta_inputs(causal_conv(qkv_pad, lp['conv_w']), a_b, b_b, lp['a_log'], lp['dt_bias'])
    o_d, s_fin = delta_recurrent(delta_state, qd, kd, vd, g, beta)
    o_b = delta_output(o_d, gate_b, lp['o_norm_g'])
    x = x + gt1 * merge_out(o_a, o_b, merge, lp)
    x = x + gt2 * moe_apply(modulate(x, lp['norm2_g'], sh2, sc2), lp)
    return x, kv_rows, new_win, qkv_pad[:, S:], s_fin.astype(x.dtype)


def setup_inputs(seed: int = 0) -> dict:
    key = jax.random.key(seed)
    ks = jax.random.split(key, 32)
    n_pages = PAST_LEN // PAGE_SIZE
    n_used = DEC_BATCH * n_pages
    n_phys = n_used + max(1, n_used // 4)
    win_buf = min(WINDOW, PAST_LEN)

    def nrm(k, shape, s):
        return jax.random.normal(k, shape, F32) * s

    page_table = jax.random.permutation(ks[3], n_phys)[:n_used].reshape(DEC_BATCH, n_pages).astype(jnp.int32)
    a_log = jnp.log(jax.random.uniform(ks[19], (DEPTH, B_HEADS), F32, 1.0, 16.0))
    dt = jnp.exp(jax.random.uniform(ks[20], (DEPTH, B_HEADS), F32, math.log(1e-3), math.log(1e-1)))
    dt_bias = dt + jnp.log(-jnp.expm1(-dt))
    return {
        'x_prompt': nrm(ks[0], (BATCH, SEQ, D_MODEL), 1.0),
        'x_sample': nrm(ks[1], (DEC_BATCH, DEC_SEQ, D_MODEL), 1.0),
        'cache_nsa_kv': nrm(ks[2], (DEPTH, n_phys, PAGE_SIZE, N_KV_SLOTS, A_KV_HEADS, HEAD_DIM), 1.0),
        'page_table': page_table,
        'cache_win_kv': nrm(ks[4], (DEPTH, DEC_BATCH, win_buf, 2, A_KV_HEADS, HEAD_DIM), 1.0),
        'state_conv': nrm(ks[5], (DEPTH, DEC_BATCH, CONV_W - 1, CONV_CH), 1.0),
        'state_delta': nrm(ks[6], (DEPTH, DEC_BATCH, B_HEADS, B_DK, B_DV), 0.1),
        'c_prompt': nrm(ks[7], (BATCH, D_MODEL), 1.0),
        'c_sample': nrm(ks[8], (DEC_BATCH, D_MODEL), 1.0),
        'norm1_g': 1.0 + nrm(ks[9], (DEPTH, D_MODEL), 0.05),
        'norm2_g': 1.0 + nrm(ks[10], (DEPTH, D_MODEL), 0.05),
        'w_ada': nrm(ks[11], (DEPTH, D_MODEL, 6 * D_MODEL), 0.5 * D_MODEL ** -0.5),
        'b_ada': nrm(ks[12], (DEPTH, 6 * D_MODEL), 0.02),
        'w_in': nrm(ks[13], (DEPTH, D_MODEL, IN_WIDTH), D_MODEL ** -0.5),
        'q_norm_g': 1.0 + nrm(ks[14], (DEPTH, HEAD_DIM), 0.05),
        'k_norm_g': 1.0 + nrm(ks[15], (DEPTH, 3, HEAD_DIM), 0.05),
        'w_cmp': nrm(ks[16], (DEPTH, 2, CMP_BLOCK, HEAD_DIM, HEAD_DIM), (CMP_BLOCK * HEAD_DIM) ** -0.5),
        'pe_cmp': nrm(ks[17], (DEPTH, 2, CMP_BLOCK, HEAD_DIM), 0.1),
        'conv_w': nrm(ks[18], (DEPTH, CONV_W, CONV_CH), CONV_W ** -0.5),
        'a_log': a_log,
        'dt_bias': dt_bias,
        'o_norm_g': 1.0 + nrm(ks[21], (DEPTH, B_DV), 0.05),
        'w_proj_a': nrm(ks[22], (DEPTH, A_WIDTH, D_MODEL), A_WIDTH ** -0.5),
        'w_proj_b': nrm(ks[23], (DEPTH, B_WIDTH, D_MODEL), B_WIDTH ** -0.5),
        'w_out': nrm(ks[24], (DEPTH, D_MODEL, D_MODEL), D_MODEL ** -0.5),
        'w_router': nrm(ks[25], (DEPTH, D_MODEL, N_EXPERTS), D_MODEL ** -0.5),
        'b_router': nrm(ks[26], (DEPTH, N_EXPERTS), 0.01),
        'w_exp_gu': nrm(ks[27], (DEPTH, N_EXPERTS, D_MODEL, 2 * D_EXPERT), D_MODEL ** -0.5),
        'w_exp_down': nrm(ks[28], (DEPTH, N_EXPERTS, D_EXPERT, D_MODEL), D_EXPERT ** -0.5),
        'w_sh_gu': nrm(ks[29], (DEPTH, D_MODEL, 2 * D_SHARED), D_MODEL ** -0.5),
        'w_sh_down': nrm(ks[30], (DEPTH, D_SHARED, D_MODEL), D_SHARED ** -0.5),
    }


def reference(x_prompt, x_sample, cache_nsa_kv, page_table, cache_win_kv, state_conv, state_delta,
              c_prompt, c_sample, norm1_g, norm2_g, w_ada, b_ada, w_in, q_norm_g, k_norm_g, w_cmp, pe_cmp,
              conv_w, a_log, dt_bias, o_norm_g, w_proj_a, w_proj_b, w_out, w_router, b_router,
              w_exp_gu, w_exp_down, w_sh_gu, w_sh_down):
    xp, xs = x_prompt, x_sample
    kvp_l, winp_l, convp_l, dp_l = [], [], [], []
    kvs_l, wins_l, convs_l, ds_l = [], [], [], []
    for l in range(DEPTH):
        lp = {
            'norm1_g': norm1_g[l], 'norm2_g': norm2_g[l], 'w_ada': w_ada[l], 'b_ada': b_ada[l],
            'w_in': w_in[l], 'q_norm_g': q_norm_g[l], 'k_norm_g': k_norm_g[l], 'w_cmp': w_cmp[l],
            'pe_cmp': pe_cmp[l], 'conv_w': conv_w[l], 'a_log': a_log[l], 'dt_bias': dt_bias[l],
            'o_norm_g': o_norm_g[l], 'w_proj_a': w_proj_a[l], 'w_proj_b': w_proj_b[l], 'w_out': w_out[l],
            'w_router': w_router[l], 'b_router': b_router[l], 'w_exp_gu': w_exp_gu[l],
            'w_exp_down': w_exp_down[l], 'w_sh_gu': w_sh_gu[l], 'w_sh_down': w_sh_down[l],
        }
        xp, kvp, winp, convp, dp = layer_prompt(xp, c_prompt, lp)
        xs, kvs, wins, convs, ds = layer_sample(xs, c_sample, cache_nsa_kv[l], page_table, cache_win_kv[l],
                                                state_conv[l], state_delta[l], lp)
        kvp_l.append(kvp); winp_l.append(winp); convp_l.append(convp); dp_l.append(dp)
        kvs_l.append(kvs); wins_l.append(wins); convs_l.append(convs); ds_l.append(ds)
    kv_rows_prompt = jnp.stack(kvp_l)
    win_prompt = jnp.stack(winp_l)
    conv_prompt = jnp.stack(convp_l)
    delta_prompt = jnp.stack(dp_l)
    kv_rows_sample = jnp.stack(kvs_l)
    win_sample = jnp.stack(wins_l)
    conv_sample = jnp.stack(convs_l)
    delta_sample = jnp.stack(ds_l)
    return (xp, xs, kv_rows_prompt, win_prompt, conv_prompt, delta_prompt,
            kv_rows_sample, win_sample, conv_sample, delta_sample)
```

```python
import numpy as np
import concourse.bass as bass
import concourse.mybir as mybir
from concourse.bass_utils import run_bass_kernel_spmd
from contextlib import ExitStack

F32 = mybir.dt.float32
BF16 = mybir.dt.bfloat16
I32 = mybir.dt.int32
AF = mybir.ActivationFunctionType
ALU = mybir.AluOpType
AX = mybir.AxisListType


class Op:
    __slots__ = ("eng", "fn", "deps", "signal", "semval", "kind", "chan", "chanval", "idx")

    def __init__(self, eng, fn, kind):
        self.eng = eng
        self.fn = fn
        self.kind = kind
        self.deps = []
        self.signal = False
        self.semval = 0
        self.chan = None
        self.chanval = 0


class Sched:
    ENGS = ("pe", "act", "dve", "pool", "sp")

    def __init__(self, nc, stack):
        self.nc = nc
        self.stack = stack
        self.ops = {e: [] for e in self.ENGS}
        self.lastw = {}
        self.readers = {}
        self.chans = {}
        self.sems = {e: stack.enter_context(nc.semaphore("s_" + e)) for e in self.ENGS}
        self.n = 0
        self.chan_last = {}
        self._bar_deps = []
        self._bar_pending = set()

    def barrier(self):
        deps = []
        for e in self.ENGS:
            for o in reversed(self.ops[e]):
                if o.kind != "dma":
                    deps.append(o)
                    break
        deps += list(self.chan_last.values())
        self._bar_deps = deps
        self._bar_pending = set(self.ENGS)

    def chan(self, name):
        c = self.chans.get(name)
        if c is None:
            c = [self.stack.enter_context(self.nc.semaphore("c_" + name)), 0]
            self.chans[name] = c
        return c

    def _add(self, o, reads, writes):
        deps = []
        seen = set()

        def add(d, raw):
            if d is None or id(d) in seen:
                return
            if d.kind != "dma" and o.kind != "dma" and d.eng == o.eng:
                if o.eng == "pe":
                    return
            seen.add(id(d))
            deps.append(d)

        for k in reads:
            add(self.lastw.get(k), True)
        for k in writes:
            add(self.lastw.get(k), False)
            for r in self.readers.get(k, {}).values():
                if isinstance(r, list):
                    for rr in r:
                        add(rr, False)
                else:
                    add(r, False)
        if o.eng in self._bar_pending:
            self._bar_pending.discard(o.eng)
            for d in self._bar_deps:
                if d.kind == "dma" or d.eng != o.eng:
                    if id(d) not in seen:
                        seen.add(id(d))
                        deps.append(d)
        if o.kind == "dma":
            self.chan_last[id(o.chan)] = o
        for k in reads:
            rd = self.readers.setdefault(k, {})
            if o.kind == "dma":
                rd.setdefault("dma", []).append(o)
            else:
                rd[o.eng] = o
        for k in writes:
            self.lastw[k] = o
            self.readers[k] = {}
        o.deps = deps
        o.idx = self.n
        self.n += 1
        self.ops[o.eng].append(o)
        return o

    def op(self, eng, fn, reads=(), writes=()):
        return self._add(Op(eng, fn, "c"), reads, writes)

    def dma(self, q, chan, out, in_, reads=(), writes=(), **kw):
        o = Op(q, lambda e: e.dma_start(out=out, in_=in_, **kw), "dma")
        c = self.chan(chan)
        c[1] += 16
        o.chan = c[0]
        o.chanval = c[1]
        return self._add(o, reads, writes)

    def dma_fn(self, q, chan, fn, reads=(), writes=()):
        o = Op(q, fn, "dma")
        c = self.chan(chan)
        c[1] += 16
        o.chan = c[0]
        o.chanval = c[1]
        return self._add(o, reads, writes)

    def finalize(self, final_waits=()):
        for e in self.ENGS:
            for o in self.ops[e]:
                for d in o.deps:
                    if d.kind != "dma":
                        d.signal = True
        for e in self.ENGS:
            c = 0
            for o in self.ops[e]:
                if o.kind != "dma" and o.signal:
                    c += 1
                    o.semval = c
        sems = self.sems
        ops = self.ops
        chans = self.chans

        def emit(ename, eng):
            waited = {}
            for o in ops[ename]:
                for d in o.deps:
                    if d.kind == "dma":
                        s, v = d.chan, d.chanval
                    else:
                        s, v = sems[d.eng], d.semval
                    key = id(s)
                    if waited.get(key, 0) < v:
                        eng.wait_ge(s, v)
                        waited[key] = v
                ins = o.fn(eng)
                if o.kind == "dma":
                    ins.then_inc(o.chan, 16)
                elif o.signal:
                    ins.then_inc(sems[ename], 1)
            if ename == "sp":
                for name, (s, v) in chans.items():
                    if v > 0 and waited.get(id(s), 0) < v:
                        eng.wait_ge(s, v)

        with self.nc.Block() as block:
            @block.tensor
            def _(e):
                emit("pe", e)

            @block.scalar
            def _(e):
                emit("act", e)

            @block.vector
            def _(e):
                emit("dve", e)

            @block.gpsimd
            def _(e):
                emit("pool", e)

            @block.sync
            def _(e):
                emit("sp", e)


REG = {}


class Arena:
    def __init__(self, S, base):
        self.S = S
        self.base = base
        self.W = base.shape[1]
        self.off = 0

    def alloc(self, shape, dt, parts=None):
        n = 1
        for s in shape[1:]:
            n *= s
        words = n if dt in (F32, I32) else (n + 1) // 2
        assert self.off + words <= self.W, ("arena overflow", self.off, words, self.W)
        v = self.base[:, self.off:self.off + words]
        self.off += words
        if dt == BF16:
            v = v.bitcast(BF16)[:, 0:n]
        elif dt == I32:
            v = v.bitcast(I32)
        if len(shape) == 3:
            v = v.rearrange("p (a b) -> p a b", b=shape[2])
        elif len(shape) == 4:
            v = v.rearrange("p (a b c) -> p a b c", b=shape[2], c=shape[3])
        if shape[0] != 128:
            v = v[0:shape[0]]
        return v

    def mark(self):
        return self.off

    def release(self, m):
        self.off = m
        self.S.barrier()


D = 1024
T = 16384
NC = 8
NQB = 16
SB_ = 4
INW = 5408
O_Q, O_KV, O_G, O_QKV, O_A, O_B, O_GATE, O_MERGE = 0, 512, 1280, 1304, 2840, 2844, 2848, 3360
EPS = 1e-6
import os
STAGE = int(os.environ.get('K_STAGE', '9'))


def build_nc(NQB=NQB):
    nc = bass.Bass("TRN2", target_bir_lowering=False)
    dt_in = lambda n, s, d=F32: nc.dram_tensor(n, s, d, kind="ExternalInput").ap()
    dt_out = lambda n, s, d=F32: nc.dram_tensor(n, s, d, kind="ExternalOutput").ap()
    NBLK = 8 * NQB + 7
    xp = dt_in("xp", [NBLK, 128, D])
    vmask_d = dt_in("vmask", [128, 8])
    xs = dt_in("xs", [SB_, D])
    call = dt_in("call", [33, D])
    w_ada = dt_in("w_ada", [D, 6 * D])
    b_ada = dt_in("b_ada", [1, 6 * D])
    norm1_g = dt_in("norm1_g", [1, D])
    norm2_g = dt_in("norm2_g", [1, D])
    w_proj_a = dt_in("w_proj_a", [512, D])
    w_proj_b = dt_in("w_proj_b", [512, D])
    w_out = dt_in("w_out", [D, D])
    w_router = dt_in("w_router", [D, 64])
    b_router = dt_in("b_router", [1, 64])
    w_exp_gu = dt_in("w_exp_gu", [64, D, 256])
    w_exp_down = dt_in("w_exp_down", [64, 128, D])
    w_sh_gu = dt_in("w_sh_gu", [D, 256])
    w_sh_down = dt_in("w_sh_down", [128, D])
    k_norm_g = dt_in("k_norm_g", [3, 64])
    q_norm_g = dt_in("q_norm_g", [1, 64])
    w_cmp = dt_in("w_cmp", [2, 32, 64, 64])
    pe_cmp = dt_in("pe_cmp", [2, 32, 64])
    NBLKT = max(NBLK, 135)
    LLOC = NBLKT * 128
    NCB = 8 * NBLKT
    NCT = (NCB + 127) // 128
    NSBK = 2 * NBLKT
    kaug_pos = dt_in("kaug_pos", [5, LLOC], BF16)
    kaug_cmp = dt_in("kaug_cmp", [5, NCT * 128], BF16)
    qaug = dt_in("qaug", [NQB, 5, 8, 128], BF16)
    band_tab = dt_in("band_tab", [NCT * 128, NSBK], BF16)
    availneg = dt_in("availneg", [128, NSBK])
    forced0 = dt_in("forced0", [128, NSBK])
    kvalid = dt_in("kvalid", [128, NCT])
    qaug_s = dt_in("qaug_s", [5, 8, 128], BF16)
    piota = dt_in("piota", [128, 1])
    ptab = dt_in("ptab", [SB_, 128], I32)
    cache = dt_in("cache", [5120 * 128, 512])
    w_in = dt_in("w_in", [D, INW])
    cwin = dt_in("cwin", [SB_, 512, 256])
    sconv = dt_in("sconv", [SB_, 3, 1536])
    sdelta = dt_in("sdelta", [SB_, 4, 128, 128])
    conv_w = dt_in("conv_w", [4, 1536])
    a_log = dt_in("a_log", [1, 4])
    dt_bias = dt_in("dt_bias", [1, 4])
    o_norm_g = dt_in("o_norm_g", [1, 128])

    o_kvp = dt_out("o_kvp", [NQB, 128, 512])
    o_winp = dt_out("o_winp", [128, 256])
    o_convp = dt_out("o_convp", [3, 1536])
    o_kvs = dt_out("o_kvs", [SB_, 512])
    o_wins = dt_out("o_wins", [SB_, 512, 256])
    o_convs = dt_out("o_convs", [SB_, 3, 1536])
    o_ds = dt_out("o_ds", [SB_, 4, 128, 128])
    o_dp = dt_out("o_dp", [4, 128, 128])
    o_yp = dt_out("o_yp", [NQB, 128, D])
    o_ys = dt_out("o_ys", [SB_, D])
    ob_s = nc.dram_tensor("ob_s", [NQB + 1, 128, 512], BF16).ap()
    oa_s = nc.dram_tensor("oa_s", [NQB + 1, 128, 512], F32).ap()
    KT_s = nc.dram_tensor("KT_s", [4, 128, LLOC], BF16).ap()
    V_s = nc.dram_tensor("V_s", [LLOC, 4, 65], BF16).ap()
    KcT_s = nc.dram_tensor("KcT_s", [128, NCT * 128], BF16).ap()

    hT_s = nc.dram_tensor("hT_s", [NQB + 1, 128, 8, 128], BF16).ap()

    with ExitStack() as st:
        S = Sched(nc, st)
        sb = lambda name, shape, dt: st.enter_context(nc.sbuf_tensor(name, shape, dt))
        ps = lambda name, shape, dt: st.enter_context(nc.psum_tensor(name, shape, dt))

        arena_t = sb("arena", [128, 43000], F32)
        ar = Arena(S, arena_t[:])
        def tb(name, shape, dt):
            REG[name] = (ar.off, shape, str(dt))
            return ar.alloc(shape, dt)
        identf = sb("identf", [128, 128], F32)
        ident = sb("ident", [128, 128], BF16)
        onesf = sb("onesf", [128, 128], F32)
        S.op("pool", lambda e: e.memset(identf[:], 0.0), writes=["identf"])
        S.op("pool", lambda e: e.affine_select(out=identf[:], in_=identf[:], pattern=[[-1, 128]],
                                               compare_op=ALU.not_equal, fill=1.0, base=0, channel_multiplier=1),
             reads=["identf"], writes=["identf"])
        S.op("pool", lambda e: e.tensor_copy(out=ident[:], in_=identf[:]), reads=["identf"], writes=["ident"])
        S.op("pool", lambda e: e.memset(onesf[:], 1.0), writes=["onesf"])

        pAB = ps("pAB", [128, 1024], F32)
        pA = pAB[:, 0:512]
        pB = pAB[:, 512:1024]
        pC = ps("pz2", [128, 512], F32)
        pD = ps("pz3", [128, 512], F32)
        pT = ps("pT", [128, 8, 128], BF16)
        pTf = ps("pTf", [128, 8, 64], F32)

        mod = sb("mod", [33, 6 * D], F32)
        NW1 = 768 + 1536 + 520
        m_passA = ar.mark()
        winb = tb("winb", [128, 8, NW1], BF16)
        gs1p = tb("gs1p", [128, D], F32)
        sh1p = tb("sh1p", [128, D], F32)
        m_ada = ar.mark()
        c_in = tb("c_in", [33, D], F32)
        cT = tb("cT", [128, 8, 33], F32)
        badab = tb("badab", [33, 6 * D], F32)
        g1b = tb("g1b", [33, D], F32)
        gs1 = sb("gs1", [33, D], F32)
        wst = [tb("wst%d" % i, [128, 8, 512], F32) for i in range(2)]
        S.dma("sp", "ld_c", c_in[:], call[:, :], writes=["c_in"])
        S.dma("sp", "ld_bada", badab[:], b_ada[0:1, :].partition_broadcast(33), writes=["badab"])
        S.dma("sp", "ld_g1", g1b[:], norm1_g[0:1, :].partition_broadcast(33), writes=["g1b"])
        S.op("act", lambda e: e.activation(out=c_in[:], in_=c_in[:], func=AF.Silu), reads=["c_in"], writes=["c_in"])
        for k in range(8):
            S.op("pe", lambda e, k=k: e.transpose(out=pTf[:, k, 0:33], in_=c_in[:, k * 128:(k + 1) * 128],
                                                  identity=identf[0:33, 0:33]),
                 reads=["c_in", "identf"], writes=["pTf"])
        S.op("dve", lambda e: e.tensor_copy(out=cT[:], in_=pTf[:, :, 0:33]), reads=["pTf"], writes=["cT"])
        w_ada_v = w_ada.rearrange("(k p) n -> p k n", p=128)
        pmod = [pA, pB]
        for g in range(12):
            wb_ = wst[g % 2]
            wk = "wst%d" % (g % 2)
            S.dma("sp", "ld_" + wk, wb_[:], w_ada_v[:, :, g * 512:(g + 1) * 512], writes=[wk])
            pm = pmod[g % 2]
            pk = "pz%d" % (g % 2)
            for k in range(8):
                S.op("pe", lambda e, k=k, wb_=wb_, pm=pm: e.matmul(pm[0:33, :], lhsT=cT[:, k, :], rhs=wb_[:, k, :],
                                                                 start=(k == 0), stop=(k == 7)),
                     reads=["cT", wk], writes=[pk])
            S.op("dve", lambda e, g=g, pm=pm: e.tensor_tensor(out=mod[:, g * 512:(g + 1) * 512], in0=pm[0:33, :],
                                                            in1=badab[:, g * 512:(g + 1) * 512], op=ALU.add),
                 reads=[pk, "badab"], writes=["mod%d" % g])
        modk = lambda a, b: ["mod%d" % g for g in range(a // 512, (b + 511) // 512)]
        S.op("dve", lambda e: e.scalar_tensor_tensor(out=gs1[:], in0=mod[:, D:2 * D], scalar=1.0, in1=g1b[:],
                                                     op0=ALU.add, op1=ALU.mult),
             reads=modk(D, 2 * D) + ["g1b"], writes=["gs1"])

        def bcast_row(dst, dkey, src_ap, skeys):
            for hh in range(2):
                S.op("pe", lambda e, hh=hh: e.matmul(pC[:, :], lhsT=onesf[32:33, :], rhs=src_ap[:, hh * 512:(hh + 1) * 512],
                                                     start=True, stop=True),
                     reads=skeys + ["onesf"], writes=["pz2"])
                S.op("act", lambda e, hh=hh: e.copy(out=dst[:, hh * 512:(hh + 1) * 512], in_=pC[:, :]),
                     reads=["pz2"], writes=[dkey])

        bcast_row(gs1p, "gs1p", gs1[32:33, :], ["gs1"])
        bcast_row(sh1p, "sh1p", mod[32:33, 0:D], modk(0, D))
        gs2 = sb("gs2", [33, D], F32)
        S.dma("sp", "ld_g1", g1b[:], norm2_g[0:1, :].partition_broadcast(33), reads=["gs1"], writes=["g1b"])
        S.op("dve", lambda e: e.scalar_tensor_tensor(out=gs2[:], in0=mod[:, 4 * D:5 * D], scalar=1.0, in1=g1b[:],
                                                     op0=ALU.add, op1=ALU.mult),
             reads=modk(4 * D, 5 * D) + ["g1b"], writes=["gs2"])

        w_in_v = w_in.rearrange("(k p) n -> p k n", p=128)
        col_src = [(O_KV, 512), (O_KV + 512, 256), (O_QKV, 512), (O_QKV + 512, 512), (O_QKV + 1024, 512),
                   (O_A, 8), (O_GATE, 512)]
        off = 0
        wcol = []
        for i, (c0, n) in enumerate(col_src):
            wb_ = wst[i % 2]
            wk = "wst%d" % (i % 2)
            S.dma("sp", "ld_" + wk, wb_[:, :, 0:n], w_in_v[:, :, c0:c0 + n], writes=[wk])
            eng = "pool" if i % 2 else "dve"
            S.op(eng, lambda e, wb_=wb_, n=n, off=off: e.tensor_copy(out=winb[:, :, off:off + n], in_=wb_[:, :, 0:n]),
                 reads=[wk], writes=["winb%d" % i])
            wcol.append((off, n, "winb%d" % i))
            off += n

        ar.release(m_ada)
        xts = [tb("xt%d" % i, [128, D], F32) for i in range(2)]
        junk = tb("junk", [128, D], BF16)
        ssq = [tb("ssq%d" % i, [128, 1], F32) for i in range(2)]
        tmpf = tb("tmpf", [128, D], F32)
        hb = tb("hb", [128, D], BF16)
        hTs = [tb("hT%d" % i, [128, 8, 128], BF16) for i in range(2)]
        _z0 = tb("z0", [128, NW1], F32)
        zs = [_z0, _z0]
        pz = [pA, pB, pC, pD]
        cnt = {"t": 0}

        def front(P, x_ap, gs_ap, sh_ap, gkeys, outs, vcol=None, groups=None):
            i = cnt["t"] % 2
            cnt["t"] += 1
            xt, xk = xts[i], "xt%d" % i
            sq, sk = ssq[i], "ssq%d" % i
            hT, hk = hTs[i], "hT%d" % i
            z, zk = zs[i], "z0"
            S.dma("sp", "ld_" + xk, xt[0:P, :], x_ap, writes=[xk])
            S.op("act", lambda e: e.activation(out=junk[0:P, :], in_=xt[0:P, :], func=AF.Square, accum_out=sq[0:P, :]),
                 reads=[xk], writes=["junk", sk])
            S.op("act", lambda e: e.activation(out=sq[0:P, :], in_=sq[0:P, :], func=AF.Sqrt, scale=1.0 / D, bias=EPS),
                 reads=[sk], writes=[sk])
            S.op("dve", lambda e: e.reciprocal(out=sq[0:P, :], in_=sq[0:P, :]), reads=[sk], writes=[sk])
            S.op("dve", lambda e: e.scalar_tensor_tensor(out=tmpf[0:P, :], in0=xt[0:P, :], scalar=sq[0:P, 0:1], in1=gs_ap,
                                                         op0=ALU.mult, op1=ALU.mult),
                 reads=[xk, sk] + gkeys, writes=["tmpf"])
            S.op("pool", lambda e: e.tensor_tensor(out=hb[0:P, :], in0=tmpf[0:P, :], in1=sh_ap, op=ALU.add),
                 reads=["tmpf"] + gkeys, writes=["hb"])
            if vcol is not None:
                S.op("dve", lambda e: e.tensor_scalar(out=hb[0:P, :], in0=hb[0:P, :], scalar1=vcol, scalar2=None,
                                                      op0=ALU.mult), reads=["hb", "vmask"], writes=["hb"])
            for k in range(8):
                S.op("pe", lambda e, k=k: e.transpose(out=pT[:, k, 0:P], in_=hb[0:P, k * 128:(k + 1) * 128],
                                                      identity=ident[0:P, 0:P]),
                     reads=["hb", "ident"], writes=["pT"])
            S.op("act", lambda e: e.copy(out=hT[:, :, 0:P], in_=pT[:, :, 0:P]), reads=["pT"], writes=[hk])
            for j, (off, n, wkey) in enumerate(wcol):
                if groups is not None and j not in groups:
                    continue
                pp = pz[j % 4]
                pk = "pz%d" % (j % 4)
                for k in range(8):
                    S.op("pe", lambda e, k=k, pp=pp, off=off, n=n: e.matmul(pp[0:P, 0:n], lhsT=hT[:, k, 0:P],
                                                                            rhs=winb[:, k, off:off + n],
                                                                            start=(k == 0), stop=(k == 7)),
                         reads=[hk, wkey], writes=[pk])
                eng = "act" if j % 2 else "dve"
                if eng == "act":
                    S.op("act", lambda e, pp=pp, off=off, n=n: e.copy(out=z[0:P, off:off + n], in_=pp[0:P, 0:n]),
                         reads=[pk], writes=[zk])
                else:
                    S.op("dve", lambda e, pp=pp, off=off, n=n: e.tensor_copy(out=z[0:P, off:off + n], in_=pp[0:P, 0:n]),
                         reads=[pk], writes=[zk])
            outs(z, zk)
            return hT, hk, z, zk


        ZQ, ZA, ZG = 768, 768 + 1536, 768 + 1536 + 8

        def sample_delta(z, zk):
            P = SB_
            m_sd = ar.mark()
            acc = tb("sd_acc", [P, 1536], F32)
            sm = tb("sd_sm", [P, 64], F32)
            alb = tb("sd_alb", [P, 8], F32)
            onb = tb("sd_onb", [P, 128], F32)
            m_sd2 = ar.mark()
            cst = tb("sd_cst", [P, 3, 1536], F32)
            cwr = tb("sd_cwr", [P, 1536], F32)
            tmp = tb("sd_tmp", [P, 1536], F32)
            S.dma("sp", "ld_sd1", cst[:], sconv[:, :, :], writes=["sd_cst"])
            S.dma("sp", "ld_sd3", alb[:, 0:4], a_log[0:1, :].partition_broadcast(P), writes=["sd_alb"])
            S.dma("sp", "ld_sd4", alb[:, 4:8], dt_bias[0:1, :].partition_broadcast(P), writes=["sd_alb"])
            S.dma("sp", "ld_sd5", onb[:], o_norm_g[0:1, :].partition_broadcast(P), writes=["sd_onb"])
            for j in range(4):
                S.dma("sp", "ld_sd2", cwr[:], conv_w[j:j + 1, :].partition_broadcast(P), writes=["sd_cwr"])
                row = cst[:, j, :] if j < 3 else z[0:P, ZQ:ZQ + 1536]
                rkeys = ["sd_cst"] if j < 3 else [zk]
                if j == 0:
                    S.op("dve", lambda e, row=row: e.tensor_tensor(out=acc[:], in0=row, in1=cwr[:], op=ALU.mult),
                         reads=rkeys + ["sd_cwr"], writes=["sd_acc"])
                else:
                    S.op("dve", lambda e, row=row: e.tensor_tensor(out=tmp[:], in0=row, in1=cwr[:], op=ALU.mult),
                         reads=rkeys + ["sd_cwr"], writes=["sd_tmp"])
                    S.op("dve", lambda e: e.tensor_tensor(out=acc[:], in0=acc[:], in1=tmp[:], op=ALU.add),
                         reads=["sd_tmp", "sd_acc"], writes=["sd_acc"])
            S.op("act", lambda e: e.activation(out=acc[:], in_=acc[:], func=AF.Silu), reads=["sd_acc"], writes=["sd_acc"])
            S.op("dve", lambda e: e.tensor_tensor(out=tmp[:, 0:1024], in0=acc[:, 0:1024], in1=acc[:, 0:1024], op=ALU.mult),
                 reads=["sd_acc"], writes=["sd_tmp"])
            S.op("dve", lambda e: e.tensor_reduce(out=sm[:, 0:8], in_=tmp[:, 0:1024].rearrange("p (h d) -> p h d", d=128),
                                                  axis=AX.X, op=ALU.add),
                 reads=["sd_tmp"], writes=["sd_sm"])
            S.op("act", lambda e: e.activation(out=sm[:, 0:8], in_=sm[:, 0:8], func=AF.Sqrt, bias=EPS),
                 reads=["sd_sm"], writes=["sd_sm"])
            S.op("dve", lambda e: e.reciprocal(out=sm[:, 0:8], in_=sm[:, 0:8]), reads=["sd_sm"], writes=["sd_sm"])
            S.op("dve", lambda e: e.tensor_scalar(out=sm[:, 0:4], in0=sm[:, 0:4], scalar1=128.0 ** -0.5, scalar2=None,
                                                  op0=ALU.mult), reads=["sd_sm"], writes=["sd_sm"])
            S.op("dve", lambda e: e.tensor_tensor(out=acc[:, 0:1024].rearrange("p (h d) -> p h d", d=128),
                                                  in0=acc[:, 0:1024].rearrange("p (h d) -> p h d", d=128),
                                                  in1=sm[:, 0:8].unsqueeze(2).broadcast_to([P, 8, 128]), op=ALU.mult),
                 reads=["sd_sm", "sd_acc"], writes=["sd_acc"])
            ar.release(m_sd2)
            xx, ax, sp_, ea = sm[:, 16:20], sm[:, 20:24], sm[:, 24:28], sm[:, 28:32]
            S.op("dve", lambda e: e.tensor_tensor(out=xx, in0=z[0:P, ZA:ZA + 4], in1=alb[:, 4:8], op=ALU.add),
                 reads=[zk, "sd_alb"], writes=["sd_sm"])
            S.op("dve", lambda e: e.scalar_tensor_tensor(out=ax, in0=xx, scalar=-1.0, in1=xx, op0=ALU.mult, op1=ALU.max),
                 reads=["sd_sm"], writes=["sd_sm"])
            S.op("act", lambda e: e.activation(out=ax, in_=ax, func=AF.Exp, scale=-1.0), reads=["sd_sm"], writes=["sd_sm"])
            S.op("act", lambda e: e.activation(out=ax, in_=ax, func=AF.Ln, bias=1.0), reads=["sd_sm"], writes=["sd_sm"])
            S.op("dve", lambda e: e.scalar_tensor_tensor(out=sp_, in0=xx, scalar=0.0, in1=ax, op0=ALU.max, op1=ALU.add),
                 reads=["sd_sm"], writes=["sd_sm"])
            S.op("act", lambda e: e.activation(out=ea, in_=alb[:, 0:4], func=AF.Exp), reads=["sd_alb"], writes=["sd_sm"])
            S.op("dve", lambda e: e.scalar_tensor_tensor(out=sm[:, 8:12], in0=ea, scalar=-1.0, in1=sp_,
                                                         op0=ALU.mult, op1=ALU.mult), reads=["sd_sm"], writes=["sd_sm"])
            S.op("act", lambda e: e.activation(out=sm[:, 8:12], in_=sm[:, 8:12], func=AF.Exp), reads=["sd_sm"], writes=["sd_sm"])
            S.op("act", lambda e: e.activation(out=sm[:, 12:16], in_=z[0:P, ZA + 4:ZA + 8], func=AF.Sigmoid),
                 reads=[zk], writes=["sd_sm"])
            if STAGE < 2:
                ar.release(m_sd); return
            scr = nc.dram_tensor("sd_scr", [P, 1536 + 8], F32).ap()
            S.dma("sp", "st_sd1", scr[:, 0:1536], acc[:], reads=["sd_acc"], writes=["sd_scr"])
            S.dma("sp", "st_sd2", scr[:, 1536:1544], sm[:, 8:16], reads=["sd_sm"], writes=["sd_scr"])
            egb = tb("sd_egb", [128, P, 8], F32)
            krow = tb("sd_krow", [1, P, 512], F32)
            vrow = tb("sd_vrow", [1, P, 512], F32)
            S.dma("sp", "ld_sd6", egb[:], scr[:, 1536:1544].rearrange("(o s) e -> o s e", o=1).partition_broadcast(128)
                  if False else scr[:, 1536:1544].partition_broadcast(128), reads=["sd_scr"], writes=["sd_egb"])
            S.dma("sp", "ld_sd7", krow[:], scr[:, 512:1024].rearrange("(o s) e -> o s e", o=1), reads=["sd_scr"], writes=["sd_krow"])
            S.dma("sp", "ld_sd8", vrow[:], scr[:, 1024:1536].rearrange("(o s) e -> o s e", o=1), reads=["sd_scr"], writes=["sd_vrow"])
            if STAGE < 3:
                ar.release(m_sd); return
            qkT = tb("sd_qkT", [128, 8, P], F32)
            for j in range(8):
                S.op("pe", lambda e, j=j: e.transpose(out=pTf[:, j, 0:P], in_=acc[:, j * 128:(j + 1) * 128],
                                                      identity=identf[0:P, 0:P]),
                     reads=["sd_acc", "identf"], writes=["pTf"])
            S.op("dve", lambda e: e.tensor_copy(out=qkT[:], in_=pTf[:, :, 0:P]), reads=["pTf"], writes=["sd_qkT"])
            S0 = tb("sd_S0", [128, P * 4, 128], F32)
            S1 = S0
            for s_ in range(P):
                S.dma("sp", "ld_sd9", S0[:, s_ * 4:(s_ + 1) * 4, :], sdelta[s_].rearrange("h k v -> k h v"), writes=["sd_S0"])
            if STAGE < 4:
                ar.release(m_sd); return
            urow = tb("sd_urow", [1, P * 4, 128], F32)
            orow = vrow.rearrange("o s (h d) -> o (s h) d", d=128)
            for s_ in range(P):
                for h in range(4):
                    sh = s_ * 4 + h
                    S.op("pe", lambda e, s_=s_, h=h, sh=sh: e.matmul(pD[0:1, 0:128], lhsT=qkT[:, 4 + h, s_:s_ + 1],
                                                                    rhs=S0[:, sh, :], start=True, stop=True),
                         reads=["sd_qkT", "sd_S0"], writes=["pz3"])
                    S.op("dve", lambda e, s_=s_, h=h, sh=sh: e.scalar_tensor_tensor(
                        out=urow[0:1, sh, :], in0=pD[0:1, 0:128], scalar=egb[0:1, s_, h:h + 1],
                        in1=vrow[0:1, s_, h * 128:(h + 1) * 128], op0=ALU.mult, op1=ALU.subtract),
                        reads=["pz3", "sd_egb", "sd_vrow"], writes=["sd_urow"])
                    S.op("dve", lambda e, s_=s_, h=h, sh=sh: e.tensor_scalar(
                        out=urow[0:1, sh, :], in0=urow[0:1, sh, :], scalar1=egb[0:1, s_, 4 + h:5 + h], scalar2=-1.0,
                        op0=ALU.mult, op1=ALU.mult), reads=["sd_urow", "sd_egb"], writes=["sd_urow"])
                    S.op("pe", lambda e, s_=s_, h=h, sh=sh: e.matmul(pC[:, 0:128], lhsT=krow[0:1, s_, h * 128:(h + 1) * 128],
                                                                    rhs=urow[0:1, sh, :], start=True, stop=True),
                         reads=["sd_krow", "sd_urow"], writes=["pz2"])
                    S.op("dve", lambda e, s_=s_, h=h, sh=sh: e.scalar_tensor_tensor(
                        out=S1[:, sh, :], in0=S0[:, sh, :], scalar=egb[:, s_, h:h + 1], in1=pC[:, 0:128],
                        op0=ALU.mult, op1=ALU.add), reads=["sd_S0", "sd_egb", "pz2"], writes=["sd_S0"])
                    S.op("pe", lambda e, s_=s_, h=h, sh=sh: e.matmul(pD[0:1, 128:256], lhsT=qkT[:, h, s_:s_ + 1],
                                                                    rhs=S1[:, sh, :], start=True, stop=True),
                         reads=["sd_qkT", "sd_S0"], writes=["pz3"])
                    S.op("act", lambda e, sh=sh: e.copy(out=orow[0:1, sh, :], in_=pD[0:1, 128:256]),
                         reads=["pz3"], writes=["sd_vrow"])
            if STAGE < 5:
                ar.release(m_sd); return
            for s_ in range(P):
                S.dma("pool", "st_ds", o_ds[s_].rearrange("h k v -> k h v"), S1[:, s_ * 4:(s_ + 1) * 4, :], reads=["sd_S0"])
            scr2 = nc.dram_tensor("sd_scr2", [P, 512], F32).ap()
            S.dma("sp", "st_sd3", scr2.rearrange("(o s) e -> o s e", o=1), vrow[:], reads=["sd_vrow"], writes=["sd_scr2"])
            od_s = tb("sd_od", [P, 4, 128], F32)
            S.dma("sp", "ld_sd10", od_s[:], scr2.rearrange("s (h d) -> s h d", d=128), reads=["sd_scr2"], writes=["sd_od"])
            emit_ob(P, od_s, "sd_od", z[0:P, 2312:2824], [zk], NQB)
            ar.release(m_sd)

        def outs_sample(z, zk):
            S.dma("pool", "st_kvs", o_kvs[:, :], z[0:SB_, 0:512], reads=[zk], writes=["o_kvs"])
            S.dma("pool", "st_wins", o_wins[:, 511, :], z[0:SB_, 512:768], reads=[zk], writes=["o_wins"])
            S.dma("pool", "st_convs", o_convs[:, 2, :], z[0:SB_, 768:768 + 1536], reads=[zk])

        S.dma("pool", "cp_win", o_wins[:, 0:511, :], cwin[:, 1:512, :])
        S.dma("pool", "cp_conv", o_convs[:, 0:2, :], sconv[:, 1:3, :])

        pE = ps("pE", [128, 512], F32)
        pF = ps("pF", [128, 512], F32)
        banks = [(pA, "pz0"), (pB, "pz1"), (pC, "pz2"), (pD, "pz3"), (pE, "pE"), (pF, "pF")]
        bctr = {"i": 0}

        def nextbank():
            t = banks[bctr["i"] % len(banks)]
            bctr["i"] += 1
            return t

        vmask = sb("vmask_sb", [128, 8], F32)
        S.dma("sp", "ld_vm", vmask[:], vmask_d[:, :], writes=["vmask"])
        onesb = sb("onesb", [128, 128], BF16)
        negones = tb("negones", [128, 128], F32)
        triU = tb("triU", [128, 128], F32)
        NMs = tb("NMs", [128, 128], F32)
        NMi = tb("NMi", [128, 128], F32)
        S.op("pool", lambda e: e.memset(onesb[:], 1.0), writes=["onesb"])
        S.op("pool", lambda e: e.memset(negones[:], -1.0), writes=["negones"])
        S.op("pool", lambda e: e.memset(triU[:], 1.0), writes=["triU"])
        S.op("pool", lambda e: e.affine_select(out=triU[:], in_=triU[:], pattern=[[1, 128]], compare_op=ALU.is_ge,
                                               fill=0.0, base=0, channel_multiplier=-1), reads=["triU"], writes=["triU"])
        S.op("pool", lambda e: e.memset(NMs[:], 0.0), writes=["NMs"])
        S.op("pool", lambda e: e.affine_select(out=NMs[:], in_=NMs[:], pattern=[[-1, 128]], compare_op=ALU.is_ge,
                                               fill=-30000.0, base=-1, channel_multiplier=1), reads=["NMs"], writes=["NMs"])
        S.op("pool", lambda e: e.memset(NMi[:], 0.0), writes=["NMi"])
        S.op("pool", lambda e: e.affine_select(out=NMi[:], in_=NMi[:], pattern=[[1, 128]], compare_op=ALU.is_ge,
                                               fill=-30000.0, base=0, channel_multiplier=-1), reads=["NMi"], writes=["NMi"])
        cwT = tb("cwT", [128, 12, 4], F32)
        dw = tb("dw", [128, 12, 4, 128], BF16)
        for tap in range(4):
            S.dma("sp", "ld_cw", cwT[:, :, tap], conv_w[tap, :].rearrange("(c p) -> p c", p=128), writes=["cwT"],
                  allow_slow_non_contiguous=True)
        for ct in range(12):
            for tap in range(4):
                S.op("pool" if (ct + tap) % 2 else "dve",
                     lambda e, ct=ct, tap=tap: e.tensor_scalar(out=dw[:, ct, tap, :], in0=identf[:], scalar1=cwT[:, ct, tap:tap + 1],
                                                               scalar2=None, op0=ALU.mult),
                     reads=["identf", "cwT"], writes=["dw"])
        albp = tb("albp", [128, 8], F32)
        S.dma("sp", "ld_alb1", albp[:, 0:4], a_log[0:1, :].partition_broadcast(128), writes=["albp"])
        S.dma("sp", "ld_alb2", albp[:, 4:8], dt_bias[0:1, :].partition_broadcast(128), writes=["albp"])
        eAp = tb("eAp", [128, 4], F32)
        S.op("act", lambda e: e.activation(out=eAp[:], in_=albp[:, 0:4], func=AF.Exp), reads=["albp"], writes=["eAp"])
        Sf = tb("Sf", [128, 4, 128], F32)
        Sb = tb("Sb", [128, 4, 128], BF16)
        S.op("pool", lambda e: e.memset(Sf[:], 0.0), writes=["Sf"])
        S.op("pool", lambda e: e.memset(Sb[:], 0.0), writes=["Sb"])

        onp = tb("onp", [128, 128], F32)
        S.dma("sp", "ld_onp", onp[:], o_norm_g[0:1, :].partition_broadcast(128), writes=["onp"])
        ob_t1 = tb("ob_t1", [128, 4, 128], F32)
        ob_sg = tb("ob_sg", [128, 512], F32)
        ob_ms = tb("ob_ms", [128, 4], F32)
        ob_bf = tb("ob_bf", [128, 512], BF16)

        def emit_ob(P, od, odk, gate_ap, gkeys, slot):
            S.op("pool", lambda e: e.tensor_tensor(out=ob_t1[0:P], in0=od[0:P], in1=od[0:P], op=ALU.mult),
                 reads=[odk], writes=["ob_t1"])
            S.op("dve", lambda e: e.tensor_reduce(out=ob_ms[0:P, :], in_=ob_t1[0:P], axis=AX.X, op=ALU.add),
                 reads=["ob_t1"], writes=["ob_ms"])
            S.op("act", lambda e: e.activation(out=ob_ms[0:P, :], in_=ob_ms[0:P, :], func=AF.Sqrt, scale=1.0 / 128, bias=EPS),
                 reads=["ob_ms"], writes=["ob_ms"])
            S.op("dve", lambda e: e.reciprocal(out=ob_ms[0:P, :], in_=ob_ms[0:P, :]), reads=["ob_ms"], writes=["ob_ms"])
            S.op("dve", lambda e: e.tensor_tensor(out=ob_t1[0:P], in0=od[0:P],
                                                  in1=ob_ms[0:P, :].unsqueeze(2).broadcast_to([P, 4, 128]), op=ALU.mult),
                 reads=[odk, "ob_ms"], writes=["ob_t1"])
            S.op("pool", lambda e: e.tensor_tensor(out=ob_t1[0:P], in0=ob_t1[0:P],
                                                   in1=onp[0:P, :].unsqueeze(1).broadcast_to([P, 4, 128]), op=ALU.mult),
                 reads=["ob_t1", "onp"], writes=["ob_t1"])
            S.op("act", lambda e: e.activation(out=ob_sg[0:P, :], in_=gate_ap, func=AF.Silu), reads=gkeys, writes=["ob_sg"])
            S.op("dve", lambda e: e.tensor_tensor(out=ob_bf[0:P, :], in0=ob_t1[0:P].rearrange("p c t -> p (c t)"),
                                                  in1=ob_sg[0:P, :], op=ALU.mult), reads=["ob_t1", "ob_sg"], writes=["ob_bf"])
            S.dma("pool", "st_ob", ob_s[slot, 0:P, :], ob_bf[0:P, :], reads=["ob_bf"], writes=["ob_s%d" % slot])

        hTsamp, hksamp, zsamp, zsk = front(SB_, xs[:, :], gs1[0:SB_, :], mod[0:SB_, 0:D], ["gs1"] + modk(0, D), outs_sample)
        S.dma("pool", "st_hT", hT_s[NQB, :, :, 0:SB_], hTsamp[:, :, 0:SB_], reads=[hksamp], writes=["hT_s%d" % NQB])
        sample_delta(zsamp, zsk)

        def kv_tiles():
            KV = dict(kng=tb("kng", [128, 2, 64], F32), sq=tb("kv_sq", [128, 2, 2, 64], F32), ms=tb("kv_ms", [128, 2, 2], F32),
                      kn=tb("kv_kn", [128, 2, 2, 64], BF16), raw=tb("kv_raw", [128, 256], BF16), kT=tb("kT_st", [128, 4, 128], BF16),
                      vst=tb("vst", [128, 4, 65], BF16))
            S.dma("sp", "ld_kng", KV["kng"][:], k_norm_g[1:3, :].partition_broadcast(128), writes=["kng"])
            S.op("pool", lambda e: e.memset(KV["vst"][:], 1.0), writes=["vst"])
            return KV

        def kvprep(j, z, zk, KV):
            kng, kv_sq, kv_ms, kv_kn, kv_raw, kT_st, vst = KV["kng"], KV["sq"], KV["ms"], KV["kn"], KV["raw"], KV["kT"], KV["vst"]
            kview = z[:, 256:768].rearrange("p (s r) -> p s r", r=256)[:, :, 0:128].rearrange("p s (k d) -> p s k d", d=64)
            vview = z[:, 256:768].rearrange("p (s r) -> p s r", r=256)[:, :, 128:256].rearrange("p s (k d) -> p s k d", d=64)
            S.op("pool", lambda e: e.tensor_tensor(out=kv_sq[:], in0=kview, in1=kview, op=ALU.mult), reads=[zk], writes=["kv_sq"])
            S.op("dve", lambda e: e.tensor_reduce(out=kv_ms[:], in_=kv_sq[:], axis=AX.X, op=ALU.add), reads=["kv_sq"], writes=["kv_ms"])
            S.op("act", lambda e: e.activation(out=kv_ms[:], in_=kv_ms[:], func=AF.Sqrt, scale=1.0 / 64, bias=EPS),
                 reads=["kv_ms"], writes=["kv_ms"])
            S.op("dve", lambda e: e.reciprocal(out=kv_ms[:], in_=kv_ms[:]), reads=["kv_ms"], writes=["kv_ms"])
            S.op("dve", lambda e: e.tensor_tensor(out=kv_sq[:], in0=kview, in1=kv_ms[:].unsqueeze(3).broadcast_to([128, 2, 2, 64]),
                                                  op=ALU.mult), reads=[zk, "kv_ms", "kv_sq"], writes=["kv_sq"])
            S.op("pool", lambda e: e.tensor_tensor(out=kv_kn[:], in0=kv_sq[:], in1=kng[:].unsqueeze(2).broadcast_to([128, 2, 2, 64]),
                                                   op=ALU.mult), reads=["kv_sq", "kng"], writes=["kv_kn"])
            S.op("act", lambda e: e.copy(out=kv_raw[:], in_=z[:, 0:256]), reads=[zk], writes=["kv_raw"])
            srcs = [kv_kn[:, 0].rearrange("p k d -> p (k d)"), kv_kn[:, 1].rearrange("p k d -> p (k d)"), kv_raw[:, 0:128], kv_raw[:, 128:256]]
            for ti, s_ in enumerate(srcs):
                S.op("pe", lambda e, ti=ti, s_=s_: e.transpose(out=pT[:, ti, :], in_=s_, identity=ident[:]),
                     reads=["kv_kn", "kv_raw", "ident"], writes=["pT"])
            S.op("act", lambda e: e.copy(out=kT_st[:], in_=pT[:, 0:4, :]), reads=["pT"], writes=["kT_st"])
            S.dma("pool", "st_KT", KT_s[:, :, j * 128:(j + 1) * 128].rearrange("t p c -> p t c"), kT_st[:], reads=["kT_st"], writes=["KT_s"])
            S.op("pool", lambda e: e.tensor_copy(out=vst[:].rearrange("p (s k) c -> p s k c", k=2)[:, :, :, 0:64], in_=vview),
                 reads=[zk, "vst"], writes=["vst"])
            S.dma("pool", "st_V", V_s[j * 128:(j + 1) * 128, :, :], vst[:], reads=["vst"], writes=["V_s"])

        KVA = kv_tiles()
        if NBLK < NBLKT:
            zfill = tb("zfill", [128, 2048], BF16)
            S.op("pool", lambda e: e.memset(zfill[:], 0.0), writes=["zfill"])
            for ty in range(4):
                for c0 in range(0, LLOC, 2048):
                    n = min(2048, LLOC - c0)
                    S.dma("sp", "zfK", KT_s[ty, :, c0:c0 + n], zfill[:, 0:n], reads=["zfill"], writes=["KT_s"])
            for r0 in range(0, LLOC, 128 * 7):
                n = min(128 * 7, LLOC - r0)
                S.dma("sp", "zfV", V_s[r0:r0 + n].rearrange("(t p) a c -> p t (a c)", p=128), zfill[:, 0:(n // 128) * 260].rearrange("p (t x) -> p t x", x=260),
                      reads=["zfill"], writes=["V_s"])

        zT = {nm: [tb("zT", [128, 4, 131], BF16) for _ in range(2)] for nm in ("k", "v")}
        zTq = tb("zTq", [128, 4, 131], BF16)
        for nm in ("k", "v"):
            for b_ in range(2):
                S.op("pool", lambda e, nm=nm, b_=b_: e.memset(zT[nm][b_][:], 0.0), writes=["zT%s%d" % (nm, b_)])
        cTk = tb("cTk", [128, 4, 128], F32)
        cTq = tb("cTq", [128, 4, 128], F32)
        cTv = tb("cTv", [128, 4, 128], BF16)
        sqb = tb("sqb", [128, 512], BF16)
        rn = tb("rn", [128, 512], F32)
        knT = tb("knT", [128, 4, 128], BF16)
        qnT = [tb("qnT", [128, 4, 128], BF16) for _ in range(2)]
        ktv = tb("ktv", [128, 8, 128], BF16)
        gsm = tb("gsm", [128, 64], F32)
        Tg = tb("Tg", [128, 4, 128], F32)
        Esb = tb("Esb", [128, 4, 128], F32)
        ETsb = tb("ETsb", [128, 4, 128], F32)
        Nk = [tb("Nk", [128, 4, 128], BF16) for _ in range(2)]
        Uk = [tb("Uk", [128, 4, 128], BF16) for _ in range(2)]
        Rt = [tb("Rt", [128, 4, 128], BF16) for _ in range(2)]
        kbv = tb("kbv", [128, 8, 128], BF16)
        rec = [dict(u=tb("u", [128, 4, 128], F32), wT=tb("wT", [128, 4, 128], BF16), kd=tb("kd", [128, 4, 128], BF16),
                    QKT=tb("QKT", [128, 4, 128], BF16), sc=tb("sc", [128, 8], F32)) for _ in range(2)]
        vnew = tb("vnew", [128, 4, 128], BF16)
        o2sb = tb("o2sb", [128, 4, 128], F32)
        osb = tb("osb", [128, 4, 128], F32)

        def delta_prep(j, hT, hk, hTp, hkp, own):
            b_ = j % 2
            R_ = rec[b_]
            rk = "rec%d" % b_
            groups = [("k", 4), ("v", 8)] + ([("q", 0)] if own else [])
            for nm, ct0 in groups:
                pz_, pzk = nextbank()
                zt = zTq if nm == "q" else zT[nm][b_]
                ztk = "zTq" if nm == "q" else "zT%s%d" % (nm, b_)
                for ct in range(4):
                    c0 = 768 + (ct0 + ct) * 128
                    for k in range(8):
                        S.op("pe", lambda e, ct=ct, k=k, c0=c0, pz_=pz_: e.matmul(
                            pz_[:, ct * 128:(ct + 1) * 128], lhsT=winb[:, k, c0:c0 + 128], rhs=hT[:, k, :],
                            start=(k == 0), stop=(k == 7)), reads=["winb2", "winb3", "winb4", hk], writes=[pzk])
                S.op("act", lambda e, pz_=pz_, zt=zt: e.copy(out=zt[:, :, 3:131], in_=pz_[:, :].rearrange("p (c t) -> p c t", t=128)),
                     reads=[pzk], writes=[ztk])
                if nm == "q":
                    pz2, pzk2 = nextbank()
                    for ct in range(4):
                        c0 = 768 + ct * 128
                        for k in range(8):
                            S.op("pe", lambda e, ct=ct, k=k, c0=c0, pz2=pz2: e.matmul(
                                pz2[:, ct * 4:ct * 4 + 3], lhsT=winb[:, k, c0:c0 + 128], rhs=hTp[:, k, 125:128],
                                start=(k == 0), stop=(k == 7)), reads=["winb2", hkp], writes=[pzk2])
                    S.op("act", lambda e, pz2=pz2, zt=zt: e.copy(out=zt[:, :, 0:3],
                                                                in_=pz2[:, 0:16].rearrange("p (c t) -> p c t", t=4)[:, :, 0:3]),
                         reads=[pzk2], writes=[ztk])
                else:
                    prev = zT[nm][1 - b_]
                    S.op("pool", lambda e, zt=zt, prev=prev: e.tensor_copy(out=zt[:, :, 0:3], in_=prev[:, :, 128:131]),
                         reads=["zT%s%d" % (nm, 1 - b_)], writes=[ztk])
                pc_, pck = nextbank()
                for ct in range(4):
                    for tap in range(4):
                        S.op("pe", lambda e, ct=ct, tap=tap, pc_=pc_, zt=zt, ct0=ct0: e.matmul(
                            pc_[:, ct * 128:(ct + 1) * 128], lhsT=dw[:, ct0 + ct, tap, :], rhs=zt[:, ct, tap:tap + 128],
                            start=(tap == 0), stop=(tap == 3)), reads=["dw", ztk], writes=[pck])
                dst = {"k": cTk, "v": cTv, "q": cTq}[nm]
                S.op("act", lambda e, pc_=pc_, dst=dst: e.activation(out=dst[:], in_=pc_[:, :].rearrange("p (c t) -> p c t", t=128),
                                                                    func=AF.Silu), reads=[pck], writes=["cT" + nm])
            for nm in (["k", "q"] if own else ["k"]):
                src_ = cTk if nm == "k" else cTq
                dst = knT if nm == "k" else qnT[b_]
                dk_ = "knT" if nm == "k" else rk
                S.op("pool", lambda e, src_=src_: e.tensor_tensor(out=sqb[:], in0=src_[:].rearrange("p c t -> p (c t)"),
                                                                in1=src_[:].rearrange("p c t -> p (c t)"), op=ALU.mult),
                     reads=["cT" + nm], writes=["sqb"])
                pss, pssk = nextbank()
                S.op("pe", lambda e, pss=pss: e.matmul(pss[:, :], lhsT=onesb[:], rhs=sqb[:], start=True, stop=True),
                     reads=["onesb", "sqb"], writes=[pssk])
                S.op("act", lambda e, pss=pss: e.activation(out=rn[:], in_=pss[:, :], func=AF.Sqrt, bias=EPS),
                     reads=[pssk], writes=["rn"])
                S.op("dve", lambda e: e.reciprocal(out=rn[:], in_=rn[:]), reads=["rn"], writes=["rn"])
                if nm == "q":
                    S.op("dve", lambda e, src_=src_, dst=dst: e.scalar_tensor_tensor(
                        out=dst[:].rearrange("p c t -> p (c t)"), in0=src_[:].rearrange("p c t -> p (c t)"),
                        scalar=128.0 ** -0.5, in1=rn[:], op0=ALU.mult, op1=ALU.mult), reads=["cTq", "rn"], writes=[dk_])
                else:
                    S.op("dve", lambda e, src_=src_, dst=dst: e.tensor_tensor(
                        out=dst[:].rearrange("p c t -> p (c t)"), in0=src_[:].rearrange("p c t -> p (c t)"),
                        in1=rn[:], op=ALU.mult), reads=["cTk", "rn"], writes=[dk_])
            for h in range(4):
                S.op("pe", lambda e, h=h: e.transpose(out=pT[:, h, :], in_=knT[:, h, :], identity=ident[:]),
                     reads=["knT", "ident"], writes=["pT"])
                S.op("pe", lambda e, h=h: e.transpose(out=pT[:, 4 + h, :], in_=cTv[:, h, :], identity=ident[:]),
                     reads=["cTv", "ident"], writes=["pT"])
            S.op("act", lambda e: e.copy(out=ktv[:], in_=pT[:]), reads=["pT"], writes=["ktv"])
            pab, pabk = nextbank()
            for k in range(8):
                S.op("pe", lambda e, k=k, pab=pab: e.matmul(pab[:, 0:8], lhsT=hT[:, k, :], rhs=winb[:, k, 2304:2312],
                                                           start=(k == 0), stop=(k == 7)), reads=[hk, "winb5"], writes=[pabk])
            xx, ax, sp_, gc, beta, nbeta = (gsm[:, 0:4], gsm[:, 4:8], gsm[:, 8:12], gsm[:, 12:16], gsm[:, 16:20], gsm[:, 20:24])
            S.op("dve", lambda e, pab=pab: e.tensor_tensor(out=xx, in0=pab[:, 0:4], in1=albp[:, 4:8], op=ALU.add),
                 reads=[pabk, "albp"], writes=["gsm"])
            S.op("act", lambda e, pab=pab: e.activation(out=beta, in_=pab[:, 4:8], func=AF.Sigmoid), reads=[pabk], writes=["gsm"])
            S.op("dve", lambda e: e.scalar_tensor_tensor(out=ax, in0=xx, scalar=-1.0, in1=xx, op0=ALU.mult, op1=ALU.max),
                 reads=["gsm"], writes=["gsm"])
            S.op("act", lambda e: e.activation(out=ax, in_=ax, func=AF.Exp, scale=-1.0), reads=["gsm"], writes=["gsm"])
            S.op("act", lambda e: e.activation(out=ax, in_=ax, func=AF.Ln, bias=1.0), reads=["gsm"], writes=["gsm"])
            S.op("dve", lambda e: e.scalar_tensor_tensor(out=sp_, in0=xx, scalar=0.0, in1=ax, op0=ALU.max, op1=ALU.add),
                 reads=["gsm"], writes=["gsm"])
            S.op("dve", lambda e: e.scalar_tensor_tensor(out=gc, in0=eAp[:], scalar=-1.0, in1=sp_, op0=ALU.mult, op1=ALU.mult),
                 reads=["gsm", "eAp"], writes=["gsm"])
            S.op("dve", lambda e: e.tensor_scalar(out=nbeta, in0=beta, scalar1=-1.0, scalar2=None, op0=ALU.mult),
                 reads=["gsm"], writes=["gsm"])
            pG, pGk = nextbank()
            S.op("pe", lambda e, pG=pG: e.matmul(pG[:, 0:4], lhsT=triU[:], rhs=gc, start=True, stop=True),
                 reads=["triU", "gsm"], writes=[pGk])
            S.op("pe", lambda e, pG=pG: e.matmul(pG[:, 4:8], lhsT=onesf[:], rhs=gc, start=True, stop=True),
                 reads=["onesf", "gsm"], writes=[pGk])
            sc = R_["sc"]
            egl, bke = gsm[:, 24:28], gsm[:, 28:32]
            S.op("act", lambda e, pG=pG: e.activation(out=sc[:, 0:8], in_=pG[:, 0:8], func=AF.Exp), reads=[pGk], writes=[rk])
            S.op("dve", lambda e, pG=pG: e.tensor_tensor(out=egl, in0=pG[:, 4:8], in1=pG[:, 0:4], op=ALU.subtract)
                 if False else e.tensor_copy(out=egl, in_=pG[:, 0:4]), reads=[pGk], writes=["gsm"])
            S.op("dve", lambda e, pG=pG: e.scalar_tensor_tensor(out=egl, in0=egl, scalar=-1.0, in1=pG[:, 4:8],
                                                               op0=ALU.mult, op1=ALU.add), reads=[pGk, "gsm"], writes=["gsm"])
            S.op("act", lambda e: e.activation(out=egl, in_=egl, func=AF.Exp), reads=["gsm"], writes=["gsm"])
            S.op("dve", lambda e: e.tensor_tensor(out=bke, in0=beta, in1=sc[:, 0:4], op=ALU.mult), reads=["gsm", rk], writes=["gsm"])
            bc = lambda ap: ap.unsqueeze(2).broadcast_to([128, 4, 128])
            S.op("dve", lambda e: e.tensor_tensor(out=kbv[:, 0:4, :], in0=ktv[:, 0:4, :], in1=bc(bke), op=ALU.mult),
                 reads=["ktv", "gsm"], writes=["kbv"])
            S.op("pool", lambda e: e.tensor_tensor(out=kbv[:, 4:8, :], in0=ktv[:, 4:8, :], in1=bc(beta), op=ALU.mult),
                 reads=["ktv", "gsm"], writes=["kbv"])
            S.op("pool", lambda e: e.tensor_tensor(out=R_["kd"][:], in0=ktv[:, 0:4, :], in1=bc(egl), op=ALU.mult),
                 reads=["ktv", "gsm"], writes=[rk])
            for h in range(4):
                S.op("dve" if h % 2 else "pool", lambda e, h=h: e.tensor_scalar(out=Tg[:, h, :], in0=triU[:], scalar1=gc[:, h:h + 1],
                                                                                 scalar2=None, op0=ALU.mult),
                     reads=["triU", "gsm"], writes=["Tg"])
            pEm, pEk = nextbank()
            for h in range(4):
                o_ = pEm[:, h * 128:(h + 1) * 128]
                S.op("pe", lambda e, o_=o_, h=h: e.matmul(o_, lhsT=Tg[:, h, :], rhs=onesf[:], start=True, stop=False),
                     reads=["Tg", "onesf"], writes=[pEk])
                S.op("pe", lambda e, o_=o_, h=h: e.matmul(o_, lhsT=negones[:], rhs=Tg[:, h, :], start=False, stop=False),
                     reads=["Tg", "negones"], writes=[pEk])
                S.op("pe", lambda e, o_=o_, h=h: e.matmul(o_, lhsT=identf[:], rhs=NMs[:], start=False, stop=True),
                     reads=["identf", "NMs"], writes=[pEk])
            S.op("act", lambda e, pEm=pEm: e.activation(out=Esb[:].rearrange("p c t -> p (c t)"), in_=pEm[:, :], func=AF.Exp),
                 reads=[pEk], writes=["Esb"])
            if own:
                pEt, pEtk = nextbank()
                for h in range(4):
                    o_ = pEt[:, h * 128:(h + 1) * 128]
                    S.op("pe", lambda e, o_=o_, h=h: e.matmul(o_, lhsT=onesf[:], rhs=Tg[:, h, :], start=True, stop=False),
                         reads=["Tg", "onesf"], writes=[pEtk])
                    S.op("pe", lambda e, o_=o_, h=h: e.matmul(o_, lhsT=Tg[:, h, :], rhs=negones[:], start=False, stop=False),
                         reads=["Tg", "negones"], writes=[pEtk])
                    S.op("pe", lambda e, o_=o_, h=h: e.matmul(o_, lhsT=identf[:], rhs=NMi[:], start=False, stop=True),
                         reads=["identf", "NMi"], writes=[pEtk])
                S.op("act", lambda e, pEt=pEt: e.activation(out=ETsb[:].rearrange("p c t -> p (c t)"), in_=pEt[:, :], func=AF.Exp),
                     reads=[pEtk], writes=["ETsb"])
                pkq, pkqk = nextbank()
                for h in range(4):
                    S.op("pe", lambda e, h=h, pkq=pkq: e.matmul(pkq[:, h * 128:(h + 1) * 128], lhsT=knT[:, h, :], rhs=qnT[b_][:, h, :],
                                                               start=True, stop=True), reads=["knT", rk], writes=[pkqk])
                S.op("dve", lambda e, pkq=pkq: e.tensor_tensor(out=R_["QKT"][:].rearrange("p c t -> p (c t)"), in0=pkq[:, :],
                                                              in1=ETsb[:].rearrange("p c t -> p (c t)"), op=ALU.mult),
                     reads=[pkqk, "ETsb"], writes=[rk])
            pkk, pkkk = nextbank()
            for h in range(4):
                S.op("pe", lambda e, h=h, pkk=pkk: e.matmul(pkk[:, h * 128:(h + 1) * 128], lhsT=knT[:, h, :], rhs=knT[:, h, :],
                                                           start=True, stop=True), reads=["knT"], writes=[pkkk])
            S.op("dve", lambda e, pkk=pkk: e.tensor_tensor(out=Esb[:].rearrange("p c t -> p (c t)"), in0=pkk[:, :],
                                                          in1=Esb[:].rearrange("p c t -> p (c t)"), op=ALU.mult),
                 reads=[pkkk, "Esb"], writes=["Esb"])
            S.op("dve", lambda e: e.tensor_tensor(out=Nk[0][:], in0=Esb[:], in1=bc(nbeta), op=ALU.mult),
                 reads=["Esb", "gsm"], writes=["Nk0"])
            for h in range(4):
                S.op("pe", lambda e, h=h: e.transpose(out=pT[:, h, :], in_=Nk[0][:, h, :], identity=ident[:]),
                     reads=["Nk0", "ident"], writes=["pT"])
            S.op("act", lambda e: e.copy(out=Uk[0][:], in_=pT[:, 0:4, :]), reads=["pT"], writes=["Uk0"])
            S.op("dve", lambda e: e.tensor_tensor(out=Rt[0][:], in0=Uk[0][:],
                                                  in1=ident[:].unsqueeze(1).broadcast_to([128, 4, 128]), op=ALU.add),
                 reads=["Uk0", "ident"], writes=["Rt0"])
            cur = 0
            rcur = 0
            for step in range(1, 7):
                nxt = 1 - cur
                pN, pNk = nextbank()
                for h in range(4):
                    S.op("pe", lambda e, h=h, pN=pN, cur=cur: e.matmul(pN[:, h * 128:(h + 1) * 128], lhsT=Uk[cur][:, h, :],
                                                                      rhs=Nk[cur][:, h, :], start=True, stop=True),
                         reads=["Uk%d" % cur, "Nk%d" % cur], writes=[pNk])
                if step < 6:
                    pU, pUk = nextbank()
                    for h in range(4):
                        S.op("pe", lambda e, h=h, pU=pU, cur=cur: e.matmul(pU[:, h * 128:(h + 1) * 128], lhsT=Nk[cur][:, h, :],
                                                                          rhs=Uk[cur][:, h, :], start=True, stop=True),
                             reads=["Uk%d" % cur, "Nk%d" % cur], writes=[pUk])
                S.op("act", lambda e, pN=pN, nxt=nxt: e.copy(out=Nk[nxt][:].rearrange("p c t -> p (c t)"), in_=pN[:, :]),
                     reads=[pNk], writes=["Nk%d" % nxt])
                if step < 6:
                    S.op("dve", lambda e, pU=pU, nxt=nxt: e.tensor_copy(out=Uk[nxt][:].rearrange("p c t -> p (c t)"), in_=pU[:, :]),
                         reads=[pUk], writes=["Uk%d" % nxt])
                pR, pRk = nextbank()
                for h in range(4):
                    S.op("pe", lambda e, h=h, pR=pR, nxt=nxt, rcur=rcur: e.matmul(pR[:, h * 128:(h + 1) * 128], lhsT=Nk[nxt][:, h, :],
                                                                                 rhs=Rt[rcur][:, h, :], start=True, stop=True),
                         reads=["Nk%d" % nxt, "Rt%d" % rcur], writes=[pRk])
                S.op("dve", lambda e, pR=pR, rcur=rcur: e.tensor_tensor(out=Rt[1 - rcur][:].rearrange("p c t -> p (c t)"), in0=pR[:, :],
                                                                       in1=Rt[rcur][:].rearrange("p c t -> p (c t)"), op=ALU.add),
                     reads=[pRk, "Rt%d" % rcur], writes=["Rt%d" % (1 - rcur)])
                cur = nxt
                rcur = 1 - rcur
            Rf = Rt[rcur]
            Rfk = "Rt%d" % rcur
            pu, puk = nextbank()
            for h in range(4):
                S.op("pe", lambda e, h=h, pu=pu: e.matmul(pu[:, h * 128:(h + 1) * 128], lhsT=Rf[:, h, :], rhs=kbv[:, 4 + h, :],
                                                         start=True, stop=True), reads=[Rfk, "kbv"], writes=[puk])
            S.op("act", lambda e, pu=pu: e.copy(out=R_["u"][:].rearrange("p c t -> p (c t)"), in_=pu[:, :]), reads=[puk], writes=[rk])
            pw, pwk = nextbank()
            for h in range(4):
                S.op("pe", lambda e, h=h, pw=pw: e.matmul(pw[:, h * 128:(h + 1) * 128], lhsT=kbv[:, h, :], rhs=Rf[:, h, :],
                                                         start=True, stop=True), reads=[Rfk, "kbv"], writes=[pwk])
            S.op("dve", lambda e, pw=pw: e.tensor_copy(out=R_["wT"][:].rearrange("p c t -> p (c t)"), in_=pw[:, :]),
                 reads=[pwk], writes=[rk])


        def delta_recur(j, own, zown, zownk):
            b_ = j % 2
            R_ = rec[b_]
            rk = "rec%d" % b_
            bc = lambda ap: ap.unsqueeze(2).broadcast_to([128, 4, 128])
            pws, pwsk = nextbank()
            for h in range(4):
                S.op("pe", lambda e, h=h, pws=pws: e.matmul(pws[:, h * 128:(h + 1) * 128], lhsT=R_["wT"][:, h, :], rhs=Sb[:, h, :],
                                                           start=True, stop=True), reads=[rk, "Sb"], writes=[pwsk])
            S.op("dve", lambda e, pws=pws: e.tensor_tensor(out=vnew[:].rearrange("p c t -> p (c t)"),
                                                          in0=R_["u"][:].rearrange("p c t -> p (c t)"), in1=pws[:, :], op=ALU.subtract),
                 reads=[rk, pwsk], writes=["vnew"])
            if own:
                po1, po1k = nextbank()
                po2, po2k = nextbank()
                for h in range(4):
                    S.op("pe", lambda e, h=h, po1=po1: e.matmul(po1[:, h * 128:(h + 1) * 128], lhsT=qnT[b_][:, h, :], rhs=Sb[:, h, :],
                                                               start=True, stop=True), reads=[rk, "Sb"], writes=[po1k])
                    S.op("pe", lambda e, h=h, po2=po2: e.matmul(po2[:, h * 128:(h + 1) * 128], lhsT=R_["QKT"][:, h, :], rhs=vnew[:, h, :],
                                                               start=True, stop=True), reads=[rk, "vnew"], writes=[po2k])
                S.op("act", lambda e, po2=po2: e.copy(out=o2sb[:].rearrange("p c t -> p (c t)"), in_=po2[:, :]),
                     reads=[po2k], writes=["o2sb"])
                S.op("dve", lambda e, po1=po1: e.tensor_tensor(out=osb[:], in0=po1[:, :].rearrange("p (c t) -> p c t", t=128),
                                                              in1=bc(R_["sc"][:, 0:4]), op=ALU.mult),
                     reads=[po1k, rk], writes=["osb"])
                S.op("dve", lambda e: e.tensor_tensor(out=osb[:], in0=osb[:], in1=o2sb[:], op=ALU.add),
                     reads=["osb", "o2sb"], writes=["osb"])
            if own:
                emit_ob(128, osb, "osb", zown[:, 2312:2824], [zownk], j // 8)
            pS_, pSk = nextbank()
            for h in range(4):
                S.op("pe", lambda e, h=h, pS_=pS_: e.matmul(pS_[:, h * 128:(h + 1) * 128], lhsT=R_["kd"][:, h, :], rhs=vnew[:, h, :],
                                                           start=True, stop=True), reads=[rk, "vnew"], writes=[pSk])
            S.op("dve", lambda e: e.tensor_tensor(out=Sf[:], in0=Sf[:], in1=bc(R_["sc"][:, 4:8]), op=ALU.mult),
                 reads=["Sf", rk], writes=["Sf"])
            S.op("dve", lambda e, pS_=pS_: e.tensor_tensor(out=Sf[:].rearrange("p c t -> p (c t)"), in0=Sf[:].rearrange("p c t -> p (c t)"),
                                                          in1=pS_[:, :], op=ALU.add), reads=["Sf", pSk], writes=["Sf"])
            S.op("act", lambda e: e.copy(out=Sb[:], in_=Sf[:]), reads=["Sf"], writes=["Sb"])

        prev = None
        pend = None
        for j in range(NBLK):
            own = (j % 8 == 7)
            i = j // 8
            last = own and (i == NQB - 1)

            def outs_prompt(z, zk, i=i, own=own, last=last):
                if own:
                    S.dma("pool", "st_kvp%d" % (i % 2), o_kvp[i, :, :], z[:, 0:512], reads=[zk])
                if last:
                    S.dma("pool", "st_winp", o_winp[:, :], z[:, 512:768], reads=[zk])
                    S.dma("pool", "st_convp", o_convp[:, :], z[125:128, 768:768 + 1536], reads=[zk])
            grp = [0, 1] + ([2, 3, 4] if last else []) + ([6] if own else [])
            hT, hk, z, zk = front(128, xp[j, :, :], gs1p[:, :], sh1p[:, :], ["gs1p", "sh1p"], outs_prompt,
                                  vcol=(vmask[:, j:j + 1] if j < 7 else None), groups=grp)
            kvprep(j, z, zk, KVA)
            if own:
                S.dma("pool", "st_hT", hT_s[i], hT[:], reads=[hk], writes=["hT_s%d" % i])
            delta_prep(j, hT, hk, prev[0] if prev else None, prev[1] if prev else None, own)
            if pend is not None:
                delta_recur(*pend)
            pend = (j, own, z, zk)
            prev = (hT, hk)
        delta_recur(*pend)
        S.dma("pool", "st_dp", o_dp.rearrange("h k v -> k h v"), Sf[:], reads=["Sf"])

        ar.release(m_passA)
        m_passB = ar.mark()
        NEGB, BIGB = -1e30, 1e30
        VW = 65 + NSBK
        blk1 = tb("blk1", [128, 128], BF16)
        S.op("pool", lambda e: e.memset(blk1[:], 0.0), writes=["blk1"])
        S.op("pool", lambda e: e.memset(blk1[0:64, 0:64], 1.0), writes=["blk1"])
        S.op("pool", lambda e: e.memset(blk1[64:128, 64:128], 1.0), writes=["blk1"])
        Eall = tb("Eall", [128, 64, 128], BF16)
        S.op("pool", lambda e: e.memset(Eall[:], 1.0), writes=["Eall"])
        S.op("pool", lambda e: e.affine_select(out=Eall[:].rearrange("p r (a b) -> p r a b", b=64),
                                               in_=Eall[:].rearrange("p r (a b) -> p r a b", b=64),
                                               pattern=[[-2, 64], [-1, 2], [0, 64]], compare_op=ALU.is_equal, fill=0.0,
                                               base=0, channel_multiplier=1), reads=["Eall"], writes=["Eall"])
        qw = tb("qw", [128, 8, 536], BF16)
        qng = tb("qng", [128, 64], F32)
        avn = tb("avn", [128, NSBK], F32)
        fz0 = tb("fz0", [128, NSBK], F32)
        kvl = tb("kvl", [128, NCT], F32)
        gk = tb("gk", [128, 1], F32)
        S.dma("sp", "ld_b1", qng[:], q_norm_g[0:1, :].partition_broadcast(128), writes=["qng"])
        S.dma("sp", "ld_b2", avn[:], availneg[:, :], writes=["avn"])
        S.dma("sp", "ld_b3", fz0[:], forced0[:, :], writes=["fz0"])
        S.dma("sp", "ld_b4", kvl[:], kvalid[:, :], writes=["kvl"])
        for hf in range(2):
            S.dma("sp", "ld_b5", gk[hf * 64:(hf + 1) * 64, :], k_norm_g[0, :].rearrange("(d o) -> d o", o=1), writes=["gk"])
        kcT = tb("kcT", [69, 2, NCT * 128], BF16)
        vcx = tb("vcx", [128, NCT, 2, VW], BF16)
        S.op("pool", lambda e: e.memset(kcT[:], 0.0), writes=["kcT"])
        S.op("pool", lambda e: e.memset(vcx[:], 0.0), writes=["vcx"])
        S.op("pool", lambda e: e.memset(vcx[:, :, :, 64:65], 1.0), reads=["vcx"], writes=["vcx"])
        McolT = tb("McolT", [128, 3, 4, 128], BF16)
        S.op("pool", lambda e: e.memset(McolT[:], 0.0), writes=["McolT"])
        wstg = tb("wstg", [128, 32, 128], F32)
        for (c0, n, o0) in ((O_Q, 512, 0), (O_G, 24, 512)):
            S.dma("sp", "ld_wstg", wstg[:].rearrange("p a b -> p (a b)")[:, 0:8 * n].rearrange("p (k n) -> p k n", n=n),
                  w_in_v[:, :, c0:c0 + n], writes=["wstg"])
            S.op("dve", lambda e, n=n, o0=o0: e.tensor_copy(
                out=qw[:, :, o0:o0 + n], in_=wstg[:].rearrange("p a b -> p (a b)")[:, 0:8 * n].rearrange("p (k n) -> p k n", n=n)),
                reads=["wstg"], writes=["qw"])
        KR = tb("KR", [128, LLOC + 32], BF16)
        wblk = tb("wblk", [128, 32, 128], BF16)
        pe2 = tb("pe2", [32, 128], F32)
        pecol = tb("pecol", [128, 32], BF16)
        biask = tb("biask", [128, 1], F32)
        biasv = tb("biasv", [1, 128], BF16)
        onesrow = tb("onesrow", [1, 128], BF16)
        kcp = tb("kcp", [128, 512], F32)
        ksq = tb("ksq", [128, 512], BF16)
        krn = tb("krn", [128, 512], F32)
        kcn = tb("kcn", [128, 512], BF16)
        S.op("pool", lambda e: e.memset(onesrow[:], 1.0), writes=["onesrow"])
        def compress():
            for ty in range(2):
                S.op("pool", lambda e: e.memset(wstg[:], 0.0), reads=["wstg"], writes=["wstg"])
                for hf in range(2):
                    for jh in range(2):
                        S.dma("sp", "ld_wstg", wstg[hf * 64:(hf + 1) * 64, jh * 16:(jh + 1) * 16, hf * 64:(hf + 1) * 64],
                              w_cmp[ty, jh * 16:(jh + 1) * 16].rearrange("j d e -> d j e"), reads=["wstg"], writes=["wstg"])
                    S.dma("sp", "ld_pe2", pe2[:, hf * 64:(hf + 1) * 64], pe_cmp[ty], writes=["pe2"])
                S.op("dve", lambda e: e.tensor_copy(out=wblk[:], in_=wstg[:]), reads=["wstg"], writes=["wblk"])
                S.op("pe", lambda e: e.transpose(out=pTf[:, 0, 0:32], in_=pe2[:], identity=identf[0:32, 0:32]),
                     reads=["pe2", "identf"], writes=["pTf"])
                S.op("act", lambda e: e.copy(out=pecol[:], in_=pTf[:, 0, 0:32]), reads=["pTf"], writes=["pecol"])
                S.op("pool", lambda e: e.memset(KR[:, LLOC:LLOC + 32], 0.0), reads=["KR"], writes=["KR"])
                for q4 in range(4):
                    c0 = q4 * (LLOC // 4)
                    S.dma("sp", "ld_KR", KR[:, c0:c0 + LLOC // 4], KT_s[2 + ty, :, c0:c0 + LLOC // 4], reads=["KT_s", "KR"], writes=["KR"])
                if ty == 0:
                    pb_, pbk = nextbank()
                    for j in range(32):
                        S.op("pe", lambda e, j=j, pb_=pb_: e.matmul(pb_[:, 0:1], lhsT=wblk[:, j, :], rhs=pecol[:, j:j + 1],
                                                                   start=(j == 0), stop=(j == 31)), reads=["wblk", "pecol"], writes=[pbk])
                    S.op("act", lambda e, pb_=pb_: e.copy(out=biask[:], in_=pb_[:, 0:1]), reads=[pbk], writes=["biask"])
                    for n0 in range(0, NCB, 512):
                        N = min(512, NCB - n0)
                        pk_, pkk_ = nextbank()
                        for j in range(32):
                            S.op("pe", lambda e, j=j, n0=n0, N=N, pk_=pk_: e.matmul(
                                pk_[:, 0:N], lhsT=wblk[:, j, :], rhs=KR[:, j + 16 * n0:j + 16 * (n0 + N):16],
                                start=(j == 0), stop=(j == 31)), reads=["wblk", "KR"], writes=[pkk_])
                        S.op("act", lambda e, N=N, pk_=pk_: e.activation(out=kcp[:, 0:N], in_=pk_[:, 0:N], func=AF.Identity, bias=biask[:, 0:1]),
                             reads=[pkk_, "biask"], writes=["kcp"])
                        S.op("pool", lambda e, N=N: e.tensor_tensor(out=ksq[:, 0:N], in0=kcp[:, 0:N], in1=kcp[:, 0:N], op=ALU.mult),
                             reads=["kcp"], writes=["ksq"])
                        ps_, psk_ = nextbank()
                        S.op("pe", lambda e, N=N, ps_=ps_: e.matmul(ps_[:, 0:N], lhsT=blk1[:], rhs=ksq[:, 0:N], start=True, stop=True),
                             reads=["blk1", "ksq"], writes=[psk_])
                        S.op("act", lambda e, N=N, ps_=ps_: e.activation(out=krn[:, 0:N], in_=ps_[:, 0:N], func=AF.Sqrt, scale=1.0 / 64, bias=EPS),
                             reads=[psk_], writes=["krn"])
                        S.op("dve", lambda e, N=N: e.reciprocal(out=krn[:, 0:N], in_=krn[:, 0:N]), reads=["krn"], writes=["krn"])
                        S.op("dve", lambda e, N=N: e.scalar_tensor_tensor(out=kcn[:, 0:N], in0=kcp[:, 0:N], scalar=gk[:, 0:1], in1=krn[:, 0:N],
                                                                         op0=ALU.mult, op1=ALU.mult), reads=["kcp", "gk", "krn"], writes=["kcn"])
                        S.dma("pool", "st_kc", KcT_s[:, n0:n0 + N], kcn[:, 0:N], reads=["kcn"], writes=["KcT_s"])
                    for kh in range(2):
                        S.dma("sp", "ld_kcT", kcT[0:64, kh, 0:NCB], KcT_s[kh * 64:(kh + 1) * 64, 0:NCB], reads=["KcT_s", "kcT"], writes=["kcT"])
                        S.dma("sp", "ld_kcT", kcT[64:69, kh, :], kaug_cmp[:, :], reads=["kcT"], writes=["kcT"])
                else:
                    pb_, pbk = nextbank()
                    for j in range(32):
                        S.op("pe", lambda e, j=j, pb_=pb_: e.matmul(pb_[0:1, 0:128], lhsT=pecol[:, j:j + 1], rhs=wblk[:, j, :],
                                                                   start=(j == 0), stop=(j == 31)), reads=["wblk", "pecol"], writes=[pbk])
                    S.op("act", lambda e, pb_=pb_: e.copy(out=biasv[:], in_=pb_[0:1, 0:128]), reads=[pbk], writes=["biasv"])
                    for t in range(NCT):
                        n0 = t * 128
                        N = min(128, NCB - n0)
                        pv_, pvk_ = nextbank()
                        for j in range(32):
                            S.op("pe", lambda e, j=j, n0=n0, N=N, pv_=pv_: e.matmul(
                                pv_[0:N, 0:128], lhsT=KR[:, j + 16 * n0:j + 16 * (n0 + N):16], rhs=wblk[:, j, :],
                                start=(j == 0), stop=False), reads=["wblk", "KR"], writes=[pvk_])
                        S.op("pe", lambda e, N=N, pv_=pv_: e.matmul(pv_[0:N, 0:128], lhsT=onesrow[0:1, 0:N], rhs=biasv[0:1, :],
                                                                   start=False, stop=True), reads=["onesrow", "biasv"], writes=[pvk_])
                        S.op("act", lambda e, t=t, N=N, pv_=pv_: e.copy(out=vcx[0:N, t, :, 0:64],
                                                                       in_=pv_[0:N, 0:128].rearrange("p (k d) -> p k d", d=64)),
                             reads=[pvk_, "vcx"], writes=["vcx"])
                        for kh in range(2):
                            S.dma("sp", "ld_band", vcx[:, t, kh, 65:VW], band_tab[n0:n0 + 128, :], reads=["vcx"], writes=["vcx"])
        compress()
        b_hT = tb("b_hT", [128, 8, 128], BF16)
        b_qsq = tb("b_qsq", [128, 8, 64], F32)
        b_qms = tb("b_qms", [128, 8], F32)
        b_qn = tb("b_qn", [128, 8, 64], BF16)
        b_gts = tb("b_gts", [128, 24], F32)
        QT = tb("QT", [69, 8, 128], BF16)
        PTc = tb("PTc", [128, NCT, 512], BF16)
        PTs = [tb("PTs", [128, 512], BF16) for _ in range(2)]
        CMcache = {}
        zb = tb("zb", [128, 512], BF16)
        S.op("pool", lambda e: e.memset(zb[:], 0.0), writes=["zb"])
        oc = tb("oc", [128, 4, 64], F32)
        imp = tb("imp", [128, NSBK], F32)
        imp3 = tb("imp3", [128, NSBK], F32)
        selt = tb("selt", [128, 3 * 128], F32)
        m8 = tb("m8", [128, 16], F32)
        rz = tb("rz", [128, 8], F32)
        oa_t = tb("oa_t", [128, 512], F32)
        CH = 8
        Kbuf = [tb("Kbuf", [69, CH * 128], BF16) for _ in range(2)]
        Vbuf = [tb("Vbuf", [128, CH, 65], BF16) for _ in range(2)]
        sctr = {"i": 0, "c": 0}

        def s_tile(lhsK, rhsQ, kkeys, mask_mm=None):
            i_ = sctr["i"] % 2
            sctr["i"] += 1
            pS, pSk = (pA, "pz0") if i_ == 0 else (pB, "pz1")
            S.op("pe", lambda e: e.matmul(pS[:, :], lhsT=lhsK, rhs=rhsQ, start=True, stop=(mask_mm is None)),
                 reads=kkeys + ["QT"], writes=[pSk])
            if mask_mm is not None:
                S.op("pe", lambda e: e.matmul(pS[:, :], lhsT=mask_mm[0], rhs=mask_mm[1], start=False, stop=True),
                     reads=["Eall", "McolT"], writes=[pSk])
            return pS, pSk

        def nsa_block(i, J, samp, qaug_ap, kvl_, avn_, fz0_, out_ap, outk, dbg_ap):
            if samp is None:
                S.dma("sp", "ld_bhT", b_hT[:], hT_s[i], reads=["hT_s%d" % i], writes=["b_hT"])
            else:
                S.op("pool", lambda e: e.memset(b_hT[:], 0.0), writes=["b_hT"])
                S.dma("sp", "ld_bhT", b_hT[:, :, 0:1], hT_s[NQB, :, :, samp:samp + 1], reads=["hT_s%d" % NQB, "b_hT"], writes=["b_hT"],
                      allow_slow_non_contiguous=True)
            pq, pqk = pC, "pz2"
            pg, pgk = pD, "pz3"
            for k in range(8):
                S.op("pe", lambda e, k=k: e.matmul(pq[:, :], lhsT=b_hT[:, k, :], rhs=qw[:, k, 0:512], start=(k == 0), stop=(k == 7)),
                     reads=["b_hT", "qw"], writes=[pqk])
            for k in range(8):
                S.op("pe", lambda e, k=k: e.matmul(pg[:, 0:24], lhsT=b_hT[:, k, :], rhs=qw[:, k, 512:536], start=(k == 0), stop=(k == 7)),
                     reads=["b_hT", "qw"], writes=[pgk])
            S.op("act", lambda e: e.activation(out=b_gts[:], in_=pg[:, 0:24], func=AF.Sigmoid), reads=[pgk], writes=["b_gts"])
            S.op("act", lambda e: e.activation(out=b_qsq[:].rearrange("p h d -> p (h d)"), in_=pq[:, :], func=AF.Square),
                 reads=[pqk], writes=["b_qsq"])
            S.op("dve", lambda e: e.tensor_reduce(out=b_qms[:], in_=b_qsq[:], axis=AX.X, op=ALU.add), reads=["b_qsq"], writes=["b_qms"])
            S.op("act", lambda e: e.activation(out=b_qms[:], in_=b_qms[:], func=AF.Sqrt, scale=1.0 / 64, bias=EPS),
                 reads=["b_qms"], writes=["b_qms"])
            S.op("dve", lambda e: e.reciprocal(out=b_qms[:], in_=b_qms[:]), reads=["b_qms"], writes=["b_qms"])
            S.op("dve", lambda e: e.tensor_tensor(out=b_qsq[:], in0=pq[:, :].rearrange("p (h d) -> p h d", d=64),
                                                  in1=b_qms[:].unsqueeze(2).broadcast_to([128, 8, 64]), op=ALU.mult),
                 reads=[pqk, "b_qms", "b_qsq"], writes=["b_qsq"])
            S.op("dve", lambda e: e.scalar_tensor_tensor(out=b_qn[:], in0=b_qsq[:], scalar=0.125,
                                                         in1=qng[:].unsqueeze(1).broadcast_to([128, 8, 64]), op0=ALU.mult, op1=ALU.mult),
                 reads=["b_qsq", "qng"], writes=["b_qn"])
            for h in range(8):
                S.op("pe", lambda e, h=h: e.transpose(out=pT[0:64, h, :], in_=b_qn[:, h, :], identity=ident[:]),
                     reads=["b_qn", "ident"], writes=["pT"])
            S.op("act", lambda e: e.copy(out=QT[0:64, :, :], in_=pT[0:64, :, :]), reads=["pT"], writes=["QT"])
            S.dma("sp", "ld_qaug", QT[64:69, :, :], qaug_ap, reads=["QT"], writes=["QT"])
            for kh in range(2):
                rhsQ = QT[:, 4 * kh:4 * kh + 4, :].rearrange("p h q -> p (h q)")
                nt = (8 * J + 6) // 128 + 1
                for t in range(nt):
                    mm = None
                    if t >= nt - 2 and (128 * J - 2048 * t - 31 - 16 * 127) < 0:
                        base_ = 128 * J - 2048 * t - 31
                        if base_ not in CMcache:
                            CM_ = tb("CMc%d" % len(CMcache), [128, 512], BF16)
                            CMk = "CMc%d" % len(CMcache)
                            CMcache[base_] = (CM_, CMk)
                            S.op("pool", lambda e, CM_=CM_: e.memset(CM_[:], 0.0), writes=[CMk])
                            S.op("pool", lambda e, CM_=CM_, base_=base_: e.affine_select(
                                out=CM_[:].rearrange("p (h q) -> p h q", q=128), in_=CM_[:].rearrange("p (h q) -> p h q", q=128),
                                pattern=[[0, 4], [1, 128]], compare_op=ALU.is_ge, fill=-30000.0, base=base_, channel_multiplier=-16),
                                reads=[CMk], writes=[CMk])
                        CM_, CMk = CMcache[base_]
                        mm = (ident[:], CM_[:])
                    pS, pSk = s_tile(kcT[:, kh, t * 128:(t + 1) * 128], rhsQ, ["kcT"] + ([CMk] if mm else []), mask_mm=mm)
                    S.op("act", lambda e, t=t, pS=pS: e.activation(out=PTc[:, t, :], in_=pS[:, :], func=AF.Exp), reads=[pSk], writes=["PTc"])
                    if t == 0:
                        S.op("dve", lambda e: e.tensor_scalar(out=PTc[:, 0, :], in0=PTc[:, 0, :], scalar1=kvl_[:, 0:1], scalar2=None, op0=ALU.mult),
                             reads=["PTc", "kvl"], writes=["PTc"])
                for h in range(4):
                    pO, pOk = (pC, "pz2") if h % 2 == 0 else (pD, "pz3")
                    for t in range(nt):
                        S.op("pe", lambda e, t=t, h=h, pO=pO, kh=kh: e.matmul(pO[:, 0:VW], lhsT=PTc[:, t, h * 128:(h + 1) * 128], rhs=vcx[:, t, kh, :],
                                                                            start=(t == 0), stop=(t == nt - 1)), reads=["PTc", "vcx"], writes=[pOk])
                    S.op("dve", lambda e, h=h, pO=pO: e.tensor_scalar(out=rz[:, h:h + 1], in0=pO[:, 64:65], scalar1=1e-30, scalar2=None, op0=ALU.max),
                         reads=[pOk], writes=["rz"])
                    S.op("dve", lambda e, h=h: e.reciprocal(out=rz[:, h:h + 1], in_=rz[:, h:h + 1]), reads=["rz"], writes=["rz"])
                    S.op("dve", lambda e, h=h, pO=pO: e.tensor_scalar(out=oc[:, h, :], in0=pO[:, 0:64], scalar1=rz[:, h:h + 1], scalar2=None, op0=ALU.mult),
                         reads=[pOk, "rz"], writes=["oc"])
                    if h == 0:
                        S.op("dve", lambda e, pO=pO: e.tensor_scalar(out=imp[:], in0=pO[:, 65:VW], scalar1=rz[:, 0:1], scalar2=None, op0=ALU.mult),
                             reads=[pOk, "rz"], writes=["imp"])
                    else:
                        S.op("dve", lambda e, h=h, pO=pO: e.scalar_tensor_tensor(out=imp[:], in0=pO[:, 65:VW], scalar=rz[:, h:h + 1], in1=imp[:],
                                                                                op0=ALU.mult, op1=ALU.add), reads=[pOk, "rz", "imp"], writes=["imp"])
                S.op("dve", lambda e: e.tensor_tensor(out=imp[:], in0=imp[:], in1=avn_[:], op=ALU.add), reads=["imp", "avn"], writes=["imp"])
                S.op("dve", lambda e: e.tensor_tensor(out=imp[:], in0=imp[:], in1=fz0_[:], op=ALU.max), reads=["imp", "fz0"], writes=["imp"])
                for hf in range(2):
                    if 2 * J + hf + 1 < NSBK:
                        S.op("pool", lambda e, hf=hf: e.memset(imp[hf * 64:(hf + 1) * 64, 2 * J + hf + 1:NSBK], NEGB), reads=["imp"], writes=["imp"])
                    S.op("pool", lambda e, hf=hf: e.memset(imp[hf * 64:(hf + 1) * 64, 2 * J + hf:2 * J + hf + 1], BIGB), reads=["imp"], writes=["imp"])
                S.op("dve", lambda e: e.max(out=m8[:, 0:8], in_=imp[:]), reads=["imp"], writes=["m8"])
                S.op("dve", lambda e: e.match_replace(out=imp3[:], in_to_replace=m8[:, 0:8], in_values=imp[:], imm_value=-3e38),
                     reads=["imp", "m8"], writes=["imp3"])
                S.op("dve", lambda e: e.max(out=m8[:, 8:16], in_=imp3[:]), reads=["imp3"], writes=["m8"])
                S.op("dve", lambda e: e.tensor_scalar(out=imp3[:], in0=imp[:], scalar1=m8[:, 15:16], scalar2=None, op0=ALU.is_ge),
                     reads=["imp", "m8", "imp3"], writes=["imp3"])
                S.op("dve", lambda e: e.tensor_scalar(out=imp[:], in0=imp[:], scalar1=-1e29, scalar2=None, op0=ALU.is_ge),
                     reads=["imp"], writes=["imp"])
                S.op("dve", lambda e: e.tensor_tensor(out=imp3[:], in0=imp3[:], in1=imp[:], op=ALU.mult), reads=["imp", "imp3"], writes=["imp3"])
                S.op("dve", lambda e: e.tensor_scalar(out=selt[:, 0:NSBK], in0=imp3[:], scalar1=-1.0, scalar2=30000.0, op0=ALU.add, op1=ALU.mult),
                     reads=["imp3"], writes=["selt"])
                pX, pXk = pC, "pz2"
                nb3 = [(b0__, min(128, NSBK - b0__)) for b0__ in range(0, NSBK, 128)]
                for c_, (b0_, nb) in enumerate(nb3):
                    S.op("pe", lambda e, c_=c_, b0_=b0_, nb=nb: e.transpose(out=pX[0:nb, c_ * 128:(c_ + 1) * 128], in_=selt[:, b0_:b0_ + nb], identity=identf[:]),
                         reads=["selt", "identf"], writes=[pXk])
                for c_, (b0_, nb) in enumerate(nb3):
                    S.op("act", lambda e, c_=c_, nb=nb: e.copy(out=McolT[0:nb, c_, :, :],
                                                               in_=pX[0:nb, c_ * 128:(c_ + 1) * 128].unsqueeze(1).broadcast_to([nb, 4, 128])),
                         reads=[pXk, "McolT"], writes=["McolT"])
                S.op("pe", lambda e: e.matmul(pE[:, 0:260], lhsT=zb[:, 0:128], rhs=zb[:, 0:260], start=True, stop=False),
                     reads=["zb"], writes=["pE"])
                for c0 in range(0, J + 1, CH):
                    n = min(CH, J + 1 - c0)
                    bi = sctr["c"] % 2
                    sctr["c"] += 1
                    Kb, Vb = Kbuf[bi], Vbuf[bi]
                    kbk, vbk = "Kbuf%d" % bi, "Vbuf%d" % bi
                    S.dma("sp", "ld_" + kbk, Kb[0:64, 0:n * 128], KT_s[0, kh * 64:(kh + 1) * 64, c0 * 128:(c0 + n) * 128], reads=["KT_s"], writes=[kbk])
                    S.dma("sp", "ld_" + kbk, Kb[64:69, 0:n * 128], kaug_pos[:, c0 * 128:(c0 + n) * 128], reads=[kbk], writes=[kbk])
                    S.dma("sp", "ld_" + vbk, Vb[:, 0:n, :], V_s[c0 * 128:(c0 + n) * 128, kh, :].rearrange("(t p) c -> p t c", p=128),
                          reads=["V_s"], writes=[vbk])
                    for tt in range(n):
                        kt = c0 + tt
                        cb, ri = (2 * kt) // 128, ((2 * kt) % 128) // 2
                        pS, pSk = s_tile(Kb[:, tt * 128:(tt + 1) * 128], rhsQ, [kbk],
                                         mask_mm=(Eall[:, ri, :], McolT[:, cb, :, :].rearrange("p h q -> p (h q)")))
                        pi = sctr["i"] % 2
                        PT_, PTk = PTs[pi], "PTs%d" % pi
                        S.op("act", lambda e, pS=pS, PT_=PT_: e.activation(out=PT_[:], in_=pS[:, :], func=AF.Exp), reads=[pSk], writes=[PTk])
                        if kt == J:
                            S.op("pool", lambda e, PT_=PT_: e.affine_select(
                                out=PT_[:].rearrange("p (h q) -> p h q", q=128), in_=PT_[:].rearrange("p (h q) -> p h q", q=128),
                                pattern=[[0, 4], [1, 128]], compare_op=ALU.is_ge, fill=0.0, base=0, channel_multiplier=-1),
                                reads=[PTk], writes=[PTk])
                        for h in range(4):
                            S.op("pe", lambda e, h=h, tt=tt, kt=kt, PT_=PT_, Vb=Vb: e.matmul(pE[:, h * 65:(h + 1) * 65], lhsT=PT_[:, h * 128:(h + 1) * 128], rhs=Vb[:, tt, :],
                                                                                       start=False, stop=(kt == J and h == 3)), reads=[PTk, vbk], writes=["pE"])
                w0 = max(0, J - 4)
                n = J + 1 - w0
                bi = sctr["c"] % 2
                sctr["c"] += 1
                Kb, Vb = Kbuf[bi], Vbuf[bi]
                kbk, vbk = "Kbuf%d" % bi, "Vbuf%d" % bi
                S.dma("sp", "ld_" + kbk, Kb[0:64, 0:n * 128], KT_s[1, kh * 64:(kh + 1) * 64, w0 * 128:(w0 + n) * 128], reads=["KT_s"], writes=[kbk])
                S.dma("sp", "ld_" + kbk, Kb[64:69, 0:n * 128], kaug_pos[:, w0 * 128:(w0 + n) * 128], reads=[kbk], writes=[kbk])
                S.dma("sp", "ld_" + vbk, Vb[:, 0:n, :], V_s[w0 * 128:(w0 + n) * 128, 2 + kh, :].rearrange("(t p) c -> p t c", p=128),
                      reads=["V_s"], writes=[vbk])
                S.op("pe", lambda e: e.matmul(pF[:, 0:260], lhsT=zb[:, 0:128], rhs=zb[:, 0:260], start=True, stop=False),
                     reads=["zb"], writes=["pF"])
                for tt in range(n):
                    kt = w0 + tt
                    pS, pSk = s_tile(Kb[:, tt * 128:(tt + 1) * 128], rhsQ, [kbk])
                    pi = sctr["i"] % 2
                    PT_, PTk = PTs[pi], "PTs%d" % pi
                    S.op("act", lambda e, pS=pS, PT_=PT_: e.activation(out=PT_[:], in_=pS[:, :], func=AF.Exp), reads=[pSk], writes=[PTk])
                    if kt == J:
                        S.op("pool", lambda e, PT_=PT_: e.affine_select(
                            out=PT_[:].rearrange("p (h q) -> p h q", q=128), in_=PT_[:].rearrange("p (h q) -> p h q", q=128),
                            pattern=[[0, 4], [1, 128]], compare_op=ALU.is_ge, fill=0.0, base=0, channel_multiplier=-1), reads=[PTk], writes=[PTk])
                    if kt == J - 4:
                        S.op("pool", lambda e, PT_=PT_: e.affine_select(
                            out=PT_[:].rearrange("p (h q) -> p h q", q=128), in_=PT_[:].rearrange("p (h q) -> p h q", q=128),
                            pattern=[[0, 4], [-1, 128]], compare_op=ALU.is_ge, fill=0.0, base=0, channel_multiplier=1), reads=[PTk], writes=[PTk])
                    if samp is None and kt < 7:
                        S.op("dve", lambda e, PT_=PT_, kt=kt: e.tensor_scalar(out=PT_[:], in0=PT_[:], scalar1=vmask[:, kt:kt + 1], scalar2=None, op0=ALU.mult),
                             reads=[PTk, "vmask"], writes=[PTk])
                    for h in range(4):
                        S.op("pe", lambda e, h=h, tt=tt, PT_=PT_, Vb=Vb, n=n: e.matmul(pF[:, h * 65:(h + 1) * 65], lhsT=PT_[:, h * 128:(h + 1) * 128], rhs=Vb[:, tt, :],
                                                                                start=False, stop=(tt == n - 1 and h == 3)), reads=[PTk, vbk], writes=["pF"])
                for h in range(4):
                    hh = 4 * kh + h
                    S.op("dve", lambda e, h=h: e.reciprocal(out=rz[:, 4:5], in_=pE[:, h * 65 + 64:h * 65 + 65]), reads=["pE", "rz"], writes=["rz"])
                    S.op("dve", lambda e, h=h: e.reciprocal(out=rz[:, 5:6], in_=pF[:, h * 65 + 64:h * 65 + 65]), reads=["pF", "rz"], writes=["rz"])
                    S.op("dve", lambda e, hh=hh: e.tensor_tensor(out=rz[:, 4:6], in0=rz[:, 4:6], in1=b_gts[:, hh * 3 + 1:hh * 3 + 3], op=ALU.mult),
                         reads=["rz", "b_gts"], writes=["rz"])
                    S.op("dve", lambda e, h=h, hh=hh: e.tensor_scalar(out=oa_t[:, hh * 64:(hh + 1) * 64], in0=oc[:, h, :], scalar1=b_gts[:, hh * 3:hh * 3 + 1],
                                                                      scalar2=None, op0=ALU.mult), reads=["oc", "b_gts", "oa_t"], writes=["oa_t"])
                    S.op("dve", lambda e, h=h, hh=hh: e.scalar_tensor_tensor(out=oa_t[:, hh * 64:(hh + 1) * 64], in0=pE[:, h * 65:h * 65 + 64], scalar=rz[:, 4:5],
                                                                             in1=oa_t[:, hh * 64:(hh + 1) * 64], op0=ALU.mult, op1=ALU.add),
                         reads=["pE", "rz", "oa_t"], writes=["oa_t"])
                    S.op("dve", lambda e, h=h, hh=hh: e.scalar_tensor_tensor(out=oa_t[:, hh * 64:(hh + 1) * 64], in0=pF[:, h * 65:h * 65 + 64], scalar=rz[:, 5:6],
                                                                             in1=oa_t[:, hh * 64:(hh + 1) * 64], op0=ALU.mult, op1=ALU.add),
                         reads=["pF", "rz", "oa_t"], writes=["oa_t"])
            if samp is None:
                S.dma("pool", "st_oa", out_ap, oa_t[:], reads=["oa_t"], writes=[outk])
            else:
                S.dma("pool", "st_oa", out_ap, oa_t[0:1, :], reads=["oa_t"], writes=[outk])

        for i in range(NQB):
            nsa_block(i, 8 * i + 7, None, qaug[i], kvl, avn, fz0, oa_s[i], "oa_s%d" % i, None)

        kvl_s = tb("kvl_s", [128, NCT], F32)
        avn_s = tb("avn_s", [128, NSBK], F32)
        fz0_s = tb("fz0_s", [128, NSBK], F32)
        S.op("pool", lambda e: e.memset(kvl_s[:], 1.0), writes=["kvl_s"])
        S.op("pool", lambda e: e.memset(avn_s[:], 0.0), writes=["avn_s"])
        S.op("pool", lambda e: e.memset(fz0_s[:], -3e38), writes=["fz0_s"])
        S.op("pool", lambda e: e.memset(fz0_s[:, 0:1], 1e30), reads=["fz0_s"], writes=["fz0_s"])
        KVB = kv_tiles()
        zts = [tb("zt_s", [128, 768], F32) for _ in range(2)]
        ptf = tb("ptf", [128, 128], F32)
        pti = tb("pti", [128, 128], I32)
        idxi = tb("idxi", [128, 128], I32)
        piota_t = tb("piota_t", [128, 1], F32)
        S.dma("sp", "ld_piota", piota_t[:], piota[:, :], writes=["piota_t"])
        for s_ in range(SB_):
            S.dma("sp", "ld_pti", pti[:], ptab[s_:s_ + 1, :].partition_broadcast(128), reads=["idxi"], writes=["pti"])
            S.op("dve", lambda e: e.tensor_copy(out=ptf[:], in_=pti[:]), reads=["pti"], writes=["ptf"])
            S.op("dve", lambda e: e.tensor_scalar(out=ptf[:], in0=ptf[:], scalar1=128.0, scalar2=piota_t[:, 0:1], op0=ALU.mult, op1=ALU.add),
                 reads=["ptf", "piota_t"], writes=["ptf"])
            S.op("dve", lambda e: e.tensor_copy(out=idxi[:], in_=ptf[:]), reads=["ptf"], writes=["idxi"])
            for j in range(129):
                zt = zts[j % 2]
                ztk = "zt_s%d" % (j % 2)
                if j < 128:
                    S.dma_fn("pool", "ld_" + ztk, lambda e, zt=zt, j=j: e.indirect_dma_start(
                        out=zt[:, 0:512], out_offset=None, in_=cache[:, :],
                        in_offset=bass.IndirectOffsetOnAxis(ap=idxi[:, j:j + 1], axis=0)), reads=["idxi"], writes=[ztk])
                    if j >= 124:
                        S.dma("sp", "ld2_" + ztk, zt[:, 512:768], cwin[s_, (j - 124) * 128:(j - 123) * 128, :], reads=[ztk], writes=[ztk])
                    else:
                        S.op("pool", lambda e, zt=zt: e.memset(zt[:, 512:768], 0.0), reads=[ztk], writes=[ztk])
                else:
                    S.op("pool", lambda e, zt=zt: e.memset(zt[:], 0.0), writes=[ztk])
                    S.dma("sp", "ld2_" + ztk, zt[0:1, 0:512], o_kvs[s_:s_ + 1, :], reads=[ztk, "o_kvs"], writes=[ztk])
                    S.dma("sp", "ld2_" + ztk, zt[0:1, 512:768], o_wins[s_, 511:512, :], reads=[ztk, "o_wins"], writes=[ztk])
                kvprep(j, zt, ztk, KVB)
            compress()
            nsa_block(NQB, 128, s_, qaug_s[:, :, :], kvl_s, avn_s, fz0_s, oa_s[NQB, s_:s_ + 1, :], "oa_s%d" % NQB, None)

        ar.release(m_passB)
        NT = NQB + 1
        h2T_all = tb("h2T_all", [128, 8, NT * 128], BF16)
        gates_all = tb("gates_all", [128, NT, 65], F32)
        S.op("pool", lambda e: e.memset(gates_all[:], 1.0), writes=["gates_all"])
        m_passC = ar.mark()
        gt1p = tb("gt1p", [128, D], F32)
        gs2p = tb("gs2p", [128, D], F32)
        sh2p = tb("sh2p", [128, D], F32)
        bcast_row(gt1p, "gt1p", mod[32:33, 2 * D:3 * D], modk(2 * D, 3 * D))
        bcast_row(gs2p, "gs2p", gs2[32:33, :], ["gs2"])
        bcast_row(sh2p, "sh2p", mod[32:33, 3 * D:4 * D], modk(3 * D, 4 * D))
        wm = tb("wm", [128, 8, 2048], BF16)
        wpa = tb("wpa", [128, 4, 1024], BF16)
        wpb = tb("wpb", [128, 4, 1024], BF16)
        wo = tb("wo", [128, 8, 1024], BF16)
        wr = tb("wr", [128, 8, 64], BF16)
        brb = tb("brb", [128, 64], F32)
        wst2 = [tb("wst2", [128, 8, 256], F32) for _ in range(2)]
        S.dma("sp", "ld_brb", brb[:], b_router[0:1, :].partition_broadcast(128), writes=["brb"])
        loads = []
        for g in range(8):
            loads.append((w_in_v[:, :, O_MERGE + g * 256:O_MERGE + (g + 1) * 256], wm[:, :, g * 256:(g + 1) * 256], 8, 256, "wm"))
        wpa_v = w_proj_a.rearrange("(k p) n -> p k n", p=128)
        wpb_v = w_proj_b.rearrange("(k p) n -> p k n", p=128)
        wo_v = w_out.rearrange("(k p) n -> p k n", p=128)
        for hf in range(4):
            loads.append((wpa_v[:, :, hf * 256:(hf + 1) * 256], wpa[:, :, hf * 256:(hf + 1) * 256], 4, 256, "wpa"))
            loads.append((wpb_v[:, :, hf * 256:(hf + 1) * 256], wpb[:, :, hf * 256:(hf + 1) * 256], 4, 256, "wpb"))
            loads.append((wo_v[:, :, hf * 256:(hf + 1) * 256], wo[:, :, hf * 256:(hf + 1) * 256], 8, 256, "wo"))
        loads.append((w_router.rearrange("(k p) n -> p k n", p=128), wr[:, :, :], 8, 64, "wr"))
        for li, (src_ap, dst_ap, nk, ncol, key) in enumerate(loads):
            wb_ = wst2[li % 2]
            wk = "wst2_%d" % (li % 2)
            S.dma("sp", "ld_" + wk, wb_[:, 0:nk, 0:ncol], src_ap, writes=[wk])
            S.op("pool" if li % 2 else "dve",
                 lambda e, wb_=wb_, dst_ap=dst_ap, nk=nk, ncol=ncol: e.tensor_copy(out=dst_ap, in_=wb_[:, 0:nk, 0:ncol]),
                 reads=[wk], writes=[key])
        c_hT = tb("c_hT", [128, 8, 128], BF16)
        c_oa = tb("c_oa", [128, 512], F32)
        c_oab = tb("c_oab", [128, 1024], BF16)
        c_oabT = tb("c_oabT", [128, 8, 128], BF16)
        c_x = tb("c_x", [128, D], F32)
        c_sgm = tb("c_sgm", [128, 2048], F32)
        c_t1 = tb("c_t1", [128, 512], F32)
        c_t2 = tb("c_t2", [128, 512], F32)
        c_mb = tb("c_mb", [128, D], BF16)
        c_mT = tb("c_mT", [128, 8, 128], BF16)
        c_x1 = tb("c_x1", [128, D], F32)
        c_ss = tb("c_ss", [128, 1], F32)
        c_h2 = tb("c_h2", [128, D], BF16)
        c_sc = tb("c_sc", [128, 64], F32)
        c_sbias = tb("c_sbias", [128, 64], F32)
        c_top = tb("c_top", [128, 8], F32)
        c_den = tb("c_den", [128, 1], F32)

        def passC(t):
            smp = (t == NQB)
            P = SB_ if smp else 128
            cols = slice(t * 128, t * 128 + P)
            gt1_ap = mod[0:P, 2 * D:3 * D] if smp else gt1p[:, :]
            gs2_ap = gs2[0:P, :] if smp else gs2p[:, :]
            sh2_ap = mod[0:P, 3 * D:4 * D] if smp else sh2p[:, :]
            akeys = (modk(2 * D, 4 * D) + ["gs2"]) if smp else ["gt1p", "gs2p", "sh2p"]
            S.dma("sp", "ld_chT", c_hT[:, :, 0:P], hT_s[t, :, :, 0:P], reads=["hT_s%d" % t], writes=["c_hT"])
            S.dma("sp", "ld_coa", c_oa[0:P, :], oa_s[t, 0:P, :], reads=["oa_s%d" % t], writes=["c_oa"])
            S.dma("sp", "ld_cob", c_oab[0:P, 512:1024], ob_s[t, 0:P, :], reads=["ob_s%d" % t], writes=["c_oab_b"])
            S.dma("sp", "ld_cx", c_x[0:P, :], xs[:, :] if smp else xp[8 * t + 7, :, :], writes=["c_x"])
            S.op("pool", lambda e: e.tensor_copy(out=c_oab[0:P, 0:512], in_=c_oa[0:P, :]), reads=["c_oa"], writes=["c_oab_a"])
            for k in range(8):
                S.op("pe", lambda e, k=k: e.transpose(out=pT[:, k, 0:P], in_=c_oab[0:P, k * 128:(k + 1) * 128], identity=ident[0:P, 0:P]),
                     reads=["c_oab_a", "c_oab_b", "ident"], writes=["pT"])
            S.op("act", lambda e: e.copy(out=c_oabT[:, :, 0:P], in_=pT[:, :, 0:P]), reads=["pT"], writes=["c_oabT"])
            for g in range(4):
                pp, pk = nextbank()
                for k in range(8):
                    S.op("pe", lambda e, k=k, g=g, pp=pp: e.matmul(pp[0:P, :], lhsT=c_hT[:, k, 0:P], rhs=wm[:, k, g * 512:(g + 1) * 512],
                                                                  start=(k == 0), stop=(k == 7)), reads=["c_hT", "wm"], writes=[pk])
                S.op("act", lambda e, g=g, pp=pp: e.activation(out=c_sgm[0:P, g * 512:(g + 1) * 512], in_=pp[0:P, :], func=AF.Sigmoid),
                     reads=[pk], writes=["c_sgm"])
            for hf in range(2):
                pya, pyak = nextbank()
                pyb, pybk = nextbank()
                for k in range(4):
                    S.op("pe", lambda e, k=k, hf=hf, pya=pya: e.matmul(pya[0:P, :], lhsT=c_oabT[:, k, 0:P], rhs=wpa[:, k, hf * 512:(hf + 1) * 512],
                                                                      start=(k == 0), stop=(k == 3)), reads=["c_oabT", "wpa"], writes=[pyak])
                for k in range(4):
                    S.op("pe", lambda e, k=k, hf=hf, pyb=pyb: e.matmul(pyb[0:P, :], lhsT=c_oabT[:, 4 + k, 0:P], rhs=wpb[:, k, hf * 512:(hf + 1) * 512],
                                                                      start=(k == 0), stop=(k == 3)), reads=["c_oabT", "wpb"], writes=[pybk])
                S.op("dve", lambda e, hf=hf, pya=pya: e.tensor_tensor(out=c_t1[0:P, :], in0=pya[0:P, :], in1=c_sgm[0:P, hf * 512:(hf + 1) * 512],
                                                                     op=ALU.mult), reads=[pyak, "c_sgm"], writes=["c_t1"])
                S.op("dve", lambda e, hf=hf, pyb=pyb: e.tensor_tensor(out=c_t2[0:P, :], in0=pyb[0:P, :],
                                                                     in1=c_sgm[0:P, 1024 + hf * 512:1024 + (hf + 1) * 512], op=ALU.mult),
                     reads=[pybk, "c_sgm"], writes=["c_t2"])
                S.op("pool", lambda e, hf=hf: e.tensor_tensor(out=c_mb[0:P, hf * 512:(hf + 1) * 512], in0=c_t1[0:P, :], in1=c_t2[0:P, :], op=ALU.add),
                     reads=["c_t1", "c_t2"], writes=["c_mb"])
            for k in range(8):
                S.op("pe", lambda e, k=k: e.transpose(out=pT[:, k, 0:P], in_=c_mb[0:P, k * 128:(k + 1) * 128], identity=ident[0:P, 0:P]),
                     reads=["c_mb", "ident"], writes=["pT"])
            S.op("act", lambda e: e.copy(out=c_mT[:, :, 0:P], in_=pT[:, :, 0:P]), reads=["pT"], writes=["c_mT"])
            for hf in range(2):
                pmo, pmok = nextbank()
                for k in range(8):
                    S.op("pe", lambda e, k=k, hf=hf, pmo=pmo: e.matmul(pmo[0:P, :], lhsT=c_mT[:, k, 0:P], rhs=wo[:, k, hf * 512:(hf + 1) * 512],
                                                                      start=(k == 0), stop=(k == 7)), reads=["c_mT", "wo"], writes=[pmok])
                S.op("dve", lambda e, hf=hf, pmo=pmo: e.tensor_tensor(out=c_x1[0:P, hf * 512:(hf + 1) * 512], in0=pmo[0:P, :],
                                                                     in1=gt1_ap[:, hf * 512:(hf + 1) * 512], op=ALU.mult),
                     reads=[pmok] + akeys, writes=["c_x1"])
            S.op("pool", lambda e: e.tensor_tensor(out=c_x1[0:P, :], in0=c_x1[0:P, :], in1=c_x[0:P, :], op=ALU.add),
                 reads=["c_x1", "c_x"], writes=["c_x1"])
            oy = o_ys[:, :] if smp else o_yp[t, :, :]
            S.dma("pool", "st_x1", oy, c_x1[0:P, :], reads=["c_x1"], writes=["oy%d" % t])
            S.op("act", lambda e: e.activation(out=c_mb[0:P, :], in_=c_x1[0:P, :], func=AF.Square, accum_out=c_ss[0:P, :]),
                 reads=["c_x1"], writes=["c_mb", "c_ss"])
            S.op("act", lambda e: e.activation(out=c_ss[0:P, :], in_=c_ss[0:P, :], func=AF.Sqrt, scale=1.0 / D, bias=EPS),
                 reads=["c_ss"], writes=["c_ss"])
            S.op("dve", lambda e: e.reciprocal(out=c_ss[0:P, :], in_=c_ss[0:P, :]), reads=["c_ss"], writes=["c_ss"])
            S.op("dve", lambda e: e.scalar_tensor_tensor(out=c_x[0:P, :], in0=c_x1[0:P, :], scalar=c_ss[0:P, 0:1], in1=gs2_ap,
                                                         op0=ALU.mult, op1=ALU.mult), reads=["c_x1", "c_ss", "c_x"] + akeys, writes=["c_x"])
            S.op("pool", lambda e: e.tensor_tensor(out=c_h2[0:P, :], in0=c_x[0:P, :], in1=sh2_ap, op=ALU.add),
                 reads=["c_x"] + akeys, writes=["c_h2"])
            for k in range(8):
                S.op("pe", lambda e, k=k: e.transpose(out=pT[:, k, 0:P], in_=c_h2[0:P, k * 128:(k + 1) * 128], identity=ident[0:P, 0:P]),
                     reads=["c_h2", "ident"], writes=["pT"])
            S.op("act", lambda e: e.copy(out=h2T_all[:, :, cols], in_=pT[:, :, 0:P]), reads=["pT"], writes=["h2T_all"])
            pr, prk = nextbank()
            for k in range(8):
                S.op("pe", lambda e, k=k, pr=pr: e.matmul(pr[0:P, 0:64], lhsT=h2T_all[:, k, cols], rhs=wr[:, k, :],
                                                         start=(k == 0), stop=(k == 7)), reads=["h2T_all", "wr"], writes=[prk])
            S.op("act", lambda e, pr=pr: e.activation(out=c_sc[0:P, :], in_=pr[0:P, 0:64], func=AF.Sigmoid), reads=[prk], writes=["c_sc"])
            S.op("dve", lambda e: e.tensor_tensor(out=c_sbias[0:P, :], in0=c_sc[0:P, :], in1=brb[0:P, :], op=ALU.add),
                 reads=["c_sc", "brb"], writes=["c_sbias"])
            S.op("dve", lambda e: e.max(out=c_top[0:P, :], in_=c_sbias[0:P, :]), reads=["c_sbias"], writes=["c_top"])
            S.op("dve", lambda e: e.tensor_scalar(out=c_sbias[0:P, :], in0=c_sbias[0:P, :], scalar1=c_top[0:P, 5:6], scalar2=None,
                                                  op0=ALU.is_ge), reads=["c_sbias", "c_top"], writes=["c_sbias"])
            S.op("dve", lambda e: e.tensor_tensor(out=c_sc[0:P, :], in0=c_sc[0:P, :], in1=c_sbias[0:P, :], op=ALU.mult),
                 reads=["c_sc", "c_sbias"], writes=["c_sc"])
            S.op("dve", lambda e: e.tensor_reduce(out=c_den[0:P, :], in_=c_sc[0:P, :], axis=AX.X, op=ALU.add),
                 reads=["c_sc"], writes=["c_den"])
            S.op("dve", lambda e: e.reciprocal(out=c_den[0:P, :], in_=c_den[0:P, :]), reads=["c_den"], writes=["c_den"])
            S.op("dve", lambda e: e.tensor_scalar(out=gates_all[0:P, t, 0:64], in0=c_sc[0:P, :], scalar1=c_den[0:P, 0:1], scalar2=2.5,
                                                  op0=ALU.mult, op1=ALU.mult), reads=["c_sc", "c_den", "gates_all"], writes=["gates_all"])

        for t in range(NT):
            passC(t)

        ar.release(m_passC)
        gt2p = tb("gt2p", [128, D], F32)
        bcast_row(gt2p, "gt2p", mod[32:33, 5 * D:6 * D], modk(5 * D, 6 * D))
        yacc = tb("yacc", [128, NT, D], F32)
        S.op("pool", lambda e: e.memset(yacc[:], 0.0), writes=["yacc"])
        wgf = [tb("wgf", [128, 8, 256], F32) for _ in range(2)]
        wdf = [tb("wdf", [128, D], F32) for _ in range(2)]
        wgb = [tb("wgb", [128, 8, 256], BF16) for _ in range(2)]
        wdb = [tb("wdb", [128, D], BF16) for _ in range(2)]
        m_sa = tb("m_sa", [128, 128], F32)
        m_act = tb("m_act", [128, 128], BF16)
        m_actT = tb("m_actT", [128, 128], BF16)
        for ex in range(65):
            b_ = ex % 2
            gsrc = (w_exp_gu[ex] if ex < 64 else w_sh_gu).rearrange("(k p) f -> p k f", p=128)
            dsrc = w_exp_down[ex] if ex < 64 else w_sh_down
            S.dma("sp", "ld_wgf%d" % b_, wgf[b_][:], gsrc, writes=["wgf%d" % b_])
            S.dma("sp", "ld_wdf%d" % b_, wdf[b_][:], dsrc[:, :], writes=["wdf%d" % b_])
            S.op("pool", lambda e, b_=b_: e.tensor_copy(out=wgb[b_][:], in_=wgf[b_][:]), reads=["wgf%d" % b_], writes=["wgb%d" % b_])
            S.op("pool", lambda e, b_=b_: e.tensor_copy(out=wdb[b_][:], in_=wdf[b_][:]), reads=["wdf%d" % b_], writes=["wdb%d" % b_])
            for t in range(NT):
                P = SB_ if t == NQB else 128
                cols = slice(t * 128, t * 128 + P)
                pgu, pguk = banks[2 + (t % 4)]
                for k in range(8):
                    S.op("pe", lambda e, k=k, pgu=pgu, cols=cols, P=P, b_=b_: e.matmul(pgu[0:P, 0:256], lhsT=h2T_all[:, k, cols], rhs=wgb[b_][:, k, :],
                                                                                      start=(k == 0), stop=(k == 7)),
                         reads=["h2T_all", "wgb%d" % b_], writes=[pguk])
                S.op("act", lambda e, pgu=pgu, P=P: e.activation(out=m_sa[0:P, :], in_=pgu[0:P, 0:128], func=AF.Silu), reads=[pguk], writes=["m_sa"])
                S.op("dve", lambda e, pgu=pgu, P=P, t=t, ex=ex: e.scalar_tensor_tensor(out=m_act[0:P, :], in0=m_sa[0:P, :],
                                                                                      scalar=gates_all[0:P, t, ex:ex + 1], in1=pgu[0:P, 128:256],
                                                                                      op0=ALU.mult, op1=ALU.mult),
                     reads=["m_sa", pguk, "gates_all"], writes=["m_act"])
                S.op("pe", lambda e, P=P: e.transpose(out=pT[:, 0, 0:P], in_=m_act[0:P, :], identity=ident[0:P, 0:P]),
                     reads=["m_act", "ident"], writes=["pT"])
                S.op("act", lambda e, P=P: e.copy(out=m_actT[:, 0:P], in_=pT[:, 0, 0:P]), reads=["pT"], writes=["m_actT"])
                for hf in range(2):
                    S.op("pe", lambda e, hf=hf, P=P, b_=b_: e.matmul(pAB[0:P, hf * 512:(hf + 1) * 512], lhsT=m_actT[:, 0:P], rhs=wdb[b_][:, hf * 512:(hf + 1) * 512],
                                                                    start=True, stop=True), reads=["m_actT", "wdb%d" % b_], writes=["pz0", "pz1"])
                S.op("dve", lambda e, P=P, t=t: e.tensor_tensor(out=yacc[0:P, t, :], in0=yacc[0:P, t, :], in1=pAB[0:P, :], op=ALU.add),
                     reads=["pz0", "pz1", "yacc%d" % t], writes=["yacc%d" % t])
        f_x1 = [tb("f_x1", [128, D], F32) for _ in range(2)]
        for t in range(NT):
            smp = (t == NQB)
            P = SB_ if smp else 128
            b_ = t % 2
            gt2_ap = mod[0:P, 5 * D:6 * D] if smp else gt2p[:, :]
            akeys = modk(5 * D, 6 * D) if smp else ["gt2p"]
            oy = o_ys[:, :] if smp else o_yp[t, :, :]
            S.dma("sp", "ld_fx1_%d" % b_, f_x1[b_][0:P, :], oy, reads=["oy%d" % t], writes=["f_x1_%d" % b_])
            S.op("dve", lambda e, t=t, P=P, gt2_ap=gt2_ap: e.tensor_tensor(out=yacc[0:P, t, :], in0=yacc[0:P, t, :], in1=gt2_ap, op=ALU.mult),
                 reads=["yacc%d" % t, "yacc"] + akeys, writes=["yacc%d" % t])
            S.op("pool", lambda e, t=t, P=P, b_=b_: e.tensor_tensor(out=f_x1[b_][0:P, :], in0=f_x1[b_][0:P, :], in1=yacc[0:P, t, :], op=ALU.add),
                 reads=["yacc%d" % t, "f_x1_%d" % b_], writes=["f_x1_%d" % b_])
            S.dma("pool", "st_y%d" % b_, oy, f_x1[b_][0:P, :], reads=["f_x1_%d" % b_], writes=["oy%d" % t])

        S.finalize()
    return nc


def kernel(**inp):
    f = lambda k: np.ascontiguousarray(np.asarray(inp[k]))
    x_prompt = f("x_prompt")[0]
    x_sample = f("x_sample")[:, 0]
    c_prompt = f("c_prompt")
    c_sample = f("c_sample")
    cache_win = f("cache_win_kv")[0].reshape(32, 512, 256)
    state_conv = f("state_conv")[0]
    xq = x_prompt.reshape(T // 128, 128, D)
    NBLK = T // 128 + 7
    import ml_dtypes
    bf = ml_dtypes.bfloat16
    LLOC = NBLK * 128
    NCB = 8 * NBLK
    NCT = (NCB + 127) // 128
    NSBK = 2 * NBLK
    tpos = np.arange(LLOC)
    kaug_pos = np.stack([np.ones(LLOC), np.ones(LLOC), tpos % 128, tpos - tpos % 128, np.zeros(LLOC)]).astype(np.float32).astype(bf)
    nn = np.arange(NCT * 128)
    kaug_cmp = np.stack([np.ones_like(nn), np.ones_like(nn), 16 * (nn % 128), 2048 * (nn // 128), np.ones_like(nn)]).astype(np.float32).astype(bf)
    slopes = 2.0 ** -(np.arange(8) + 1.0)
    qi = np.arange(128)
    qaug_all = np.zeros((T // 128 + 7, 5, 8, 128), np.float32)
    for J in range(T // 128 + 7):
        qaug_all[J, 0] = -slopes[:, None] * qi[None, :]
        qaug_all[J, 1] = -slopes[:, None] * (128.0 * J)
        qaug_all[J, 2] = slopes[:, None]
        qaug_all[J, 3] = slopes[:, None]
        qaug_all[J, 4] = 31.0 * slopes[:, None]
    qaug_own = np.ascontiguousarray(qaug_all[7::8][:16]).astype(bf)
    bb = np.arange(NSBK)
    band = ((nn[:, None] >= 4 * bb[None, :] - 1) & (nn[:, None] <= 4 * bb[None, :] + 3)).astype(np.float32).astype(bf)
    nc = build_nc()
    qaug_samp = np.ascontiguousarray(qaug_all[128]).astype(bf)
    page_table = f("page_table").astype(np.int32)
    cache_rows = f("cache_nsa_kv")[0].reshape(5120 * 128, 512)
    in_maps = []
    for c in range(NC):
        call = np.zeros((33, D), np.float32)
        call[0:SB_] = c_sample[SB_ * c:SB_ * (c + 1)]
        call[32] = c_prompt[0]
        in_maps.append({
            "xp": np.concatenate([np.zeros((7 - c, 128, D), np.float32), xq, np.zeros((c, 128, D), np.float32)], 0)[:NBLK],
            "vmask": np.ascontiguousarray(np.broadcast_to((np.arange(8) >= 7 - c).astype(np.float32)[None, :], (128, 8))),
            "xs": np.ascontiguousarray(x_sample[SB_ * c:SB_ * (c + 1)]),
            "call": call,
            "w_ada": f("w_ada")[0], "b_ada": f("b_ada"), "norm1_g": f("norm1_g"), "w_in": f("w_in")[0],
            "norm2_g": f("norm2_g"), "w_proj_a": f("w_proj_a")[0], "w_proj_b": f("w_proj_b")[0], "w_out": f("w_out")[0],
            "w_router": f("w_router")[0], "b_router": f("b_router"), "w_exp_gu": f("w_exp_gu")[0],
            "w_exp_down": f("w_exp_down")[0], "w_sh_gu": f("w_sh_gu")[0], "w_sh_down": f("w_sh_down")[0],
            "k_norm_g": f("k_norm_g")[0], "q_norm_g": f("q_norm_g"), "w_cmp": f("w_cmp")[0], "pe_cmp": f("pe_cmp")[0],
            "kaug_pos": kaug_pos, "kaug_cmp": kaug_cmp, "qaug": qaug_own, "band_tab": band,
            "availneg": np.ascontiguousarray(np.broadcast_to(np.where(bb < 2 * (7 - c), -1e30, 0.0).astype(np.float32)[None, :], (128, NSBK))),
            "forced0": np.ascontiguousarray(np.broadcast_to(np.where(bb == 2 * (7 - c), 1e30, -3e38).astype(np.float32)[None, :], (128, NSBK))),
            "kvalid": np.ascontiguousarray((np.arange(NCT * 128).reshape(NCT, 128).T >= 8 * (7 - c)).astype(np.float32)),
            "qaug_s": qaug_samp, "piota": np.arange(128, dtype=np.float32).reshape(128, 1),
            "ptab": np.ascontiguousarray(page_table[SB_ * c:SB_ * (c + 1)]),
            "cache": cache_rows,
            "cwin": np.ascontiguousarray(cache_win[SB_ * c:SB_ * (c + 1)]),
            "sconv": np.ascontiguousarray(state_conv[SB_ * c:SB_ * (c + 1)]),
            "sdelta": np.ascontiguousarray(f("state_delta")[0][SB_ * c:SB_ * (c + 1)]),
            "conv_w": f("conv_w")[0], "a_log": f("a_log"), "dt_bias": f("dt_bias"), "o_norm_g": f("o_norm_g"),
        })
    res = run_bass_kernel_spmd(nc, in_maps, core_ids=list(range(NC))).results
    kernel.last_res = res
    yq = np.zeros((T // 128, 128, D), np.float32)
    for c in range(NC):
        r_ = res[c]["o_yp"]
        yq[c::NC][:r_.shape[0]] = r_
    y_prompt = yq.reshape(1, T, D)
    y_sample = np.concatenate([res[c]["o_ys"] for c in range(NC)], 0).reshape(32, 1, D)
    kvp = np.zeros((T // 128, 128, 512), np.float32)
    for c in range(NC):
        r_ = res[c]["o_kvp"]
        kvp[c::NC][:r_.shape[0]] = r_
    kv_rows_prompt = kvp.reshape(1, 1, T, 4, 2, 64)
    win_prompt = np.concatenate([res[c]["o_winp"] for c in range(4, 8)], 0).reshape(1, 1, 512, 2, 2, 64)
    conv_prompt = res[7]["o_convp"].reshape(1, 1, 3, 1536)
    delta_prompt = res[0]["o_dp"].reshape(1, 1, 4, 128, 128)
    kv_rows_sample = np.concatenate([res[c]["o_kvs"] for c in range(NC)], 0).reshape(1, 32, 1, 4, 2, 64)
    win_sample = np.concatenate([res[c]["o_wins"] for c in range(NC)], 0).reshape(1, 32, 512, 2, 2, 64)
    conv_sample = np.concatenate([res[c]["o_convs"] for c in range(NC)], 0).reshape(1, 32, 3, 1536)
    delta_sample = np.concatenate([res[c]["o_ds"] for c in range(NC)], 0).reshape(1, 32, 4, 128, 128)
    return (y_prompt, y_sample, kv_rows_prompt, win_prompt, conv_prompt, delta_prompt,
            kv_rows_sample, win_sample, conv_sample, delta_sample)
```

```python
import numpy as np
import concourse.bass as bass
import concourse.mybir as mybir
from concourse.bass_utils import run_bass_kernel_spmd
from contextlib import ExitStack

F32 = mybir.dt.float32
BF16 = mybir.dt.bfloat16
I32 = mybir.dt.int32
AF = mybir.ActivationFunctionType
ALU = mybir.AluOpType
AX = mybir.AxisListType


class Op:
    __slots__ = ("eng", "fn", "deps", "signal", "semval", "kind", "chan", "chanval", "idx")

    def __init__(self, eng, fn, kind):
        self.eng = eng
        self.fn = fn
        self.kind = kind
        self.deps = []
        self.signal = False
        self.semval = 0
        self.chan = None
        self.chanval = 0


class Sched:
    ENGS = ("pe", "act", "dve", "pool", "sp")

    def __init__(self, nc, stack):
        self.nc = nc
        self.stack = stack
        self.ops = {e: [] for e in self.ENGS}
        self.lastw = {}
        self.readers = {}
        self.chans = {}
        self.sems = {e: stack.enter_context(nc.semaphore("s_" + e)) for e in self.ENGS}
        self.n = 0
        self.chan_last = {}
        self._bar_deps = []
        self._bar_pending = set()
        self._cap = None

    def barrier(self):
        assert self._cap is None
        deps = []
        for e in self.ENGS:
            for o in reversed(self.ops[e]):
                if o.kind != "dma":
                    deps.append(o)
                    break
        deps += list(self.chan_last.values())
        self._bar_deps = deps
        self._bar_pending = set(self.ENGS)

    def chan(self, name):
        c = self.chans.get(name)
        if c is None:
            c = [self.stack.enter_context(self.nc.semaphore("c_" + name)), 0]
            self.chans[name] = c
        return c

    def _add(self, o, reads, writes):
        deps = []
        seen = set()

        def add(d, raw):
            if d is None or id(d) in seen:
                return
            if d.kind != "dma" and o.kind != "dma" and d.eng == o.eng:
                if o.eng == "pe":
                    return
            seen.add(id(d))
            deps.append(d)

        for k in reads:
            add(self.lastw.get(k), True)
        for k in writes:
            add(self.lastw.get(k), False)
            for r in self.readers.get(k, {}).values():
                if isinstance(r, list):
                    for rr in r:
                        add(rr, False)
                else:
                    add(r, False)
        if o.eng in self._bar_pending:
            self._bar_pending.discard(o.eng)
            for d in self._bar_deps:
                if d.kind == "dma" or d.eng != o.eng:
                    if id(d) not in seen:
                        seen.add(id(d))
                        deps.append(d)
        if o.kind == "dma":
            self.chan_last[id(o.chan)] = o
        for k in reads:
            rd = self.readers.setdefault(k, {})
            if o.kind == "dma":
                rd.setdefault("dma", []).append(o)
            else:
                rd[o.eng] = o
        for k in writes:
            self.lastw[k] = o
            self.readers[k] = {}
        o.deps = deps
        o.idx = self.n
        self.n += 1
        self.ops[o.eng].append(o)
        return o

    def _commit(self, o, chan, reads, writes):
        if chan is not None:
            c = self.chan(chan)
            c[1] += 16
            o.chan = c[0]
            o.chanval = c[1]
        return self._add(o, reads, writes)

    def _rec(self, o, chan, reads, writes):
        if self._cap is not None:
            self._cap.append((o, chan, list(reads), list(writes)))
            return o
        return self._commit(o, chan, reads, writes)

    def op(self, eng, fn, reads=(), writes=()):
        return self._rec(Op(eng, fn, "c"), None, reads, writes)

    def dma(self, q, chan, out, in_, reads=(), writes=(), **kw):
        return self._rec(Op(q, lambda e: e.dma_start(out=out, in_=in_, **kw), "dma"), chan, reads, writes)

    def dma_fn(self, q, chan, fn, reads=(), writes=()):
        return self._rec(Op(q, fn, "dma"), chan, reads, writes)

    def capture(self, f):
        assert self._cap is None
        self._cap = []
        try:
            f()
        finally:
            lst, self._cap = self._cap, None
        return lst

    def replay(self, chains):
        chains = [c for c in chains if c]
        pos = [0] * len(chains)
        while True:
            best, bf_ = None, None
            for i, c in enumerate(chains):
                if pos[i] < len(c):
                    fr = pos[i] / len(c)
                    if bf_ is None or fr < bf_:
                        best, bf_ = i, fr
            if best is None:
                break
            self._commit(*chains[best][pos[best]])
            pos[best] += 1

    def finalize(self, final_waits=()):
        for e in self.ENGS:
            for o in self.ops[e]:
                for d in o.deps:
                    if d.kind != "dma":
                        d.signal = True
        for e in self.ENGS:
            c = 0
            for o in self.ops[e]:
                if o.kind != "dma" and o.signal:
                    c += 1
                    o.semval = c
        sems = self.sems
        ops = self.ops
        chans = self.chans

        def emit(ename, eng):
            waited = {}
            for o in ops[ename]:
                for d in o.deps:
                    if d.kind == "dma":
                        s, v = d.chan, d.chanval
                    else:
                        s, v = sems[d.eng], d.semval
                    key = id(s)
                    if waited.get(key, 0) < v:
                        eng.wait_ge(s, v)
                        waited[key] = v
                ins = o.fn(eng)
                if o.kind == "dma":
                    ins.then_inc(o.chan, 16)
                elif o.signal:
                    ins.then_inc(sems[ename], 1)
            if ename == "sp":
                for name, (s, v) in chans.items():
                    if v > 0 and waited.get(id(s), 0) < v:
                        eng.wait_ge(s, v)

        with self.nc.Block() as block:
            @block.tensor
            def _(e):
                emit("pe", e)

            @block.scalar
            def _(e):
                emit("act", e)

            @block.vector
            def _(e):
                emit("dve", e)

            @block.gpsimd
            def _(e):
                emit("pool", e)

            @block.sync
            def _(e):
                emit("sp", e)


REG = {}


class Arena:
    def __init__(self, S, base):
        self.S = S
        self.base = base
        self.W = base.shape[1]
        self.off = 0

    def alloc(self, shape, dt, parts=None):
        n = 1
        for s in shape[1:]:
            n *= s
        words = n if dt in (F32, I32) else (n + 1) // 2
        assert self.off + words <= self.W, ("arena overflow", self.off, words, self.W)
        v = self.base[:, self.off:self.off + words]
        self.off += words
        if dt == BF16:
            v = v.bitcast(BF16)[:, 0:n]
        elif dt == I32:
            v = v.bitcast(I32)
        if len(shape) == 3:
            v = v.rearrange("p (a b) -> p a b", b=shape[2])
        elif len(shape) == 4:
            v = v.rearrange("p (a b c) -> p a b c", b=shape[2], c=shape[3])
        if shape[0] != 128:
            v = v[0:shape[0]]
        return v

    def mark(self):
        return self.off

    def release(self, m):
        self.off = m
        self.S.barrier()


D = 1024
T = 16384
NC = 8
NQB = 16
SB_ = 4
INW = 5408
O_Q, O_KV, O_G, O_QKV, O_A, O_B, O_GATE, O_MERGE = 0, 512, 1280, 1304, 2840, 2844, 2848, 3360
EPS = 1e-6
import os
STAGE = int(os.environ.get('K_STAGE', '9'))


def build_nc(NQB=NQB):
    nc = bass.Bass("TRN2", target_bir_lowering=False)
    dt_in = lambda n, s, d=F32: nc.dram_tensor(n, s, d, kind="ExternalInput").ap()
    dt_out = lambda n, s, d=F32: nc.dram_tensor(n, s, d, kind="ExternalOutput").ap()
    NBLK = 8 * NQB + 7
    xp = dt_in("xp", [NBLK, 128, D])
    vmask_d = dt_in("vmask", [128, 8])
    xs = dt_in("xs", [SB_, D])
    call = dt_in("call", [33, D])
    w_ada = dt_in("w_ada", [D, 6 * D])
    b_ada = dt_in("b_ada", [1, 6 * D])
    norm1_g = dt_in("norm1_g", [1, D])
    norm2_g = dt_in("norm2_g", [1, D])
    w_proj_a = dt_in("w_proj_a", [512, D])
    w_proj_b = dt_in("w_proj_b", [512, D])
    w_out = dt_in("w_out", [D, D])
    w_router = dt_in("w_router", [D, 64])
    b_router = dt_in("b_router", [1, 64])
    w_exp_gu = dt_in("w_exp_gu", [64, D, 256])
    w_exp_down = dt_in("w_exp_down", [64, 128, D])
    w_sh_gu = dt_in("w_sh_gu", [D, 256])
    w_sh_down = dt_in("w_sh_down", [128, D])
    k_norm_g = dt_in("k_norm_g", [3, 64])
    q_norm_g = dt_in("q_norm_g", [1, 64])
    w_cmp = dt_in("w_cmp", [2, 32, 64, 64])
    pe_cmp = dt_in("pe_cmp", [2, 32, 64])
    NBLKT = max(NBLK, 135)
    LLOC = NBLKT * 128
    NCB = 8 * NBLKT
    NCT = (NCB + 127) // 128
    NSBK = 2 * NBLKT
    kaug_pos = dt_in("kaug_pos", [5, LLOC], BF16)
    kaug_cmp = dt_in("kaug_cmp", [5, NCT * 128], BF16)
    qaug = dt_in("qaug", [NQB, 5, 8, 128], BF16)
    band_tab = dt_in("band_tab", [NCT * 128, NSBK], BF16)
    availneg = dt_in("availneg", [128, NSBK])
    forced0 = dt_in("forced0", [128, NSBK])
    kvalid = dt_in("kvalid", [128, NCT])
    qaug_s = dt_in("qaug_s", [5, 8, 128], BF16)
    piota = dt_in("piota", [128, 1])
    ptab = dt_in("ptab", [SB_, 128], I32)
    cache = dt_in("cache", [5120 * 128, 512])
    w_in = dt_in("w_in", [D, INW])
    cwin = dt_in("cwin", [SB_, 512, 256])
    sconv = dt_in("sconv", [SB_, 3, 1536])
    sdelta = dt_in("sdelta", [SB_, 4, 128, 128])
    conv_w = dt_in("conv_w", [4, 1536])
    a_log = dt_in("a_log", [1, 4])
    dt_bias = dt_in("dt_bias", [1, 4])
    o_norm_g = dt_in("o_norm_g", [1, 128])

    o_kvp = dt_out("o_kvp", [NQB, 128, 512])
    o_winp = dt_out("o_winp", [128, 256])
    o_convp = dt_out("o_convp", [3, 1536])
    o_kvs = dt_out("o_kvs", [SB_, 512])
    o_wins = dt_out("o_wins", [SB_, 512, 256])
    o_convs = dt_out("o_convs", [SB_, 3, 1536])
    o_ds = dt_out("o_ds", [SB_, 4, 128, 128])
    o_dp = dt_out("o_dp", [4, 128, 128])
    o_yp = dt_out("o_yp", [NQB, 128, D])
    o_ys = dt_out("o_ys", [SB_, D])
    ob_s = nc.dram_tensor("ob_s", [NQB + 1, 128, 512], BF16).ap()
    oa_s = nc.dram_tensor("oa_s", [NQB + 1, 128, 512], F32).ap()
    KT_s = nc.dram_tensor("KT_s", [4, 128, LLOC], BF16).ap()
    V_s = nc.dram_tensor("V_s", [LLOC, 4, 65], BF16).ap()
    KcT_s = nc.dram_tensor("KcT_s", [128, NCT * 128], BF16).ap()

    hT_s = nc.dram_tensor("hT_s", [NQB + 1, 128, 8, 128], BF16).ap()

    with ExitStack() as st:
        S = Sched(nc, st)
        sb = lambda name, shape, dt: st.enter_context(nc.sbuf_tensor(name, shape, dt))
        ps = lambda name, shape, dt: st.enter_context(nc.psum_tensor(name, shape, dt))

        arena_t = sb("arena", [128, 43000], F32)
        ar = Arena(S, arena_t[:])
        def tb(name, shape, dt):
            REG[name] = (ar.off, shape, str(dt))
            return ar.alloc(shape, dt)
        identf = sb("identf", [128, 128], F32)
        ident = sb("ident", [128, 128], BF16)
        onesf = sb("onesf", [128, 128], F32)
        S.op("pool", lambda e: e.memset(identf[:], 0.0), writes=["identf"])
        S.op("pool", lambda e: e.affine_select(out=identf[:], in_=identf[:], pattern=[[-1, 128]],
                                               compare_op=ALU.not_equal, fill=1.0, base=0, channel_multiplier=1),
             reads=["identf"], writes=["identf"])
        S.op("pool", lambda e: e.tensor_copy(out=ident[:], in_=identf[:]), reads=["identf"], writes=["ident"])
        S.op("pool", lambda e: e.memset(onesf[:], 1.0), writes=["onesf"])

        pAB = ps("pAB", [128, 1024], F32)
        pA = pAB[:, 0:512]
        pB = pAB[:, 512:1024]
        pC = ps("pz2", [128, 512], F32)
        pD = ps("pz3", [128, 512], F32)
        pT = ps("pT", [128, 8, 128], BF16)
        pTf = ps("pTf", [128, 8, 64], F32)
        pT2 = pTf[:].rearrange("p a b -> p (a b)").bitcast(BF16).rearrange("p (a b) -> p a b", b=128)

        mod = sb("mod", [33, 6 * D], F32)
        NW1 = 768 + 1536 + 520
        m_passA = ar.mark()
        winb = tb("winb", [128, 8, NW1], BF16)
        gs1p = tb("gs1p", [128, D], F32)
        sh1p = tb("sh1p", [128, D], F32)
        m_ada = ar.mark()
        c_in = tb("c_in", [33, D], F32)
        cT = tb("cT", [128, 8, 33], F32)
        badab = tb("badab", [33, 6 * D], F32)
        g1b = tb("g1b", [33, D], F32)
        gs1 = sb("gs1", [33, D], F32)
        wst = [tb("wst%d" % i, [128, 8, 512], F32) for i in range(2)]
        S.dma("sp", "ld_c", c_in[:], call[:, :], writes=["c_in"])
        S.dma("sp", "ld_bada", badab[:], b_ada[0:1, :].partition_broadcast(33), writes=["badab"])
        S.dma("sp", "ld_g1", g1b[:], norm1_g[0:1, :].partition_broadcast(33), writes=["g1b"])
        S.op("act", lambda e: e.activation(out=c_in[:], in_=c_in[:], func=AF.Silu), reads=["c_in"], writes=["c_in"])
        for k in range(8):
            S.op("pe", lambda e, k=k: e.transpose(out=pTf[:, k, 0:33], in_=c_in[:, k * 128:(k + 1) * 128],
                                                  identity=identf[0:33, 0:33]),
                 reads=["c_in", "identf"], writes=["pTf"])
        S.op("dve", lambda e: e.tensor_copy(out=cT[:], in_=pTf[:, :, 0:33]), reads=["pTf"], writes=["cT"])
        w_ada_v = w_ada.rearrange("(k p) n -> p k n", p=128)
        pmod = [pA, pB]
        for g in range(12):
            wb_ = wst[g % 2]
            wk = "wst%d" % (g % 2)
            S.dma("sp", "ld_" + wk, wb_[:], w_ada_v[:, :, g * 512:(g + 1) * 512], writes=[wk])
            pm = pmod[g % 2]
            pk = "pz%d" % (g % 2)
            for k in range(8):
                S.op("pe", lambda e, k=k, wb_=wb_, pm=pm: e.matmul(pm[0:33, :], lhsT=cT[:, k, :], rhs=wb_[:, k, :],
                                                                 start=(k == 0), stop=(k == 7)),
                     reads=["cT", wk], writes=[pk])
            S.op("dve", lambda e, g=g, pm=pm: e.tensor_tensor(out=mod[:, g * 512:(g + 1) * 512], in0=pm[0:33, :],
                                                            in1=badab[:, g * 512:(g + 1) * 512], op=ALU.add),
                 reads=[pk, "badab"], writes=["mod%d" % g])
        modk = lambda a, b: ["mod%d" % g for g in range(a // 512, (b + 511) // 512)]
        S.op("dve", lambda e: e.scalar_tensor_tensor(out=gs1[:], in0=mod[:, D:2 * D], scalar=1.0, in1=g1b[:],
                                                     op0=ALU.add, op1=ALU.mult),
             reads=modk(D, 2 * D) + ["g1b"], writes=["gs1"])

        def bcast_row(dst, dkey, src_ap, skeys):
            for hh in range(2):
                S.op("pe", lambda e, hh=hh: e.matmul(pC[:, :], lhsT=onesf[32:33, :], rhs=src_ap[:, hh * 512:(hh + 1) * 512],
                                                     start=True, stop=True),
                     reads=skeys + ["onesf"], writes=["pz2"])
                S.op("act", lambda e, hh=hh: e.copy(out=dst[:, hh * 512:(hh + 1) * 512], in_=pC[:, :]),
                     reads=["pz2"], writes=[dkey])

        bcast_row(gs1p, "gs1p", gs1[32:33, :], ["gs1"])
        bcast_row(sh1p, "sh1p", mod[32:33, 0:D], modk(0, D))
        gs2 = sb("gs2", [33, D], F32)
        S.dma("sp", "ld_g1", g1b[:], norm2_g[0:1, :].partition_broadcast(33), reads=["gs1"], writes=["g1b"])
        S.op("dve", lambda e: e.scalar_tensor_tensor(out=gs2[:], in0=mod[:, 4 * D:5 * D], scalar=1.0, in1=g1b[:],
                                                     op0=ALU.add, op1=ALU.mult),
             reads=modk(4 * D, 5 * D) + ["g1b"], writes=["gs2"])

        w_in_v = w_in.rearrange("(k p) n -> p k n", p=128)
        col_src = [(O_KV, 512), (O_KV + 512, 256), (O_QKV, 512), (O_QKV + 512, 512), (O_QKV + 1024, 512),
                   (O_A, 8), (O_GATE, 512)]
        off = 0
        wcol = []
        for i, (c0, n) in enumerate(col_src):
            wb_ = wst[i % 2]
            wk = "wst%d" % (i % 2)
            S.dma("sp", "ld_" + wk, wb_[:, :, 0:n], w_in_v[:, :, c0:c0 + n], writes=[wk])
            eng = "pool" if i % 2 else "dve"
            S.op(eng, lambda e, wb_=wb_, n=n, off=off: e.tensor_copy(out=winb[:, :, off:off + n], in_=wb_[:, :, 0:n]),
                 reads=[wk], writes=["winb%d" % i])
            wcol.append((off, n, "winb%d" % i))
            off += n

        ar.release(m_ada)
        xts = [tb("xt%d" % i, [128, D], F32) for i in range(2)]
        junk = tb("junk", [128, D], BF16)
        ssq = [tb("ssq%d" % i, [128, 1], F32) for i in range(2)]
        tmpf = tb("tmpf", [128, D], F32)
        hb = tb("hb", [128, D], BF16)
        hTs = [tb("hT%d" % i, [128, 8, 128], BF16) for i in range(3)]
        _z0 = tb("z0", [128, NW1], F32)
        zs = [_z0, _z0]
        pz = [pA, pB, pC, pD]
        cnt = {"t": 0}

        def front(P, x_ap, gs_ap, sh_ap, gkeys, outs, vcol=None, groups=None):
            i = cnt["t"] % 2
            cnt["t"] += 1
            xt, xk = xts[i], "xt%d" % i
            sq, sk = ssq[i], "ssq%d" % i
            i3 = (cnt["t"] - 1) % 3
            hT, hk = hTs[i3], "hT%d" % i3
            z, zk = zs[i], "z0"
            S.dma("sp", "ld_" + xk, xt[0:P, :], x_ap, writes=[xk])
            S.op("act", lambda e: e.activation(out=junk[0:P, :], in_=xt[0:P, :], func=AF.Square, accum_out=sq[0:P, :]),
                 reads=[xk], writes=["junk", sk])
            S.op("act", lambda e: e.activation(out=sq[0:P, :], in_=sq[0:P, :], func=AF.Sqrt, scale=1.0 / D, bias=EPS),
                 reads=[sk], writes=[sk])
            S.op("dve", lambda e: e.reciprocal(out=sq[0:P, :], in_=sq[0:P, :]), reads=[sk], writes=[sk])
            S.op("dve", lambda e: e.scalar_tensor_tensor(out=tmpf[0:P, :], in0=xt[0:P, :], scalar=sq[0:P, 0:1], in1=gs_ap,
                                                         op0=ALU.mult, op1=ALU.mult),
                 reads=[xk, sk] + gkeys, writes=["tmpf"])
            S.op("pool", lambda e: e.tensor_tensor(out=hb[0:P, :], in0=tmpf[0:P, :], in1=sh_ap, op=ALU.add),
                 reads=["tmpf"] + gkeys, writes=["hb"])
            if vcol is not None:
                S.op("dve", lambda e: e.tensor_scalar(out=hb[0:P, :], in0=hb[0:P, :], scalar1=vcol, scalar2=None,
                                                      op0=ALU.mult), reads=["hb", "vmask"], writes=["hb"])
            for k in range(8):
                S.op("pe", lambda e, k=k: e.transpose(out=pT2[:, k, 0:P], in_=hb[0:P, k * 128:(k + 1) * 128],
                                                      identity=ident[0:P, 0:P]),
                     reads=["hb", "ident"], writes=["pTf"])
            S.op("act", lambda e: e.copy(out=hT[:, :, 0:P], in_=pT2[:, :, 0:P]), reads=["pTf"], writes=[hk])
            for j, (off, n, wkey) in enumerate(wcol):
                if groups is not None and j not in groups:
                    continue
                pp = pz[j % 2]
                pk = "pz%d" % (j % 2)
                for k in range(8):
                    S.op("pe", lambda e, k=k, pp=pp, off=off, n=n: e.matmul(pp[0:P, 0:n], lhsT=hT[:, k, 0:P],
                                                                            rhs=winb[:, k, off:off + n],
                                                                            start=(k == 0), stop=(k == 7)),
                         reads=[hk, wkey], writes=[pk])
                eng = "act" if j % 2 else "dve"
                if eng == "act":
                    S.op("act", lambda e, pp=pp, off=off, n=n: e.copy(out=z[0:P, off:off + n], in_=pp[0:P, 0:n]),
                         reads=[pk], writes=[zk])
                else:
                    S.op("dve", lambda e, pp=pp, off=off, n=n: e.tensor_copy(out=z[0:P, off:off + n], in_=pp[0:P, 0:n]),
                         reads=[pk], writes=[zk])
            outs(z, zk)
            return hT, hk, z, zk


        ZQ, ZA, ZG = 768, 768 + 1536, 768 + 1536 + 8

        def sample_delta(z, zk):
            P = SB_
            m_sd = ar.mark()
            acc = tb("sd_acc", [P, 1536], F32)
            sm = tb("sd_sm", [P, 64], F32)
            alb = tb("sd_alb", [P, 8], F32)
            onb = tb("sd_onb", [P, 128], F32)
            m_sd2 = ar.mark()
            cst = tb("sd_cst", [P, 3, 1536], F32)
            cwr = tb("sd_cwr", [P, 1536], F32)
            tmp = tb("sd_tmp", [P, 1536], F32)
            S.dma("sp", "ld_sd1", cst[:], sconv[:, :, :], writes=["sd_cst"])
            S.dma("sp", "ld_sd3", alb[:, 0:4], a_log[0:1, :].partition_broadcast(P), writes=["sd_alb"])
            S.dma("sp", "ld_sd4", alb[:, 4:8], dt_bias[0:1, :].partition_broadcast(P), writes=["sd_alb"])
            S.dma("sp", "ld_sd5", onb[:], o_norm_g[0:1, :].partition_broadcast(P), writes=["sd_onb"])
            for j in range(4):
                S.dma("sp", "ld_sd2", cwr[:], conv_w[j:j + 1, :].partition_broadcast(P), writes=["sd_cwr"])
                row = cst[:, j, :] if j < 3 else z[0:P, ZQ:ZQ + 1536]
                rkeys = ["sd_cst"] if j < 3 else [zk]
                if j == 0:
                    S.op("dve", lambda e, row=row: e.tensor_tensor(out=acc[:], in0=row, in1=cwr[:], op=ALU.mult),
                         reads=rkeys + ["sd_cwr"], writes=["sd_acc"])
                else:
                    S.op("dve", lambda e, row=row: e.tensor_tensor(out=tmp[:], in0=row, in1=cwr[:], op=ALU.mult),
                         reads=rkeys + ["sd_cwr"], writes=["sd_tmp"])
                    S.op("dve", lambda e: e.tensor_tensor(out=acc[:], in0=acc[:], in1=tmp[:], op=ALU.add),
                         reads=["sd_tmp", "sd_acc"], writes=["sd_acc"])
            S.op("act", lambda e: e.activation(out=acc[:], in_=acc[:], func=AF.Silu), reads=["sd_acc"], writes=["sd_acc"])
            S.op("dve", lambda e: e.tensor_tensor(out=tmp[:, 0:1024], in0=acc[:, 0:1024], in1=acc[:, 0:1024], op=ALU.mult),
                 reads=["sd_acc"], writes=["sd_tmp"])
            S.op("dve", lambda e: e.tensor_reduce(out=sm[:, 0:8], in_=tmp[:, 0:1024].rearrange("p (h d) -> p h d", d=128),
                                                  axis=AX.X, op=ALU.add),
                 reads=["sd_tmp"], writes=["sd_sm"])
            S.op("act", lambda e: e.activation(out=sm[:, 0:8], in_=sm[:, 0:8], func=AF.Sqrt, bias=EPS),
                 reads=["sd_sm"], writes=["sd_sm"])
            S.op("dve", lambda e: e.reciprocal(out=sm[:, 0:8], in_=sm[:, 0:8]), reads=["sd_sm"], writes=["sd_sm"])
            S.op("dve", lambda e: e.tensor_scalar(out=sm[:, 0:4], in0=sm[:, 0:4], scalar1=128.0 ** -0.5, scalar2=None,
                                                  op0=ALU.mult), reads=["sd_sm"], writes=["sd_sm"])
            S.op("dve", lambda e: e.tensor_tensor(out=acc[:, 0:1024].rearrange("p (h d) -> p h d", d=128),
                                                  in0=acc[:, 0:1024].rearrange("p (h d) -> p h d", d=128),
                                                  in1=sm[:, 0:8].unsqueeze(2).broadcast_to([P, 8, 128]), op=ALU.mult),
                 reads=["sd_sm", "sd_acc"], writes=["sd_acc"])
            ar.release(m_sd2)
            xx, ax, sp_, ea = sm[:, 16:20], sm[:, 20:24], sm[:, 24:28], sm[:, 28:32]
            S.op("dve", lambda e: e.tensor_tensor(out=xx, in0=z[0:P, ZA:ZA + 4], in1=alb[:, 4:8], op=ALU.add),
                 reads=[zk, "sd_alb"], writes=["sd_sm"])
            S.op("dve", lambda e: e.scalar_tensor_tensor(out=ax, in0=xx, scalar=-1.0, in1=xx, op0=ALU.mult, op1=ALU.max),
                 reads=["sd_sm"], writes=["sd_sm"])
            S.op("act", lambda e: e.activation(out=ax, in_=ax, func=AF.Exp, scale=-1.0), reads=["sd_sm"], writes=["sd_sm"])
            S.op("act", lambda e: e.activation(out=ax, in_=ax, func=AF.Ln, bias=1.0), reads=["sd_sm"], writes=["sd_sm"])
            S.op("dve", lambda e: e.scalar_tensor_tensor(out=sp_, in0=xx, scalar=0.0, in1=ax, op0=ALU.max, op1=ALU.add),
                 reads=["sd_sm"], writes=["sd_sm"])
            S.op("act", lambda e: e.activation(out=ea, in_=alb[:, 0:4], func=AF.Exp), reads=["sd_alb"], writes=["sd_sm"])
            S.op("dve", lambda e: e.scalar_tensor_tensor(out=sm[:, 8:12], in0=ea, scalar=-1.0, in1=sp_,
                                                         op0=ALU.mult, op1=ALU.mult), reads=["sd_sm"], writes=["sd_sm"])
            S.op("act", lambda e: e.activation(out=sm[:, 8:12], in_=sm[:, 8:12], func=AF.Exp), reads=["sd_sm"], writes=["sd_sm"])
            S.op("act", lambda e: e.activation(out=sm[:, 12:16], in_=z[0:P, ZA + 4:ZA + 8], func=AF.Sigmoid),
                 reads=[zk], writes=["sd_sm"])
            if STAGE < 2:
                ar.release(m_sd); return
            scr = nc.dram_tensor("sd_scr", [P, 1536 + 8], F32).ap()
            S.dma("sp", "st_sd1", scr[:, 0:1536], acc[:], reads=["sd_acc"], writes=["sd_scr"])
            S.dma("sp", "st_sd2", scr[:, 1536:1544], sm[:, 8:16], reads=["sd_sm"], writes=["sd_scr"])
            egb = tb("sd_egb", [128, P, 8], F32)
            krow = tb("sd_krow", [1, P, 512], F32)
            vrow = tb("sd_vrow", [1, P, 512], F32)
            S.dma("sp", "ld_sd6", egb[:], scr[:, 1536:1544].rearrange("(o s) e -> o s e", o=1).partition_broadcast(128)
                  if False else scr[:, 1536:1544].partition_broadcast(128), reads=["sd_scr"], writes=["sd_egb"])
            S.dma("sp", "ld_sd7", krow[:], scr[:, 512:1024].rearrange("(o s) e -> o s e", o=1), reads=["sd_scr"], writes=["sd_krow"])
            S.dma("sp", "ld_sd8", vrow[:], scr[:, 1024:1536].rearrange("(o s) e -> o s e", o=1), reads=["sd_scr"], writes=["sd_vrow"])
            if STAGE < 3:
                ar.release(m_sd); return
            qkT = tb("sd_qkT", [128, 8, P], F32)
            for j in range(8):
                S.op("pe", lambda e, j=j: e.transpose(out=pTf[:, j, 0:P], in_=acc[:, j * 128:(j + 1) * 128],
                                                      identity=identf[0:P, 0:P]),
                     reads=["sd_acc", "identf"], writes=["pTf"])
            S.op("dve", lambda e: e.tensor_copy(out=qkT[:], in_=pTf[:, :, 0:P]), reads=["pTf"], writes=["sd_qkT"])
            S0 = tb("sd_S0", [128, P * 4, 128], F32)
            S1 = S0
            for s_ in range(P):
                S.dma("sp", "ld_sd9", S0[:, s_ * 4:(s_ + 1) * 4, :], sdelta[s_].rearrange("h k v -> k h v"), writes=["sd_S0"])
            if STAGE < 4:
                ar.release(m_sd); return
            urow = tb("sd_urow", [1, P * 4, 128], F32)
            orow = vrow.rearrange("o s (h d) -> o (s h) d", d=128)
            for s_ in range(P):
                for h in range(4):
                    sh = s_ * 4 + h
                    S.op("pe", lambda e, s_=s_, h=h, sh=sh: e.matmul(pD[0:1, 0:128], lhsT=qkT[:, 4 + h, s_:s_ + 1],
                                                                    rhs=S0[:, sh, :], start=True, stop=True),
                         reads=["sd_qkT", "sd_S0"], writes=["pz3"])
                    S.op("dve", lambda e, s_=s_, h=h, sh=sh: e.scalar_tensor_tensor(
                        out=urow[0:1, sh, :], in0=pD[0:1, 0:128], scalar=egb[0:1, s_, h:h + 1],
                        in1=vrow[0:1, s_, h * 128:(h + 1) * 128], op0=ALU.mult, op1=ALU.subtract),
                        reads=["pz3", "sd_egb", "sd_vrow"], writes=["sd_urow"])
                    S.op("dve", lambda e, s_=s_, h=h, sh=sh: e.tensor_scalar(
                        out=urow[0:1, sh, :], in0=urow[0:1, sh, :], scalar1=egb[0:1, s_, 4 + h:5 + h], scalar2=-1.0,
                        op0=ALU.mult, op1=ALU.mult), reads=["sd_urow", "sd_egb"], writes=["sd_urow"])
                    S.op("pe", lambda e, s_=s_, h=h, sh=sh: e.matmul(pC[:, 0:128], lhsT=krow[0:1, s_, h * 128:(h + 1) * 128],
                                                                    rhs=urow[0:1, sh, :], start=True, stop=True),
                         reads=["sd_krow", "sd_urow"], writes=["pz2"])
                    S.op("dve", lambda e, s_=s_, h=h, sh=sh: e.scalar_tensor_tensor(
                        out=S1[:, sh, :], in0=S0[:, sh, :], scalar=egb[:, s_, h:h + 1], in1=pC[:, 0:128],
                        op0=ALU.mult, op1=ALU.add), reads=["sd_S0", "sd_egb", "pz2"], writes=["sd_S0"])
                    S.op("pe", lambda e, s_=s_, h=h, sh=sh: e.matmul(pD[0:1, 128:256], lhsT=qkT[:, h, s_:s_ + 1],
                                                                    rhs=S1[:, sh, :], start=True, stop=True),
                         reads=["sd_qkT", "sd_S0"], writes=["pz3"])
                    S.op("act", lambda e, sh=sh: e.copy(out=orow[0:1, sh, :], in_=pD[0:1, 128:256]),
                         reads=["pz3"], writes=["sd_vrow"])
            if STAGE < 5:
                ar.release(m_sd); return
            for s_ in range(P):
                S.dma("pool", "st_ds", o_ds[s_].rearrange("h k v -> k h v"), S1[:, s_ * 4:(s_ + 1) * 4, :], reads=["sd_S0"])
            scr2 = nc.dram_tensor("sd_scr2", [P, 512], F32).ap()
            S.dma("sp", "st_sd3", scr2.rearrange("(o s) e -> o s e", o=1), vrow[:], reads=["sd_vrow"], writes=["sd_scr2"])
            od_s = tb("sd_od", [P, 4, 128], F32)
            S.dma("sp", "ld_sd10", od_s[:], scr2.rearrange("s (h d) -> s h d", d=128), reads=["sd_scr2"], writes=["sd_od"])
            emit_ob(P, od_s, "sd_od", z[0:P, 2312:2824], [zk], NQB)
            ar.release(m_sd)

        def outs_sample(z, zk):
            S.dma("pool", "st_kvs", o_kvs[:, :], z[0:SB_, 0:512], reads=[zk], writes=["o_kvs"])
            S.dma("pool", "st_wins", o_wins[:, 511, :], z[0:SB_, 512:768], reads=[zk], writes=["o_wins"])
            S.dma("pool", "st_convs", o_convs[:, 2, :], z[0:SB_, 768:768 + 1536], reads=[zk])

        S.dma("pool", "cp_win", o_wins[:, 0:511, :], cwin[:, 1:512, :])
        S.dma("pool", "cp_conv", o_convs[:, 0:2, :], sconv[:, 1:3, :])

        pE = ps("pE", [128, 512], F32)
        pF = ps("pF", [128, 512], F32)
        banks = [(pC, "pz2"), (pD, "pz3"), (pE, "pE"), (pF, "pF")]
        bctr = {"i": 0}

        def nextbank():
            t = banks[bctr["i"] % len(banks)]
            bctr["i"] += 1
            return t

        vmask = sb("vmask_sb", [128, 8], F32)
        S.dma("sp", "ld_vm", vmask[:], vmask_d[:, :], writes=["vmask"])
        onesb = sb("onesb", [128, 128], BF16)
        negones = tb("negones", [128, 128], F32)
        triU = tb("triU", [128, 128], F32)
        NMs = tb("NMs", [128, 128], F32)
        NMi = tb("NMi", [128, 128], F32)
        S.op("pool", lambda e: e.memset(onesb[:], 1.0), writes=["onesb"])
        S.op("pool", lambda e: e.memset(negones[:], -1.0), writes=["negones"])
        S.op("pool", lambda e: e.memset(triU[:], 1.0), writes=["triU"])
        S.op("pool", lambda e: e.affine_select(out=triU[:], in_=triU[:], pattern=[[1, 128]], compare_op=ALU.is_ge,
                                               fill=0.0, base=0, channel_multiplier=-1), reads=["triU"], writes=["triU"])
        S.op("pool", lambda e: e.memset(NMs[:], 0.0), writes=["NMs"])
        S.op("pool", lambda e: e.affine_select(out=NMs[:], in_=NMs[:], pattern=[[-1, 128]], compare_op=ALU.is_ge,
                                               fill=-30000.0, base=-1, channel_multiplier=1), reads=["NMs"], writes=["NMs"])
        S.op("pool", lambda e: e.memset(NMi[:], 0.0), writes=["NMi"])
        S.op("pool", lambda e: e.affine_select(out=NMi[:], in_=NMi[:], pattern=[[1, 128]], compare_op=ALU.is_ge,
                                               fill=-30000.0, base=0, channel_multiplier=-1), reads=["NMi"], writes=["NMi"])
        cwT = tb("cwT", [128, 12, 4], F32)
        dw = tb("dw", [128, 12, 4, 128], BF16)
        for tap in range(4):
            S.dma("sp", "ld_cw", cwT[:, :, tap], conv_w[tap, :].rearrange("(c p) -> p c", p=128), writes=["cwT"],
                  allow_slow_non_contiguous=True)
        for ct in range(12):
            for tap in range(4):
                S.op("pool" if (ct + tap) % 2 else "dve",
                     lambda e, ct=ct, tap=tap: e.tensor_scalar(out=dw[:, ct, tap, :], in0=identf[:], scalar1=cwT[:, ct, tap:tap + 1],
                                                               scalar2=None, op0=ALU.mult),
                     reads=["identf", "cwT"], writes=["dw"])
        albp = tb("albp", [128, 8], F32)
        S.dma("sp", "ld_alb1", albp[:, 0:4], a_log[0:1, :].partition_broadcast(128), writes=["albp"])
        S.dma("sp", "ld_alb2", albp[:, 4:8], dt_bias[0:1, :].partition_broadcast(128), writes=["albp"])
        eAp = tb("eAp", [128, 4], F32)
        S.op("act", lambda e: e.activation(out=eAp[:], in_=albp[:, 0:4], func=AF.Exp), reads=["albp"], writes=["eAp"])
        Sf = tb("Sf", [128, 4, 128], F32)
        Sb = tb("Sb", [128, 4, 128], BF16)
        S.op("pool", lambda e: e.memset(Sf[:], 0.0), writes=["Sf"])
        S.op("pool", lambda e: e.memset(Sb[:], 0.0), writes=["Sb"])

        onp = tb("onp", [128, 128], F32)
        S.dma("sp", "ld_onp", onp[:], o_norm_g[0:1, :].partition_broadcast(128), writes=["onp"])
        ob_t1 = tb("ob_t1", [128, 4, 128], F32)
        ob_sg = tb("ob_sg", [128, 512], F32)
        ob_ms = tb("ob_ms", [128, 4], F32)
        ob_bf = tb("ob_bf", [128, 512], BF16)

        def emit_ob(P, od, odk, gate_ap, gkeys, slot):
            S.op("pool", lambda e: e.tensor_tensor(out=ob_t1[0:P], in0=od[0:P], in1=od[0:P], op=ALU.mult),
                 reads=[odk], writes=["ob_t1"])
            S.op("dve", lambda e: e.tensor_reduce(out=ob_ms[0:P, :], in_=ob_t1[0:P], axis=AX.X, op=ALU.add),
                 reads=["ob_t1"], writes=["ob_ms"])
            S.op("act", lambda e: e.activation(out=ob_ms[0:P, :], in_=ob_ms[0:P, :], func=AF.Sqrt, scale=1.0 / 128, bias=EPS),
                 reads=["ob_ms"], writes=["ob_ms"])
            S.op("dve", lambda e: e.reciprocal(out=ob_ms[0:P, :], in_=ob_ms[0:P, :]), reads=["ob_ms"], writes=["ob_ms"])
            S.op("dve", lambda e: e.tensor_tensor(out=ob_t1[0:P], in0=od[0:P],
                                                  in1=ob_ms[0:P, :].unsqueeze(2).broadcast_to([P, 4, 128]), op=ALU.mult),
                 reads=[odk, "ob_ms"], writes=["ob_t1"])
            S.op("pool", lambda e: e.tensor_tensor(out=ob_t1[0:P], in0=ob_t1[0:P],
                                                   in1=onp[0:P, :].unsqueeze(1).broadcast_to([P, 4, 128]), op=ALU.mult),
                 reads=["ob_t1", "onp"], writes=["ob_t1"])
            S.op("act", lambda e: e.activation(out=ob_sg[0:P, :], in_=gate_ap, func=AF.Silu), reads=gkeys, writes=["ob_sg"])
            S.op("dve", lambda e: e.tensor_tensor(out=ob_bf[0:P, :], in0=ob_t1[0:P].rearrange("p c t -> p (c t)"),
                                                  in1=ob_sg[0:P, :], op=ALU.mult), reads=["ob_t1", "ob_sg"], writes=["ob_bf"])
            S.dma("pool", "st_ob", ob_s[slot, 0:P, :], ob_bf[0:P, :], reads=["ob_bf"], writes=["ob_s%d" % slot])

        hTsamp, hksamp, zsamp, zsk = front(SB_, xs[:, :], gs1[0:SB_, :], mod[0:SB_, 0:D], ["gs1"] + modk(0, D), outs_sample)
        S.dma("pool", "st_hT", hT_s[NQB, :, :, 0:SB_], hTsamp[:, :, 0:SB_], reads=[hksamp], writes=["hT_s%d" % NQB])
        sample_delta(zsamp, zsk)

        kvcnt = {"n": 0}

        def kv_tiles(pTsel):
            kid = "_%d" % kvcnt["n"]
            kvcnt["n"] += 1
            KV = dict(kng=tb("kng" + kid, [128, 2, 64], F32), sq=tb("kv_sq" + kid, [128, 2, 2, 64], F32), ms=tb("kv_ms" + kid, [128, 2, 2], F32),
                      kn=tb("kv_kn" + kid, [128, 2, 2, 64], BF16), raw=tb("kv_raw" + kid, [128, 256], BF16), kT=tb("kT_st" + kid, [128, 4, 128], BF16),
                      vst=tb("vst" + kid, [128, 4, 65], BF16), kid=kid, pT=pTsel)
            S.dma("sp", "ld_kng" + kid, KV["kng"][:], k_norm_g[1:3, :].partition_broadcast(128), writes=["kng" + kid])
            S.op("pool", lambda e: e.memset(KV["vst"][:], 1.0), writes=["vst" + kid])
            return KV

        def kvprep(j, z, zk, KV):
            kng, kv_sq, kv_ms, kv_kn, kv_raw, kT_st, vst = KV["kng"], KV["sq"], KV["ms"], KV["kn"], KV["raw"], KV["kT"], KV["vst"]
            kid = KV["kid"]
            kview = z[:, 256:768].rearrange("p (s r) -> p s r", r=256)[:, :, 0:128].rearrange("p s (k d) -> p s k d", d=64)
            vview = z[:, 256:768].rearrange("p (s r) -> p s r", r=256)[:, :, 128:256].rearrange("p s (k d) -> p s k d", d=64)
            S.op("pool", lambda e: e.tensor_tensor(out=kv_sq[:], in0=kview, in1=kview, op=ALU.mult), reads=[zk], writes=["kv_sq" + kid])
            S.op("dve", lambda e: e.tensor_reduce(out=kv_ms[:], in_=kv_sq[:], axis=AX.X, op=ALU.add), reads=["kv_sq" + kid], writes=["kv_ms" + kid])
            S.op("act", lambda e: e.activation(out=kv_ms[:], in_=kv_ms[:], func=AF.Sqrt, scale=1.0 / 64, bias=EPS),
                 reads=["kv_ms" + kid], writes=["kv_ms" + kid])
            S.op("dve", lambda e: e.reciprocal(out=kv_ms[:], in_=kv_ms[:]), reads=["kv_ms" + kid], writes=["kv_ms" + kid])
            S.op("dve", lambda e: e.tensor_tensor(out=kv_sq[:], in0=kview, in1=kv_ms[:].unsqueeze(3).broadcast_to([128, 2, 2, 64]),
                                                  op=ALU.mult), reads=[zk, "kv_ms" + kid, "kv_sq" + kid], writes=["kv_sq" + kid])
            S.op("pool", lambda e: e.tensor_tensor(out=kv_kn[:], in0=kv_sq[:], in1=kng[:].unsqueeze(2).broadcast_to([128, 2, 2, 64]),
                                                   op=ALU.mult), reads=["kv_sq" + kid, "kng" + kid], writes=["kv_kn" + kid])
            S.op("act", lambda e: e.copy(out=kv_raw[:], in_=z[:, 0:256]), reads=[zk], writes=["kv_raw" + kid])
            srcs = [kv_kn[:, 0].rearrange("p k d -> p (k d)"), kv_kn[:, 1].rearrange("p k d -> p (k d)"), kv_raw[:, 0:128], kv_raw[:, 128:256]]
            pTx, pTxk = KV["pT"]
            for ti, s_ in enumerate(srcs):
                S.op("pe", lambda e, ti=ti, s_=s_: e.transpose(out=pTx[:, ti, :], in_=s_, identity=ident[:]),
                     reads=["kv_kn" + kid, "kv_raw" + kid, "ident"], writes=[pTxk])
            S.op("act", lambda e: e.copy(out=kT_st[:], in_=pTx[:, 0:4, :]), reads=[pTxk], writes=["kT_st" + kid])
            S.dma("pool", "st_KT" + kid, KT_s[:, :, j * 128:(j + 1) * 128].rearrange("t p c -> p t c"), kT_st[:], reads=["kT_st" + kid], writes=["KT_s"])
            S.op("pool", lambda e: e.tensor_copy(out=vst[:].rearrange("p (s k) c -> p s k c", k=2)[:, :, :, 0:64], in_=vview),
                 reads=[zk, "vst" + kid], writes=["vst" + kid])
            S.dma("pool", "st_V" + kid, V_s[j * 128:(j + 1) * 128, :, :], vst[:], reads=["vst" + kid], writes=["V_s"])

        KVA = kv_tiles((pT2, "pTf"))
        if NBLK < NBLKT:
            zfill = tb("zfill", [128, 2048], BF16)
            S.op("pool", lambda e: e.memset(zfill[:], 0.0), writes=["zfill"])
            for ty in range(4):
                for c0 in range(0, LLOC, 2048):
                    n = min(2048, LLOC - c0)
                    S.dma("sp", "zfK", KT_s[ty, :, c0:c0 + n], zfill[:, 0:n], reads=["zfill"], writes=["KT_s"])
            for r0 in range(0, LLOC, 128 * 7):
                n = min(128 * 7, LLOC - r0)
                S.dma("sp", "zfV", V_s[r0:r0 + n].rearrange("(t p) a c -> p t (a c)", p=128), zfill[:, 0:(n // 128) * 260].rearrange("p (t x) -> p t x", x=260),
                      reads=["zfill"], writes=["V_s"])

        zT = {nm: [tb("zT", [128, 4, 131], BF16) for _ in range(2)] for nm in ("k", "v")}
        zTq = tb("zTq", [128, 4, 131], BF16)
        for nm in ("k", "v"):
            for b_ in range(2):
                S.op("pool", lambda e, nm=nm, b_=b_: e.memset(zT[nm][b_][:], 0.0), writes=["zT%s%d" % (nm, b_)])
        cTk = tb("cTk", [128, 4, 128], F32)
        cTq = tb("cTq", [128, 4, 128], F32)
        cTv = tb("cTv", [128, 4, 128], BF16)
        sqb = tb("sqb", [128, 512], BF16)
        rn = tb("rn", [128, 512], F32)
        knT = tb("knT", [128, 4, 128], BF16)
        qnT = [tb("qnT", [128, 4, 128], BF16) for _ in range(2)]
        ktv = tb("ktv", [128, 8, 128], BF16)
        gsm = tb("gsm", [128, 64], F32)
        Tg = tb("Tg", [128, 4, 128], F32)
        Esb = tb("Esb", [128, 4, 128], F32)
        ETsb = tb("ETsb", [128, 4, 128], F32)
        Nk = [tb("Nk", [128, 4, 128], BF16) for _ in range(2)]
        Uk = [tb("Uk", [128, 4, 128], BF16) for _ in range(2)]
        Rt = [tb("Rt", [128, 4, 128], BF16) for _ in range(2)]
        kbv = tb("kbv", [128, 8, 128], BF16)
        rec = [dict(u=tb("u", [128, 4, 128], F32), wT=tb("wT", [128, 4, 128], BF16), kd=tb("kd", [128, 4, 128], BF16),
                    QKT=tb("QKT", [128, 4, 128], BF16), sc=tb("sc", [128, 8], F32)) for _ in range(2)]
        vnew = tb("vnew", [128, 4, 128], BF16)
        o2sb = tb("o2sb", [128, 4, 128], F32)
        osb = tb("osb", [128, 4, 128], F32)

        def delta_prep(j, hT, hk, hTp, hkp, own):
            b_ = j % 2
            R_ = rec[b_]
            rk = "rec%d" % b_
            groups = [("k", 4), ("v", 8)] + ([("q", 0)] if own else [])
            for nm, ct0 in groups:
                pz_, pzk = nextbank()
                zt = zTq if nm == "q" else zT[nm][b_]
                ztk = "zTq" if nm == "q" else "zT%s%d" % (nm, b_)
                for ct in range(4):
                    c0 = 768 + (ct0 + ct) * 128
                    for k in range(8):
                        S.op("pe", lambda e, ct=ct, k=k, c0=c0, pz_=pz_: e.matmul(
                            pz_[:, ct * 128:(ct + 1) * 128], lhsT=winb[:, k, c0:c0 + 128], rhs=hT[:, k, :],
                            start=(k == 0), stop=(k == 7)), reads=["winb2", "winb3", "winb4", hk], writes=[pzk])
                S.op("act", lambda e, pz_=pz_, zt=zt: e.copy(out=zt[:, :, 3:131], in_=pz_[:, :].rearrange("p (c t) -> p c t", t=128)),
                     reads=[pzk], writes=[ztk])
                if nm == "q":
                    pz2, pzk2 = nextbank()
                    for ct in range(4):
                        c0 = 768 + ct * 128
                        for k in range(8):
                            S.op("pe", lambda e, ct=ct, k=k, c0=c0, pz2=pz2: e.matmul(
                                pz2[:, ct * 4:ct * 4 + 3], lhsT=winb[:, k, c0:c0 + 128], rhs=hTp[:, k, 125:128],
                                start=(k == 0), stop=(k == 7)), reads=["winb2", hkp], writes=[pzk2])
                    S.op("act", lambda e, pz2=pz2, zt=zt: e.copy(out=zt[:, :, 0:3],
                                                                in_=pz2[:, 0:16].rearrange("p (c t) -> p c t", t=4)[:, :, 0:3]),
                         reads=[pzk2], writes=[ztk])
                else:
                    prev = zT[nm][1 - b_]
                    S.op("pool", lambda e, zt=zt, prev=prev: e.tensor_copy(out=zt[:, :, 0:3], in_=prev[:, :, 128:131]),
                         reads=["zT%s%d" % (nm, 1 - b_)], writes=[ztk])
                pc_, pck = nextbank()
                for ct in range(4):
                    for tap in range(4):
                        S.op("pe", lambda e, ct=ct, tap=tap, pc_=pc_, zt=zt, ct0=ct0: e.matmul(
                            pc_[:, ct * 128:(ct + 1) * 128], lhsT=dw[:, ct0 + ct, tap, :], rhs=zt[:, ct, tap:tap + 128],
                            start=(tap == 0), stop=(tap == 3)), reads=["dw", ztk], writes=[pck])
                dst = {"k": cTk, "v": cTv, "q": cTq}[nm]
                S.op("act", lambda e, pc_=pc_, dst=dst: e.activation(out=dst[:], in_=pc_[:, :].rearrange("p (c t) -> p c t", t=128),
                                                                    func=AF.Silu), reads=[pck], writes=["cT" + nm])
            for nm in (["k", "q"] if own else ["k"]):
                src_ = cTk if nm == "k" else cTq
                dst = knT if nm == "k" else qnT[b_]
                dk_ = "knT" if nm == "k" else rk
                S.op("pool", lambda e, src_=src_: e.tensor_tensor(out=sqb[:], in0=src_[:].rearrange("p c t -> p (c t)"),
                                                                in1=src_[:].rearrange("p c t -> p (c t)"), op=ALU.mult),
                     reads=["cT" + nm], writes=["sqb"])
                pss, pssk = nextbank()
                S.op("pe", lambda e, pss=pss: e.matmul(pss[:, :], lhsT=onesb[:], rhs=sqb[:], start=True, stop=True),
                     reads=["onesb", "sqb"], writes=[pssk])
                S.op("act", lambda e, pss=pss: e.activation(out=rn[:], in_=pss[:, :], func=AF.Sqrt, bias=EPS),
                     reads=[pssk], writes=["rn"])
                S.op("dve", lambda e: e.reciprocal(out=rn[:], in_=rn[:]), reads=["rn"], writes=["rn"])
                if nm == "q":
                    S.op("dve", lambda e, src_=src_, dst=dst: e.scalar_tensor_tensor(
                        out=dst[:].rearrange("p c t -> p (c t)"), in0=src_[:].rearrange("p c t -> p (c t)"),
                        scalar=128.0 ** -0.5, in1=rn[:], op0=ALU.mult, op1=ALU.mult), reads=["cTq", "rn"], writes=[dk_])
                else:
                    S.op("dve", lambda e, src_=src_, dst=dst: e.tensor_tensor(
                        out=dst[:].rearrange("p c t -> p (c t)"), in0=src_[:].rearrange("p c t -> p (c t)"),
                        in1=rn[:], op=ALU.mult), reads=["cTk", "rn"], writes=[dk_])
            for h in range(4):
                S.op("pe", lambda e, h=h: e.transpose(out=pT[:, h, :], in_=knT[:, h, :], identity=ident[:]),
                     reads=["knT", "ident"], writes=["pT"])
                S.op("pe", lambda e, h=h: e.transpose(out=pT[:, 4 + h, :], in_=cTv[:, h, :], identity=ident[:]),
                     reads=["cTv", "ident"], writes=["pT"])
            S.op("act", lambda e: e.copy(out=ktv[:], in_=pT[:]), reads=["pT"], writes=["ktv"])
            pab, pabk = nextbank()
            for k in range(8):
                S.op("pe", lambda e, k=k, pab=pab: e.matmul(pab[:, 0:8], lhsT=hT[:, k, :], rhs=winb[:, k, 2304:2312],
                                                           start=(k == 0), stop=(k == 7)), reads=[hk, "winb5"], writes=[pabk])
            xx, ax, sp_, gc, beta, nbeta = (gsm[:, 0:4], gsm[:, 4:8], gsm[:, 8:12], gsm[:, 12:16], gsm[:, 16:20], gsm[:, 20:24])
            S.op("dve", lambda e, pab=pab: e.tensor_tensor(out=xx, in0=pab[:, 0:4], in1=albp[:, 4:8], op=ALU.add),
                 reads=[pabk, "albp"], writes=["gsm"])
            S.op("act", lambda e, pab=pab: e.activation(out=beta, in_=pab[:, 4:8], func=AF.Sigmoid), reads=[pabk], writes=["gsm"])
            S.op("dve", lambda e: e.scalar_tensor_tensor(out=ax, in0=xx, scalar=-1.0, in1=xx, op0=ALU.mult, op1=ALU.max),
                 reads=["gsm"], writes=["gsm"])
            S.op("act", lambda e: e.activation(out=ax, in_=ax, func=AF.Exp, scale=-1.0), reads=["gsm"], writes=["gsm"])
            S.op("act", lambda e: e.activation(out=ax, in_=ax, func=AF.Ln, bias=1.0), reads=["gsm"], writes=["gsm"])
            S.op("dve", lambda e: e.scalar_tensor_tensor(out=sp_, in0=xx, scalar=0.0, in1=ax, op0=ALU.max, op1=ALU.add),
                 reads=["gsm"], writes=["gsm"])
            S.op("dve", lambda e: e.scalar_tensor_tensor(out=gc, in0=eAp[:], scalar=-1.0, in1=sp_, op0=ALU.mult, op1=ALU.mult),
                 reads=["gsm", "eAp"], writes=["gsm"])
            S.op("dve", lambda e: e.tensor_scalar(out=nbeta, in0=beta, scalar1=-1.0, scalar2=None, op0=ALU.mult),
                 reads=["gsm"], writes=["gsm"])
            pG, pGk = nextbank()
            S.op("pe", lambda e, pG=pG: e.matmul(pG[:, 0:4], lhsT=triU[:], rhs=gc, start=True, stop=True),
                 reads=["triU", "gsm"], writes=[pGk])
            S.op("pe", lambda e, pG=pG: e.matmul(pG[:, 4:8], lhsT=onesf[:], rhs=gc, start=True, stop=True),
                 reads=["onesf", "gsm"], writes=[pGk])
            sc = R_["sc"]
            egl, bke = gsm[:, 24:28], gsm[:, 28:32]
            S.op("act", lambda e, pG=pG: e.activation(out=sc[:, 0:8], in_=pG[:, 0:8], func=AF.Exp), reads=[pGk], writes=[rk])
            S.op("dve", lambda e, pG=pG: e.tensor_tensor(out=egl, in0=pG[:, 4:8], in1=pG[:, 0:4], op=ALU.subtract)
                 if False else e.tensor_copy(out=egl, in_=pG[:, 0:4]), reads=[pGk], writes=["gsm"])
            S.op("dve", lambda e, pG=pG: e.scalar_tensor_tensor(out=egl, in0=egl, scalar=-1.0, in1=pG[:, 4:8],
                                                               op0=ALU.mult, op1=ALU.add), reads=[pGk, "gsm"], writes=["gsm"])
            S.op("act", lambda e: e.activation(out=egl, in_=egl, func=AF.Exp), reads=["gsm"], writes=["gsm"])
            S.op("dve", lambda e: e.tensor_tensor(out=bke, in0=beta, in1=sc[:, 0:4], op=ALU.mult), reads=["gsm", rk], writes=["gsm"])
            bc = lambda ap: ap.unsqueeze(2).broadcast_to([128, 4, 128])
            S.op("dve", lambda e: e.tensor_tensor(out=kbv[:, 0:4, :], in0=ktv[:, 0:4, :], in1=bc(bke), op=ALU.mult),
                 reads=["ktv", "gsm"], writes=["kbv"])
            S.op("pool", lambda e: e.tensor_tensor(out=kbv[:, 4:8, :], in0=ktv[:, 4:8, :], in1=bc(beta), op=ALU.mult),
                 reads=["ktv", "gsm"], writes=["kbv"])
            S.op("pool", lambda e: e.tensor_tensor(out=R_["kd"][:], in0=ktv[:, 0:4, :], in1=bc(egl), op=ALU.mult),
                 reads=["ktv", "gsm"], writes=[rk])
            for h in range(4):
                S.op("dve" if h % 2 else "pool", lambda e, h=h: e.tensor_scalar(out=Tg[:, h, :], in0=triU[:], scalar1=gc[:, h:h + 1],
                                                                                 scalar2=None, op0=ALU.mult),
                     reads=["triU", "gsm"], writes=["Tg"])
            pEm, pEk = nextbank()
            for h in range(4):
                o_ = pEm[:, h * 128:(h + 1) * 128]
                S.op("pe", lambda e, o_=o_, h=h: e.matmul(o_, lhsT=Tg[:, h, :], rhs=onesf[:], start=True, stop=False),
                     reads=["Tg", "onesf"], writes=[pEk])
                S.op("pe", lambda e, o_=o_, h=h: e.matmul(o_, lhsT=negones[:], rhs=Tg[:, h, :], start=False, stop=False),
                     reads=["Tg", "negones"], writes=[pEk])
                S.op("pe", lambda e, o_=o_, h=h: e.matmul(o_, lhsT=identf[:], rhs=NMs[:], start=False, stop=True),
                     reads=["identf", "NMs"], writes=[pEk])
            S.op("act", lambda e, pEm=pEm: e.activation(out=Esb[:].rearrange("p c t -> p (c t)"), in_=pEm[:, :], func=AF.Exp),
                 reads=[pEk], writes=["Esb"])
            if own:
                pEt, pEtk = nextbank()
                for h in range(4):
                    o_ = pEt[:, h * 128:(h + 1) * 128]
                    S.op("pe", lambda e, o_=o_, h=h: e.matmul(o_, lhsT=onesf[:], rhs=Tg[:, h, :], start=True, stop=False),
                         reads=["Tg", "onesf"], writes=[pEtk])
                    S.op("pe", lambda e, o_=o_, h=h: e.matmul(o_, lhsT=Tg[:, h, :], rhs=negones[:], start=False, stop=False),
                         reads=["Tg", "negones"], writes=[pEtk])
                    S.op("pe", lambda e, o_=o_, h=h: e.matmul(o_, lhsT=identf[:], rhs=NMi[:], start=False, stop=True),
                         reads=["identf", "NMi"], writes=[pEtk])
                S.op("act", lambda e, pEt=pEt: e.activation(out=ETsb[:].rearrange("p c t -> p (c t)"), in_=pEt[:, :], func=AF.Exp),
                     reads=[pEtk], writes=["ETsb"])
                pkq, pkqk = nextbank()
                for h in range(4):
                    S.op("pe", lambda e, h=h, pkq=pkq: e.matmul(pkq[:, h * 128:(h + 1) * 128], lhsT=knT[:, h, :], rhs=qnT[b_][:, h, :],
                                                               start=True, stop=True), reads=["knT", rk], writes=[pkqk])
                S.op("dve", lambda e, pkq=pkq: e.tensor_tensor(out=R_["QKT"][:].rearrange("p c t -> p (c t)"), in0=pkq[:, :],
                                                              in1=ETsb[:].rearrange("p c t -> p (c t)"), op=ALU.mult),
                     reads=[pkqk, "ETsb"], writes=[rk])
            pkk, pkkk = nextbank()
            for h in range(4):
                S.op("pe", lambda e, h=h, pkk=pkk: e.matmul(pkk[:, h * 128:(h + 1) * 128], lhsT=knT[:, h, :], rhs=knT[:, h, :],
                                                           start=True, stop=True), reads=["knT"], writes=[pkkk])
            S.op("dve", lambda e, pkk=pkk: e.tensor_tensor(out=Esb[:].rearrange("p c t -> p (c t)"), in0=pkk[:, :],
                                                          in1=Esb[:].rearrange("p c t -> p (c t)"), op=ALU.mult),
                 reads=[pkkk, "Esb"], writes=["Esb"])
            S.op("dve", lambda e: e.tensor_tensor(out=Nk[0][:], in0=Esb[:], in1=bc(nbeta), op=ALU.mult),
                 reads=["Esb", "gsm"], writes=["Nk0"])
            for h in range(4):
                S.op("pe", lambda e, h=h: e.transpose(out=pT[:, h, :], in_=Nk[0][:, h, :], identity=ident[:]),
                     reads=["Nk0", "ident"], writes=["pT"])
            S.op("act", lambda e: e.copy(out=Uk[0][:], in_=pT[:, 0:4, :]), reads=["pT"], writes=["Uk0"])
            S.op("dve", lambda e: e.tensor_tensor(out=Rt[0][:], in0=Uk[0][:],
                                                  in1=ident[:].unsqueeze(1).broadcast_to([128, 4, 128]), op=ALU.add),
                 reads=["Uk0", "ident"], writes=["Rt0"])
            cur = 0
            rcur = 0
            for step in range(1, 7):
                nxt = 1 - cur
                pN, pNk = nextbank()
                for h in range(4):
                    S.op("pe", lambda e, h=h, pN=pN, cur=cur: e.matmul(pN[:, h * 128:(h + 1) * 128], lhsT=Uk[cur][:, h, :],
                                                                      rhs=Nk[cur][:, h, :], start=True, stop=True),
                         reads=["Uk%d" % cur, "Nk%d" % cur], writes=[pNk])
                if step < 6:
                    pU, pUk = nextbank()
                    for h in range(4):
                        S.op("pe", lambda e, h=h, pU=pU, cur=cur: e.matmul(pU[:, h * 128:(h + 1) * 128], lhsT=Nk[cur][:, h, :],
                                                                          rhs=Uk[cur][:, h, :], start=True, stop=True),
                             reads=["Uk%d" % cur, "Nk%d" % cur], writes=[pUk])
                S.op("act", lambda e, pN=pN, nxt=nxt: e.copy(out=Nk[nxt][:].rearrange("p c t -> p (c t)"), in_=pN[:, :]),
                     reads=[pNk], writes=["Nk%d" % nxt])
                if step < 6:
                    S.op("dve", lambda e, pU=pU, nxt=nxt: e.tensor_copy(out=Uk[nxt][:].rearrange("p c t -> p (c t)"), in_=pU[:, :]),
                         reads=[pUk], writes=["Uk%d" % nxt])
                pR, pRk = nextbank()
                for h in range(4):
                    S.op("pe", lambda e, h=h, pR=pR, nxt=nxt, rcur=rcur: e.matmul(pR[:, h * 128:(h + 1) * 128], lhsT=Nk[nxt][:, h, :],
                                                                                 rhs=Rt[rcur][:, h, :], start=True, stop=True),
                         reads=["Nk%d" % nxt, "Rt%d" % rcur], writes=[pRk])
                S.op("dve", lambda e, pR=pR, rcur=rcur: e.tensor_tensor(out=Rt[1 - rcur][:].rearrange("p c t -> p (c t)"), in0=pR[:, :],
                                                                       in1=Rt[rcur][:].rearrange("p c t -> p (c t)"), op=ALU.add),
                     reads=[pRk, "Rt%d" % rcur], writes=["Rt%d" % (1 - rcur)])
                cur = nxt
                rcur = 1 - rcur
            Rf = Rt[rcur]
            Rfk = "Rt%d" % rcur
            pu, puk = nextbank()
            for h in range(4):
                S.op("pe", lambda e, h=h, pu=pu: e.matmul(pu[:, h * 128:(h + 1) * 128], lhsT=Rf[:, h, :], rhs=kbv[:, 4 + h, :],
                                                         start=True, stop=True), reads=[Rfk, "kbv"], writes=[puk])
            S.op("act", lambda e, pu=pu: e.copy(out=R_["u"][:].rearrange("p c t -> p (c t)"), in_=pu[:, :]), reads=[puk], writes=[rk])
            pw, pwk = nextbank()
            for h in range(4):
                S.op("pe", lambda e, h=h, pw=pw: e.matmul(pw[:, h * 128:(h + 1) * 128], lhsT=kbv[:, h, :], rhs=Rf[:, h, :],
                                                         start=True, stop=True), reads=[Rfk, "kbv"], writes=[pwk])
            S.op("dve", lambda e, pw=pw: e.tensor_copy(out=R_["wT"][:].rearrange("p c t -> p (c t)"), in_=pw[:, :]),
                 reads=[pwk], writes=[rk])


        def delta_recur(j, own, zown, zownk):
            b_ = j % 2
            R_ = rec[b_]
            rk = "rec%d" % b_
            bc = lambda ap: ap.unsqueeze(2).broadcast_to([128, 4, 128])
            pws, pwsk = nextbank()
            for h in range(4):
                S.op("pe", lambda e, h=h, pws=pws: e.matmul(pws[:, h * 128:(h + 1) * 128], lhsT=R_["wT"][:, h, :], rhs=Sb[:, h, :],
                                                           start=True, stop=True), reads=[rk, "Sb"], writes=[pwsk])
            S.op("dve", lambda e, pws=pws: e.tensor_tensor(out=vnew[:].rearrange("p c t -> p (c t)"),
                                                          in0=R_["u"][:].rearrange("p c t -> p (c t)"), in1=pws[:, :], op=ALU.subtract),
                 reads=[rk, pwsk], writes=["vnew"])
            if own:
                po1, po1k = nextbank()
                po2, po2k = nextbank()
                for h in range(4):
                    S.op("pe", lambda e, h=h, po1=po1: e.matmul(po1[:, h * 128:(h + 1) * 128], lhsT=qnT[b_][:, h, :], rhs=Sb[:, h, :],
                                                               start=True, stop=True), reads=[rk, "Sb"], writes=[po1k])
                    S.op("pe", lambda e, h=h, po2=po2: e.matmul(po2[:, h * 128:(h + 1) * 128], lhsT=R_["QKT"][:, h, :], rhs=vnew[:, h, :],
                                                               start=True, stop=True), reads=[rk, "vnew"], writes=[po2k])
                S.op("act", lambda e, po2=po2: e.copy(out=o2sb[:].rearrange("p c t -> p (c t)"), in_=po2[:, :]),
                     reads=[po2k], writes=["o2sb"])
                S.op("dve", lambda e, po1=po1: e.tensor_tensor(out=osb[:], in0=po1[:, :].rearrange("p (c t) -> p c t", t=128),
                                                              in1=bc(R_["sc"][:, 0:4]), op=ALU.mult),
                     reads=[po1k, rk], writes=["osb"])
                S.op("dve", lambda e: e.tensor_tensor(out=osb[:], in0=osb[:], in1=o2sb[:], op=ALU.add),
                     reads=["osb", "o2sb"], writes=["osb"])
            if own:
                emit_ob(128, osb, "osb", zown[:, 2312:2824], [zownk], j // 8)
            pS_, pSk = nextbank()
            for h in range(4):
                S.op("pe", lambda e, h=h, pS_=pS_: e.matmul(pS_[:, h * 128:(h + 1) * 128], lhsT=R_["kd"][:, h, :], rhs=vnew[:, h, :],
                                                           start=True, stop=True), reads=[rk, "vnew"], writes=[pSk])
            S.op("dve", lambda e: e.tensor_tensor(out=Sf[:], in0=Sf[:], in1=bc(R_["sc"][:, 4:8]), op=ALU.mult),
                 reads=["Sf", rk], writes=["Sf"])
            S.op("dve", lambda e, pS_=pS_: e.tensor_tensor(out=Sf[:].rearrange("p c t -> p (c t)"), in0=Sf[:].rearrange("p c t -> p (c t)"),
                                                          in1=pS_[:, :], op=ALU.add), reads=["Sf", pSk], writes=["Sf"])
            S.op("act", lambda e: e.copy(out=Sb[:], in_=Sf[:]), reads=["Sf"], writes=["Sb"])

        def fe(j):
            own = (j % 8 == 7)
            i = j // 8
            last = own and (i == NQB - 1)

            def outs_prompt(z, zk, i=i, own=own, last=last):
                if own:
                    S.dma("pool", "st_kvp%d" % (i % 2), o_kvp[i, :, :], z[:, 0:512], reads=[zk])
                if last:
                    S.dma("pool", "st_winp", o_winp[:, :], z[:, 512:768], reads=[zk])
                    S.dma("pool", "st_convp", o_convp[:, :], z[125:128, 768:768 + 1536], reads=[zk])
            grp = [0, 1] + ([2, 3, 4] if last else []) + ([6] if own else [])
            hT, hk, z, zk = front(128, xp[j, :, :], gs1p[:, :], sh1p[:, :], ["gs1p", "sh1p"], outs_prompt,
                                  vcol=(vmask[:, j:j + 1] if j < 7 else None), groups=grp)
            kvprep(j, z, zk, KVA)
            if own:
                S.dma("pool", "st_hT", hT_s[i], hT[:], reads=[hk], writes=["hT_s%d" % i])
            return hT, hk, z, zk

        fes = {}
        fes[0] = fe(0)
        pend = None
        for j in range(NBLK):
            own = (j % 8 == 7)
            hT, hk, z, zk = fes[j]
            prev = fes.get(j - 1)

            def chainA(j=j, own=own, hT=hT, hk=hk, prev=prev, pend=pend):
                delta_prep(j, hT, hk, prev[0] if prev else None, prev[1] if prev else None, own)
                if pend is not None:
                    delta_recur(*pend)

            def chainB(j=j):
                if j + 1 < NBLK:
                    fes[j + 1] = fe(j + 1)
            cB = S.capture(chainB)
            cA = S.capture(chainA)
            S.replay([cA, cB])
            pend = (j, own, z, zk)
        delta_recur(*pend)
        S.dma("pool", "st_dp", o_dp.rearrange("h k v -> k h v"), Sf[:], reads=["Sf"])

        ar.release(m_passA)
        m_passB = ar.mark()
        NEGB, BIGB = -1e30, 1e30
        VW = 65 + NSBK
        blk1 = tb("blk1", [128, 128], BF16)
        S.op("pool", lambda e: e.memset(blk1[:], 0.0), writes=["blk1"])
        S.op("pool", lambda e: e.memset(blk1[0:64, 0:64], 1.0), writes=["blk1"])
        S.op("pool", lambda e: e.memset(blk1[64:128, 64:128], 1.0), writes=["blk1"])
        Eall = tb("Eall", [128, 64, 128], BF16)
        S.op("pool", lambda e: e.memset(Eall[:], 1.0), writes=["Eall"])
        S.op("pool", lambda e: e.affine_select(out=Eall[:].rearrange("p r (a b) -> p r a b", b=64),
                                               in_=Eall[:].rearrange("p r (a b) -> p r a b", b=64),
                                               pattern=[[-2, 64], [-1, 2], [0, 64]], compare_op=ALU.is_equal, fill=0.0,
                                               base=0, channel_multiplier=1), reads=["Eall"], writes=["Eall"])
        qw = tb("qw", [128, 8, 536], BF16)
        qng = tb("qng", [128, 64], F32)
        avn = tb("avn", [128, NSBK], F32)
        fz0 = tb("fz0", [128, NSBK], F32)
        kvl = tb("kvl", [128, NCT], F32)
        gk = tb("gk", [128, 1], F32)
        S.dma("sp", "ld_b1", qng[:], q_norm_g[0:1, :].partition_broadcast(128), writes=["qng"])
        S.dma("sp", "ld_b2", avn[:], availneg[:, :], writes=["avn"])
        S.dma("sp", "ld_b3", fz0[:], forced0[:, :], writes=["fz0"])
        S.dma("sp", "ld_b4", kvl[:], kvalid[:, :], writes=["kvl"])
        for hf in range(2):
            S.dma("sp", "ld_b5", gk[hf * 64:(hf + 1) * 64, :], k_norm_g[0, :].rearrange("(d o) -> d o", o=1), writes=["gk"])
        kcT = tb("kcT", [69, 2, NCT * 128], BF16)
        vcx = tb("vcx", [128, NCT, 2, VW], BF16)
        S.op("pool", lambda e: e.memset(kcT[:], 0.0), writes=["kcT"])
        S.op("pool", lambda e: e.memset(vcx[:], 0.0), writes=["vcx"])
        S.op("pool", lambda e: e.memset(vcx[:, :, :, 64:65], 1.0), reads=["vcx"], writes=["vcx"])
        McolT = tb("McolT", [128, 3, 4, 128], BF16)
        S.op("pool", lambda e: e.memset(McolT[:], 0.0), writes=["McolT"])
        wstg = tb("wstg", [128, 32, 128], F32)
        for (c0, n, o0) in ((O_Q, 512, 0), (O_G, 24, 512)):
            S.dma("sp", "ld_wstg", wstg[:].rearrange("p a b -> p (a b)")[:, 0:8 * n].rearrange("p (k n) -> p k n", n=n),
                  w_in_v[:, :, c0:c0 + n], writes=["wstg"])
            S.op("dve", lambda e, n=n, o0=o0: e.tensor_copy(
                out=qw[:, :, o0:o0 + n], in_=wstg[:].rearrange("p a b -> p (a b)")[:, 0:8 * n].rearrange("p (k n) -> p k n", n=n)),
                reads=["wstg"], writes=["qw"])
        KR = tb("KR", [128, LLOC + 32], BF16)
        wblk = tb("wblk", [128, 32, 128], BF16)
        pe2 = tb("pe2", [32, 128], F32)
        pecol = tb("pecol", [128, 32], BF16)
        biask = tb("biask", [128, 1], F32)
        biasv = tb("biasv", [1, 128], BF16)
        onesrow = tb("onesrow", [1, 128], BF16)
        kcp = tb("kcp", [128, 512], F32)
        ksq = tb("ksq", [128, 512], BF16)
        krn = tb("krn", [128, 512], F32)
        kcn = tb("kcn", [128, 512], BF16)
        S.op("pool", lambda e: e.memset(onesrow[:], 1.0), writes=["onesrow"])
        def compress():
            for ty in range(2):
                S.op("pool", lambda e: e.memset(wstg[:], 0.0), reads=["wstg"], writes=["wstg"])
                for hf in range(2):
                    for jh in range(2):
                        S.dma("sp", "ld_wstg", wstg[hf * 64:(hf + 1) * 64, jh * 16:(jh + 1) * 16, hf * 64:(hf + 1) * 64],
                              w_cmp[ty, jh * 16:(jh + 1) * 16].rearrange("j d e -> d j e"), reads=["wstg"], writes=["wstg"])
                    S.dma("sp", "ld_pe2", pe2[:, hf * 64:(hf + 1) * 64], pe_cmp[ty], writes=["pe2"])
                S.op("dve", lambda e: e.tensor_copy(out=wblk[:], in_=wstg[:]), reads=["wstg"], writes=["wblk"])
                S.op("pe", lambda e: e.transpose(out=pTf[:, 0, 0:32], in_=pe2[:], identity=identf[0:32, 0:32]),
                     reads=["pe2", "identf"], writes=["pTf"])
                S.op("act", lambda e: e.copy(out=pecol[:], in_=pTf[:, 0, 0:32]), reads=["pTf"], writes=["pecol"])
                S.op("pool", lambda e: e.memset(KR[:, LLOC:LLOC + 32], 0.0), reads=["KR"], writes=["KR"])
                for q4 in range(4):
                    c0 = q4 * (LLOC // 4)
                    S.dma("sp", "ld_KR", KR[:, c0:c0 + LLOC // 4], KT_s[2 + ty, :, c0:c0 + LLOC // 4], reads=["KT_s", "KR"], writes=["KR"])
                if ty == 0:
                    pb_, pbk = nextbank()
                    for j in range(32):
                        S.op("pe", lambda e, j=j, pb_=pb_: e.matmul(pb_[:, 0:1], lhsT=wblk[:, j, :], rhs=pecol[:, j:j + 1],
                                                                   start=(j == 0), stop=(j == 31)), reads=["wblk", "pecol"], writes=[pbk])
                    S.op("act", lambda e, pb_=pb_: e.copy(out=biask[:], in_=pb_[:, 0:1]), reads=[pbk], writes=["biask"])
                    for n0 in range(0, NCB, 512):
                        N = min(512, NCB - n0)
                        pk_, pkk_ = nextbank()
                        for j in range(32):
                            S.op("pe", lambda e, j=j, n0=n0, N=N, pk_=pk_: e.matmul(
                                pk_[:, 0:N], lhsT=wblk[:, j, :], rhs=KR[:, j + 16 * n0:j + 16 * (n0 + N):16],
                                start=(j == 0), stop=(j == 31)), reads=["wblk", "KR"], writes=[pkk_])
                        S.op("act", lambda e, N=N, pk_=pk_: e.activation(out=kcp[:, 0:N], in_=pk_[:, 0:N], func=AF.Identity, bias=biask[:, 0:1]),
                             reads=[pkk_, "biask"], writes=["kcp"])
                        S.op("pool", lambda e, N=N: e.tensor_tensor(out=ksq[:, 0:N], in0=kcp[:, 0:N], in1=kcp[:, 0:N], op=ALU.mult),
                             reads=["kcp"], writes=["ksq"])
                        ps_, psk_ = nextbank()
                        S.op("pe", lambda e, N=N, ps_=ps_: e.matmul(ps_[:, 0:N], lhsT=blk1[:], rhs=ksq[:, 0:N], start=True, stop=True),
                             reads=["blk1", "ksq"], writes=[psk_])
                        S.op("act", lambda e, N=N, ps_=ps_: e.activation(out=krn[:, 0:N], in_=ps_[:, 0:N], func=AF.Sqrt, scale=1.0 / 64, bias=EPS),
                             reads=[psk_], writes=["krn"])
                        S.op("dve", lambda e, N=N: e.reciprocal(out=krn[:, 0:N], in_=krn[:, 0:N]), reads=["krn"], writes=["krn"])
                        S.op("dve", lambda e, N=N: e.scalar_tensor_tensor(out=kcn[:, 0:N], in0=kcp[:, 0:N], scalar=gk[:, 0:1], in1=krn[:, 0:N],
                                                                         op0=ALU.mult, op1=ALU.mult), reads=["kcp", "gk", "krn"], writes=["kcn"])
                        S.dma("pool", "st_kc", KcT_s[:, n0:n0 + N], kcn[:, 0:N], reads=["kcn"], writes=["KcT_s"])
                    for kh in range(2):
                        S.dma("sp", "ld_kcT", kcT[0:64, kh, 0:NCB], KcT_s[kh * 64:(kh + 1) * 64, 0:NCB], reads=["KcT_s", "kcT"], writes=["kcT"])
                        S.dma("sp", "ld_kcT", kcT[64:69, kh, :], kaug_cmp[:, :], reads=["kcT"], writes=["kcT"])
                else:
                    pb_, pbk = nextbank()
                    for j in range(32):
                        S.op("pe", lambda e, j=j, pb_=pb_: e.matmul(pb_[0:1, 0:128], lhsT=pecol[:, j:j + 1], rhs=wblk[:, j, :],
                                                                   start=(j == 0), stop=(j == 31)), reads=["wblk", "pecol"], writes=[pbk])
                    S.op("act", lambda e, pb_=pb_: e.copy(out=biasv[:], in_=pb_[0:1, 0:128]), reads=[pbk], writes=["biasv"])
                    for t in range(NCT):
                        n0 = t * 128
                        N = min(128, NCB - n0)
                        pv_, pvk_ = nextbank()
                        for j in range(32):
                            S.op("pe", lambda e, j=j, n0=n0, N=N, pv_=pv_: e.matmul(
                                pv_[0:N, 0:128], lhsT=KR[:, j + 16 * n0:j + 16 * (n0 + N):16], rhs=wblk[:, j, :],
                                start=(j == 0), stop=False), reads=["wblk", "KR"], writes=[pvk_])
                        S.op("pe", lambda e, N=N, pv_=pv_: e.matmul(pv_[0:N, 0:128], lhsT=onesrow[0:1, 0:N], rhs=biasv[0:1, :],
                                                                   start=False, stop=True), reads=["onesrow", "biasv"], writes=[pvk_])
                        S.op("act", lambda e, t=t, N=N, pv_=pv_: e.copy(out=vcx[0:N, t, :, 0:64],
                                                                       in_=pv_[0:N, 0:128].rearrange("p (k d) -> p k d", d=64)),
                             reads=[pvk_, "vcx"], writes=["vcx"])
                        for kh in range(2):
                            S.dma("sp", "ld_band", vcx[:, t, kh, 65:VW], band_tab[n0:n0 + 128, :], reads=["vcx"], writes=["vcx"])
        compress()
        b_hT = tb("b_hT", [128, 8, 128], BF16)
        b_qsq = tb("b_qsq", [128, 8, 64], F32)
        b_qms = tb("b_qms", [128, 8], F32)
        b_qn = tb("b_qn", [128, 8, 64], BF16)
        b_gts = tb("b_gts", [128, 24], F32)
        QT = tb("QT", [69, 8, 128], BF16)
        PTc = tb("PTc", [128, NCT, 512], BF16)
        PTs = [tb("PTs", [128, 512], BF16) for _ in range(2)]
        CMcache = {}
        zb = tb("zb", [128, 512], BF16)
        S.op("pool", lambda e: e.memset(zb[:], 0.0), writes=["zb"])
        oc = tb("oc", [128, 4, 64], F32)
        imp = tb("imp", [128, NSBK], F32)
        imp3 = tb("imp3", [128, NSBK], F32)
        selt = tb("selt", [128, 3 * 128], F32)
        m8 = tb("m8", [128, 16], F32)
        rz = tb("rz", [128, 8], F32)
        oa_t = tb("oa_t", [128, 512], F32)
        CH = 8
        Kbuf = [tb("Kbuf", [69, CH * 128], BF16) for _ in range(2)]
        Vbuf = [tb("Vbuf", [128, CH, 65], BF16) for _ in range(2)]
        sctr = {"i": 0, "c": 0}

        def s_tile(lhsK, rhsQ, kkeys, mask_mm=None):
            i_ = sctr["i"] % 2
            sctr["i"] += 1
            pS, pSk = (pA, "pz0") if i_ == 0 else (pB, "pz1")
            S.op("pe", lambda e: e.matmul(pS[:, :], lhsT=lhsK, rhs=rhsQ, start=True, stop=(mask_mm is None)),
                 reads=kkeys + ["QT"], writes=[pSk])
            if mask_mm is not None:
                S.op("pe", lambda e: e.matmul(pS[:, :], lhsT=mask_mm[0], rhs=mask_mm[1], start=False, stop=True),
                     reads=["Eall", "McolT"], writes=[pSk])
            return pS, pSk

        def nsa_block(i, J, samp, qaug_ap, kvl_, avn_, fz0_, out_ap, outk, dbg_ap):
            if samp is None:
                S.dma("sp", "ld_bhT", b_hT[:], hT_s[i], reads=["hT_s%d" % i], writes=["b_hT"])
            else:
                S.op("pool", lambda e: e.memset(b_hT[:], 0.0), writes=["b_hT"])
                S.dma("sp", "ld_bhT", b_hT[:, :, 0:1], hT_s[NQB, :, :, samp:samp + 1], reads=["hT_s%d" % NQB, "b_hT"], writes=["b_hT"],
                      allow_slow_non_contiguous=True)
            pq, pqk = pC, "pz2"
            pg, pgk = pD, "pz3"
            for k in range(8):
                S.op("pe", lambda e, k=k: e.matmul(pq[:, :], lhsT=b_hT[:, k, :], rhs=qw[:, k, 0:512], start=(k == 0), stop=(k == 7)),
                     reads=["b_hT", "qw"], writes=[pqk])
            for k in range(8):
                S.op("pe", lambda e, k=k: e.matmul(pg[:, 0:24], lhsT=b_hT[:, k, :], rhs=qw[:, k, 512:536], start=(k == 0), stop=(k == 7)),
                     reads=["b_hT", "qw"], writes=[pgk])
            S.op("act", lambda e: e.activation(out=b_gts[:], in_=pg[:, 0:24], func=AF.Sigmoid), reads=[pgk], writes=["b_gts"])
            S.op("act", lambda e: e.activation(out=b_qsq[:].rearrange("p h d -> p (h d)"), in_=pq[:, :], func=AF.Square),
                 reads=[pqk], writes=["b_qsq"])
            S.op("dve", lambda e: e.tensor_reduce(out=b_qms[:], in_=b_qsq[:], axis=AX.X, op=ALU.add), reads=["b_qsq"], writes=["b_qms"])
            S.op("act", lambda e: e.activation(out=b_qms[:], in_=b_qms[:], func=AF.Sqrt, scale=1.0 / 64, bias=EPS),
                 reads=["b_qms"], writes=["b_qms"])
            S.op("dve", lambda e: e.reciprocal(out=b_qms[:], in_=b_qms[:]), reads=["b_qms"], writes=["b_qms"])
            S.op("dve", lambda e: e.tensor_tensor(out=b_qsq[:], in0=pq[:, :].rearrange("p (h d) -> p h d", d=64),
                                                  in1=b_qms[:].unsqueeze(2).broadcast_to([128, 8, 64]), op=ALU.mult),
                 reads=[pqk, "b_qms", "b_qsq"], writes=["b_qsq"])
            S.op("dve", lambda e: e.scalar_tensor_tensor(out=b_qn[:], in0=b_qsq[:], scalar=0.125,
                                                         in1=qng[:].unsqueeze(1).broadcast_to([128, 8, 64]), op0=ALU.mult, op1=ALU.mult),
                 reads=["b_qsq", "qng"], writes=["b_qn"])
            for h in range(8):
                S.op("pe", lambda e, h=h: e.transpose(out=pT[0:64, h, :], in_=b_qn[:, h, :], identity=ident[:]),
                     reads=["b_qn", "ident"], writes=["pT"])
            S.op("act", lambda e: e.copy(out=QT[0:64, :, :], in_=pT[0:64, :, :]), reads=["pT"], writes=["QT"])
            S.dma("sp", "ld_qaug", QT[64:69, :, :], qaug_ap, reads=["QT"], writes=["QT"])
            pendpv = []

            def flushpv():
                for f_ in pendpv:
                    f_()
                del pendpv[:]
            for kh in range(2):
                rhsQ = QT[:, 4 * kh:4 * kh + 4, :].rearrange("p h q -> p (h q)")
                nt = (8 * J + 6) // 128 + 1
                for t in range(nt):
                    mm = None
                    if t >= nt - 2 and (128 * J - 2048 * t - 31 - 16 * 127) < 0:
                        base_ = 128 * J - 2048 * t - 31
                        if base_ not in CMcache:
                            CM_ = tb("CMc%d" % len(CMcache), [128, 512], BF16)
                            CMk = "CMc%d" % len(CMcache)
                            CMcache[base_] = (CM_, CMk)
                            S.op("pool", lambda e, CM_=CM_: e.memset(CM_[:], 0.0), writes=[CMk])
                            S.op("pool", lambda e, CM_=CM_, base_=base_: e.affine_select(
                                out=CM_[:].rearrange("p (h q) -> p h q", q=128), in_=CM_[:].rearrange("p (h q) -> p h q", q=128),
                                pattern=[[0, 4], [1, 128]], compare_op=ALU.is_ge, fill=-30000.0, base=base_, channel_multiplier=-16),
                                reads=[CMk], writes=[CMk])
                        CM_, CMk = CMcache[base_]
                        mm = (ident[:], CM_[:])
                    pS, pSk = s_tile(kcT[:, kh, t * 128:(t + 1) * 128], rhsQ, ["kcT"] + ([CMk] if mm else []), mask_mm=mm)
                    S.op("act", lambda e, t=t, pS=pS: e.activation(out=PTc[:, t, :], in_=pS[:, :], func=AF.Exp), reads=[pSk], writes=["PTc"])
                    if t == 0:
                        S.op("dve", lambda e: e.tensor_scalar(out=PTc[:, 0, :], in0=PTc[:, 0, :], scalar1=kvl_[:, 0:1], scalar2=None, op0=ALU.mult),
                             reads=["PTc", "kvl"], writes=["PTc"])
                for h in range(4):
                    pO, pOk = (pC, "pz2") if h % 2 == 0 else (pD, "pz3")
                    for t in range(nt):
                        S.op("pe", lambda e, t=t, h=h, pO=pO, kh=kh: e.matmul(pO[:, 0:VW], lhsT=PTc[:, t, h * 128:(h + 1) * 128], rhs=vcx[:, t, kh, :],
                                                                            start=(t == 0), stop=(t == nt - 1)), reads=["PTc", "vcx"], writes=[pOk])
                    S.op("dve", lambda e, h=h, pO=pO: e.tensor_scalar(out=rz[:, h:h + 1], in0=pO[:, 64:65], scalar1=1e-30, scalar2=None, op0=ALU.max),
                         reads=[pOk], writes=["rz"])
                    S.op("dve", lambda e, h=h: e.reciprocal(out=rz[:, h:h + 1], in_=rz[:, h:h + 1]), reads=["rz"], writes=["rz"])
                    S.op("dve", lambda e, h=h, pO=pO: e.tensor_scalar(out=oc[:, h, :], in0=pO[:, 0:64], scalar1=rz[:, h:h + 1], scalar2=None, op0=ALU.mult),
                         reads=[pOk, "rz"], writes=["oc"])
                    if h == 0:
                        S.op("dve", lambda e, pO=pO: e.tensor_scalar(out=imp[:], in0=pO[:, 65:VW], scalar1=rz[:, 0:1], scalar2=None, op0=ALU.mult),
                             reads=[pOk, "rz"], writes=["imp"])
                    else:
                        S.op("dve", lambda e, h=h, pO=pO: e.scalar_tensor_tensor(out=imp[:], in0=pO[:, 65:VW], scalar=rz[:, h:h + 1], in1=imp[:],
                                                                                op0=ALU.mult, op1=ALU.add), reads=[pOk, "rz", "imp"], writes=["imp"])
                S.op("dve", lambda e: e.tensor_tensor(out=imp[:], in0=imp[:], in1=avn_[:], op=ALU.add), reads=["imp", "avn"], writes=["imp"])
                S.op("dve", lambda e: e.tensor_tensor(out=imp[:], in0=imp[:], in1=fz0_[:], op=ALU.max), reads=["imp", "fz0"], writes=["imp"])
                for hf in range(2):
                    if 2 * J + hf + 1 < NSBK:
                        S.op("pool", lambda e, hf=hf: e.memset(imp[hf * 64:(hf + 1) * 64, 2 * J + hf + 1:NSBK], NEGB), reads=["imp"], writes=["imp"])
                    S.op("pool", lambda e, hf=hf: e.memset(imp[hf * 64:(hf + 1) * 64, 2 * J + hf:2 * J + hf + 1], BIGB), reads=["imp"], writes=["imp"])
                S.op("dve", lambda e: e.max(out=m8[:, 0:8], in_=imp[:]), reads=["imp"], writes=["m8"])
                S.op("dve", lambda e: e.match_replace(out=imp3[:], in_to_replace=m8[:, 0:8], in_values=imp[:], imm_value=-3e38),
                     reads=["imp", "m8"], writes=["imp3"])
                S.op("dve", lambda e: e.max(out=m8[:, 8:16], in_=imp3[:]), reads=["imp3"], writes=["m8"])
                S.op("dve", lambda e: e.tensor_scalar(out=imp3[:], in0=imp[:], scalar1=m8[:, 15:16], scalar2=None, op0=ALU.is_ge),
                     reads=["imp", "m8", "imp3"], writes=["imp3"])
                S.op("dve", lambda e: e.tensor_scalar(out=imp[:], in0=imp[:], scalar1=-1e29, scalar2=None, op0=ALU.is_ge),
                     reads=["imp"], writes=["imp"])
                S.op("dve", lambda e: e.tensor_tensor(out=imp3[:], in0=imp3[:], in1=imp[:], op=ALU.mult), reads=["imp", "imp3"], writes=["imp3"])
                S.op("dve", lambda e: e.tensor_scalar(out=selt[:, 0:NSBK], in0=imp3[:], scalar1=-1.0, scalar2=30000.0, op0=ALU.add, op1=ALU.mult),
                     reads=["imp3"], writes=["selt"])
                pX, pXk = pC, "pz2"
                nb3 = [(b0__, min(128, NSBK - b0__)) for b0__ in range(0, NSBK, 128)]
                for c_, (b0_, nb) in enumerate(nb3):
                    S.op("pe", lambda e, c_=c_, b0_=b0_, nb=nb: e.transpose(out=pX[0:nb, c_ * 128:(c_ + 1) * 128], in_=selt[:, b0_:b0_ + nb], identity=identf[:]),
                         reads=["selt", "identf"], writes=[pXk])
                for c_, (b0_, nb) in enumerate(nb3):
                    S.op("act", lambda e, c_=c_, nb=nb: e.copy(out=McolT[0:nb, c_, :, :],
                                                               in_=pX[0:nb, c_ * 128:(c_ + 1) * 128].unsqueeze(1).broadcast_to([nb, 4, 128])),
                         reads=[pXk, "McolT"], writes=["McolT"])
                S.op("pe", lambda e: e.matmul(pE[:, 0:260], lhsT=zb[:, 0:128], rhs=zb[:, 0:260], start=True, stop=False),
                     reads=["zb"], writes=["pE"])
                for c0 in range(0, J + 1, CH):
                    n = min(CH, J + 1 - c0)
                    bi = sctr["c"] % 2
                    sctr["c"] += 1
                    Kb, Vb = Kbuf[bi], Vbuf[bi]
                    kbk, vbk = "Kbuf%d" % bi, "Vbuf%d" % bi
                    S.dma("sp", "ld_" + kbk, Kb[0:64, 0:n * 128], KT_s[0, kh * 64:(kh + 1) * 64, c0 * 128:(c0 + n) * 128], reads=["KT_s"], writes=[kbk])
                    S.dma("sp", "ld_" + kbk, Kb[64:69, 0:n * 128], kaug_pos[:, c0 * 128:(c0 + n) * 128], reads=[kbk], writes=[kbk])
                    S.dma("sp", "ld_" + vbk, Vb[:, 0:n, :], V_s[c0 * 128:(c0 + n) * 128, kh, :].rearrange("(t p) c -> p t c", p=128),
                          reads=["V_s"], writes=[vbk])
                    for tt in range(n):
                        kt = c0 + tt
                        cb, ri = (2 * kt) // 128, ((2 * kt) % 128) // 2
                        pS, pSk = s_tile(Kb[:, tt * 128:(tt + 1) * 128], rhsQ, [kbk],
                                         mask_mm=(Eall[:, ri, :], McolT[:, cb, :, :].rearrange("p h q -> p (h q)")))
                        pi = sctr["i"] % 2
                        PT_, PTk = PTs[pi], "PTs%d" % pi
                        S.op("act", lambda e, pS=pS, PT_=PT_: e.activation(out=PT_[:], in_=pS[:, :], func=AF.Exp), reads=[pSk], writes=[PTk])
                        if kt == J:
                            S.op("pool", lambda e, PT_=PT_: e.affine_select(
                                out=PT_[:].rearrange("p (h q) -> p h q", q=128), in_=PT_[:].rearrange("p (h q) -> p h q", q=128),
                                pattern=[[0, 4], [1, 128]], compare_op=ALU.is_ge, fill=0.0, base=0, channel_multiplier=-1),
                                reads=[PTk], writes=[PTk])
                        flushpv()
                        for h in range(4):
                            pendpv.append(lambda h=h, tt=tt, kt=kt, PT_=PT_, Vb=Vb, PTk=PTk, vbk=vbk: S.op(
                                "pe", lambda e: e.matmul(pE[:, h * 65:(h + 1) * 65], lhsT=PT_[:, h * 128:(h + 1) * 128], rhs=Vb[:, tt, :],
                                                         start=False, stop=(kt == J and h == 3)), reads=[PTk, vbk], writes=["pE"]))
                flushpv()
                w0 = max(0, J - 4)
                n = J + 1 - w0
                bi = sctr["c"] % 2
                sctr["c"] += 1
                Kb, Vb = Kbuf[bi], Vbuf[bi]
                kbk, vbk = "Kbuf%d" % bi, "Vbuf%d" % bi
                S.dma("sp", "ld_" + kbk, Kb[0:64, 0:n * 128], KT_s[1, kh * 64:(kh + 1) * 64, w0 * 128:(w0 + n) * 128], reads=["KT_s"], writes=[kbk])
                S.dma("sp", "ld_" + kbk, Kb[64:69, 0:n * 128], kaug_pos[:, w0 * 128:(w0 + n) * 128], reads=[kbk], writes=[kbk])
                S.dma("sp", "ld_" + vbk, Vb[:, 0:n, :], V_s[w0 * 128:(w0 + n) * 128, 2 + kh, :].rearrange("(t p) c -> p t c", p=128),
                      reads=["V_s"], writes=[vbk])
                S.op("pe", lambda e: e.matmul(pF[:, 0:260], lhsT=zb[:, 0:128], rhs=zb[:, 0:260], start=True, stop=False),
                     reads=["zb"], writes=["pF"])
                for tt in range(n):
                    kt = w0 + tt
                    pS, pSk = s_tile(Kb[:, tt * 128:(tt + 1) * 128], rhsQ, [kbk])
                    pi = sctr["i"] % 2
                    PT_, PTk = PTs[pi], "PTs%d" % pi
                    S.op("act", lambda e, pS=pS, PT_=PT_: e.activation(out=PT_[:], in_=pS[:, :], func=AF.Exp), reads=[pSk], writes=[PTk])
                    if kt == J:
                        S.op("pool", lambda e, PT_=PT_: e.affine_select(
                            out=PT_[:].rearrange("p (h q) -> p h q", q=128), in_=PT_[:].rearrange("p (h q) -> p h q", q=128),
                            pattern=[[0, 4], [1, 128]], compare_op=ALU.is_ge, fill=0.0, base=0, channel_multiplier=-1), reads=[PTk], writes=[PTk])
                    if kt == J - 4:
                        S.op("pool", lambda e, PT_=PT_: e.affine_select(
                            out=PT_[:].rearrange("p (h q) -> p h q", q=128), in_=PT_[:].rearrange("p (h q) -> p h q", q=128),
                            pattern=[[0, 4], [-1, 128]], compare_op=ALU.is_ge, fill=0.0, base=0, channel_multiplier=1), reads=[PTk], writes=[PTk])
                    if samp is None and kt < 7:
                        S.op("dve", lambda e, PT_=PT_, kt=kt: e.tensor_scalar(out=PT_[:], in0=PT_[:], scalar1=vmask[:, kt:kt + 1], scalar2=None, op0=ALU.mult),
                             reads=[PTk, "vmask"], writes=[PTk])
                    flushpv()
                    for h in range(4):
                        pendpv.append(lambda h=h, tt=tt, PT_=PT_, Vb=Vb, n=n, PTk=PTk, vbk=vbk: S.op(
                            "pe", lambda e: e.matmul(pF[:, h * 65:(h + 1) * 65], lhsT=PT_[:, h * 128:(h + 1) * 128], rhs=Vb[:, tt, :],
                                                     start=False, stop=(tt == n - 1 and h == 3)), reads=[PTk, vbk], writes=["pF"]))
                flushpv()
                for h in range(4):
                    hh = 4 * kh + h
                    S.op("dve", lambda e, h=h: e.reciprocal(out=rz[:, 4:5], in_=pE[:, h * 65 + 64:h * 65 + 65]), reads=["pE", "rz"], writes=["rz"])
                    S.op("dve", lambda e, h=h: e.reciprocal(out=rz[:, 5:6], in_=pF[:, h * 65 + 64:h * 65 + 65]), reads=["pF", "rz"], writes=["rz"])
                    S.op("dve", lambda e, hh=hh: e.tensor_tensor(out=rz[:, 4:6], in0=rz[:, 4:6], in1=b_gts[:, hh * 3 + 1:hh * 3 + 3], op=ALU.mult),
                         reads=["rz", "b_gts"], writes=["rz"])
                    S.op("dve", lambda e, h=h, hh=hh: e.tensor_scalar(out=oa_t[:, hh * 64:(hh + 1) * 64], in0=oc[:, h, :], scalar1=b_gts[:, hh * 3:hh * 3 + 1],
                                                                      scalar2=None, op0=ALU.mult), reads=["oc", "b_gts", "oa_t"], writes=["oa_t"])
                    S.op("dve", lambda e, h=h, hh=hh: e.scalar_tensor_tensor(out=oa_t[:, hh * 64:(hh + 1) * 64], in0=pE[:, h * 65:h * 65 + 64], scalar=rz[:, 4:5],
                                                                             in1=oa_t[:, hh * 64:(hh + 1) * 64], op0=ALU.mult, op1=ALU.add),
                         reads=["pE", "rz", "oa_t"], writes=["oa_t"])
                    S.op("dve", lambda e, h=h, hh=hh: e.scalar_tensor_tensor(out=oa_t[:, hh * 64:(hh + 1) * 64], in0=pF[:, h * 65:h * 65 + 64], scalar=rz[:, 5:6],
                                                                             in1=oa_t[:, hh * 64:(hh + 1) * 64], op0=ALU.mult, op1=ALU.add),
                         reads=["pF", "rz", "oa_t"], writes=["oa_t"])
            if samp is None:
                S.dma("pool", "st_oa", out_ap, oa_t[:], reads=["oa_t"], writes=[outk])
            else:
                S.dma("pool", "st_oa", out_ap, oa_t[0:1, :], reads=["oa_t"], writes=[outk])

        for i in range(NQB):
            nsa_block(i, 8 * i + 7, None, qaug[i], kvl, avn, fz0, oa_s[i], "oa_s%d" % i, None)

        kvl_s = tb("kvl_s", [128, NCT], F32)
        avn_s = tb("avn_s", [128, NSBK], F32)
        fz0_s = tb("fz0_s", [128, NSBK], F32)
        S.op("pool", lambda e: e.memset(kvl_s[:], 1.0), writes=["kvl_s"])
        S.op("pool", lambda e: e.memset(avn_s[:], 0.0), writes=["avn_s"])
        S.op("pool", lambda e: e.memset(fz0_s[:], -3e38), writes=["fz0_s"])
        S.op("pool", lambda e: e.memset(fz0_s[:, 0:1], 1e30), reads=["fz0_s"], writes=["fz0_s"])
        KVB = kv_tiles((pT, "pT"))
        KVB2 = kv_tiles((pT2, "pTf"))
        zts = [tb("zt_s", [128, 768], F32) for _ in range(2)]
        ptf = tb("ptf", [128, 128], F32)
        pti = tb("pti", [128, 128], I32)
        idxi = tb("idxi", [128, 128], I32)
        piota_t = tb("piota_t", [128, 1], F32)
        S.dma("sp", "ld_piota", piota_t[:], piota[:, :], writes=["piota_t"])
        for s_ in range(SB_):
            S.dma("sp", "ld_pti", pti[:], ptab[s_:s_ + 1, :].partition_broadcast(128), reads=["idxi"], writes=["pti"])
            S.op("dve", lambda e: e.tensor_copy(out=ptf[:], in_=pti[:]), reads=["pti"], writes=["ptf"])
            S.op("dve", lambda e: e.tensor_scalar(out=ptf[:], in0=ptf[:], scalar1=128.0, scalar2=piota_t[:, 0:1], op0=ALU.mult, op1=ALU.add),
                 reads=["ptf", "piota_t"], writes=["ptf"])
            S.op("dve", lambda e: e.tensor_copy(out=idxi[:], in_=ptf[:]), reads=["ptf"], writes=["idxi"])
            def page(j, KV_, s_=s_):
                zt = zts[j % 2]
                ztk = "zt_s%d" % (j % 2)
                if j < 128:
                    S.dma_fn("pool", "ld_" + ztk, lambda e, zt=zt, j=j: e.indirect_dma_start(
                        out=zt[:, 0:512], out_offset=None, in_=cache[:, :],
                        in_offset=bass.IndirectOffsetOnAxis(ap=idxi[:, j:j + 1], axis=0)), reads=["idxi"], writes=[ztk])
                    if j >= 124:
                        S.dma("sp", "ld2_" + ztk, zt[:, 512:768], cwin[s_, (j - 124) * 128:(j - 123) * 128, :], reads=[ztk], writes=[ztk])
                    else:
                        S.op("pool", lambda e, zt=zt: e.memset(zt[:, 512:768], 0.0), reads=[ztk], writes=[ztk])
                else:
                    S.op("pool", lambda e, zt=zt: e.memset(zt[:], 0.0), writes=[ztk])
                    S.dma("sp", "ld2_" + ztk, zt[0:1, 0:512], o_kvs[s_:s_ + 1, :], reads=[ztk, "o_kvs"], writes=[ztk])
                    S.dma("sp", "ld2_" + ztk, zt[0:1, 512:768], o_wins[s_, 511:512, :], reads=[ztk, "o_wins"], writes=[ztk])
                kvprep(j, zt, ztk, KV_)
            for j in range(0, 129, 2):
                cs = [S.capture(lambda j=j: page(j, KVB))]
                if j + 1 < 129:
                    cs.append(S.capture(lambda j=j: page(j + 1, KVB2)))
                S.replay(cs)
            compress()
            nsa_block(NQB, 128, s_, qaug_s[:, :, :], kvl_s, avn_s, fz0_s, oa_s[NQB, s_:s_ + 1, :], "oa_s%d" % NQB, None)

        ar.release(m_passB)
        NT = NQB + 1
        h2T_all = tb("h2T_all", [128, 8, NT * 128], BF16)
        gates_all = tb("gates_all", [128, NT, 65], F32)
        S.op("pool", lambda e: e.memset(gates_all[:], 1.0), writes=["gates_all"])
        m_passC = ar.mark()
        gt1p = tb("gt1p", [128, D], F32)
        gs2p = tb("gs2p", [128, D], F32)
        sh2p = tb("sh2p", [128, D], F32)
        bcast_row(gt1p, "gt1p", mod[32:33, 2 * D:3 * D], modk(2 * D, 3 * D))
        bcast_row(gs2p, "gs2p", gs2[32:33, :], ["gs2"])
        bcast_row(sh2p, "sh2p", mod[32:33, 3 * D:4 * D], modk(3 * D, 4 * D))
        wm = tb("wm", [128, 8, 2048], BF16)
        wpa = tb("wpa", [128, 4, 1024], BF16)
        wpb = tb("wpb", [128, 4, 1024], BF16)
        wo = tb("wo", [128, 8, 1024], BF16)
        wr = tb("wr", [128, 8, 64], BF16)
        brb = tb("brb", [128, 64], F32)
        wst2 = [tb("wst2", [128, 8, 256], F32) for _ in range(2)]
        S.dma("sp", "ld_brb", brb[:], b_router[0:1, :].partition_broadcast(128), writes=["brb"])
        loads = []
        for g in range(8):
            loads.append((w_in_v[:, :, O_MERGE + g * 256:O_MERGE + (g + 1) * 256], wm[:, :, g * 256:(g + 1) * 256], 8, 256, "wm"))
        wpa_v = w_proj_a.rearrange("(k p) n -> p k n", p=128)
        wpb_v = w_proj_b.rearrange("(k p) n -> p k n", p=128)
        wo_v = w_out.rearrange("(k p) n -> p k n", p=128)
        for hf in range(4):
            loads.append((wpa_v[:, :, hf * 256:(hf + 1) * 256], wpa[:, :, hf * 256:(hf + 1) * 256], 4, 256, "wpa"))
            loads.append((wpb_v[:, :, hf * 256:(hf + 1) * 256], wpb[:, :, hf * 256:(hf + 1) * 256], 4, 256, "wpb"))
            loads.append((wo_v[:, :, hf * 256:(hf + 1) * 256], wo[:, :, hf * 256:(hf + 1) * 256], 8, 256, "wo"))
        loads.append((w_router.rearrange("(k p) n -> p k n", p=128), wr[:, :, :], 8, 64, "wr"))
        for li, (src_ap, dst_ap, nk, ncol, key) in enumerate(loads):
            wb_ = wst2[li % 2]
            wk = "wst2_%d" % (li % 2)
            S.dma("sp", "ld_" + wk, wb_[:, 0:nk, 0:ncol], src_ap, writes=[wk])
            S.op("pool" if li % 2 else "dve",
                 lambda e, wb_=wb_, dst_ap=dst_ap, nk=nk, ncol=ncol: e.tensor_copy(out=dst_ap, in_=wb_[:, 0:nk, 0:ncol]),
                 reads=[wk], writes=[key])
        c_hT = tb("c_hT", [128, 8, 128], BF16)
        c_oa = tb("c_oa", [128, 512], F32)
        c_oab = tb("c_oab", [128, 1024], BF16)
        c_oabT = tb("c_oabT", [128, 8, 128], BF16)
        c_x = tb("c_x", [128, D], F32)
        c_sgm = tb("c_sgm", [128, 2048], F32)
        c_t1 = tb("c_t1", [128, 512], F32)
        c_t2 = tb("c_t2", [128, 512], F32)
        c_mb = tb("c_mb", [128, D], BF16)
        c_mT = tb("c_mT", [128, 8, 128], BF16)
        c_x1 = tb("c_x1", [128, D], F32)
        c_ss = tb("c_ss", [128, 1], F32)
        c_h2 = tb("c_h2", [128, D], BF16)
        c_sc = tb("c_sc", [128, 64], F32)
        c_sbias = tb("c_sbias", [128, 64], F32)
        c_top = tb("c_top", [128, 8], F32)
        c_den = tb("c_den", [128, 1], F32)

        def passC(t):
            smp = (t == NQB)
            P = SB_ if smp else 128
            cols = slice(t * 128, t * 128 + P)
            gt1_ap = mod[0:P, 2 * D:3 * D] if smp else gt1p[:, :]
            gs2_ap = gs2[0:P, :] if smp else gs2p[:, :]
            sh2_ap = mod[0:P, 3 * D:4 * D] if smp else sh2p[:, :]
            akeys = (modk(2 * D, 4 * D) + ["gs2"]) if smp else ["gt1p", "gs2p", "sh2p"]
            S.dma("sp", "ld_chT", c_hT[:, :, 0:P], hT_s[t, :, :, 0:P], reads=["hT_s%d" % t], writes=["c_hT"])
            S.dma("sp", "ld_coa", c_oa[0:P, :], oa_s[t, 0:P, :], reads=["oa_s%d" % t], writes=["c_oa"])
            S.dma("sp", "ld_cob", c_oab[0:P, 512:1024], ob_s[t, 0:P, :], reads=["ob_s%d" % t], writes=["c_oab_b"])
            S.dma("sp", "ld_cx", c_x[0:P, :], xs[:, :] if smp else xp[8 * t + 7, :, :], writes=["c_x"])
            S.op("pool", lambda e: e.tensor_copy(out=c_oab[0:P, 0:512], in_=c_oa[0:P, :]), reads=["c_oa"], writes=["c_oab_a"])
            for k in range(8):
                S.op("pe", lambda e, k=k: e.transpose(out=pT[:, k, 0:P], in_=c_oab[0:P, k * 128:(k + 1) * 128], identity=ident[0:P, 0:P]),
                     reads=["c_oab_a", "c_oab_b", "ident"], writes=["pT"])
            S.op("act", lambda e: e.copy(out=c_oabT[:, :, 0:P], in_=pT[:, :, 0:P]), reads=["pT"], writes=["c_oabT"])
            for g in range(4):
                pp, pk = nextbank()
                for k in range(8):
                    S.op("pe", lambda e, k=k, g=g, pp=pp: e.matmul(pp[0:P, :], lhsT=c_hT[:, k, 0:P], rhs=wm[:, k, g * 512:(g + 1) * 512],
                                                                  start=(k == 0), stop=(k == 7)), reads=["c_hT", "wm"], writes=[pk])
                S.op("act", lambda e, g=g, pp=pp: e.activation(out=c_sgm[0:P, g * 512:(g + 1) * 512], in_=pp[0:P, :], func=AF.Sigmoid),
                     reads=[pk], writes=["c_sgm"])
            for hf in range(2):
                pya, pyak = nextbank()
                pyb, pybk = nextbank()
                for k in range(4):
                    S.op("pe", lambda e, k=k, hf=hf, pya=pya: e.matmul(pya[0:P, :], lhsT=c_oabT[:, k, 0:P], rhs=wpa[:, k, hf * 512:(hf + 1) * 512],
                                                                      start=(k == 0), stop=(k == 3)), reads=["c_oabT", "wpa"], writes=[pyak])
                for k in range(4):
                    S.op("pe", lambda e, k=k, hf=hf, pyb=pyb: e.matmul(pyb[0:P, :], lhsT=c_oabT[:, 4 + k, 0:P], rhs=wpb[:, k, hf * 512:(hf + 1) * 512],
                                                                      start=(k == 0), stop=(k == 3)), reads=["c_oabT", "wpb"], writes=[pybk])
                S.op("dve", lambda e, hf=hf, pya=pya: e.tensor_tensor(out=c_t1[0:P, :], in0=pya[0:P, :], in1=c_sgm[0:P, hf * 512:(hf + 1) * 512],
                                                                     op=ALU.mult), reads=[pyak, "c_sgm"], writes=["c_t1"])
                S.op("dve", lambda e, hf=hf, pyb=pyb: e.tensor_tensor(out=c_t2[0:P, :], in0=pyb[0:P, :],
                                                                     in1=c_sgm[0:P, 1024 + hf * 512:1024 + (hf + 1) * 512], op=ALU.mult),
                     reads=[pybk, "c_sgm"], writes=["c_t2"])
                S.op("pool", lambda e, hf=hf: e.tensor_tensor(out=c_mb[0:P, hf * 512:(hf + 1) * 512], in0=c_t1[0:P, :], in1=c_t2[0:P, :], op=ALU.add),
                     reads=["c_t1", "c_t2"], writes=["c_mb"])
            for k in range(8):
                S.op("pe", lambda e, k=k: e.transpose(out=pT[:, k, 0:P], in_=c_mb[0:P, k * 128:(k + 1) * 128], identity=ident[0:P, 0:P]),
                     reads=["c_mb", "ident"], writes=["pT"])
            S.op("act", lambda e: e.copy(out=c_mT[:, :, 0:P], in_=pT[:, :, 0:P]), reads=["pT"], writes=["c_mT"])
            for hf in range(2):
                pmo, pmok = nextbank()
                for k in range(8):
                    S.op("pe", lambda e, k=k, hf=hf, pmo=pmo: e.matmul(pmo[0:P, :], lhsT=c_mT[:, k, 0:P], rhs=wo[:, k, hf * 512:(hf + 1) * 512],
                                                                      start=(k == 0), stop=(k == 7)), reads=["c_mT", "wo"], writes=[pmok])
                S.op("dve", lambda e, hf=hf, pmo=pmo: e.tensor_tensor(out=c_x1[0:P, hf * 512:(hf + 1) * 512], in0=pmo[0:P, :],
                                                                     in1=gt1_ap[:, hf * 512:(hf + 1) * 512], op=ALU.mult),
                     reads=[pmok] + akeys, writes=["c_x1"])
            S.op("pool", lambda e: e.tensor_tensor(out=c_x1[0:P, :], in0=c_x1[0:P, :], in1=c_x[0:P, :], op=ALU.add),
                 reads=["c_x1", "c_x"], writes=["c_x1"])
            oy = o_ys[:, :] if smp else o_yp[t, :, :]
            S.dma("pool", "st_x1", oy, c_x1[0:P, :], reads=["c_x1"], writes=["oy%d" % t])
            S.op("act", lambda e: e.activation(out=c_mb[0:P, :], in_=c_x1[0:P, :], func=AF.Square, accum_out=c_ss[0:P, :]),
                 reads=["c_x1"], writes=["c_mb", "c_ss"])
            S.op("act", lambda e: e.activation(out=c_ss[0:P, :], in_=c_ss[0:P, :], func=AF.Sqrt, scale=1.0 / D, bias=EPS),
                 reads=["c_ss"], writes=["c_ss"])
            S.op("dve", lambda e: e.reciprocal(out=c_ss[0:P, :], in_=c_ss[0:P, :]), reads=["c_ss"], writes=["c_ss"])
            S.op("dve", lambda e: e.scalar_tensor_tensor(out=c_x[0:P, :], in0=c_x1[0:P, :], scalar=c_ss[0:P, 0:1], in1=gs2_ap,
                                                         op0=ALU.mult, op1=ALU.mult), reads=["c_x1", "c_ss", "c_x"] + akeys, writes=["c_x"])
            S.op("pool", lambda e: e.tensor_tensor(out=c_h2[0:P, :], in0=c_x[0:P, :], in1=sh2_ap, op=ALU.add),
                 reads=["c_x"] + akeys, writes=["c_h2"])
            for k in range(8):
                S.op("pe", lambda e, k=k: e.transpose(out=pT[:, k, 0:P], in_=c_h2[0:P, k * 128:(k + 1) * 128], identity=ident[0:P, 0:P]),
                     reads=["c_h2", "ident"], writes=["pT"])
            S.op("act", lambda e: e.copy(out=h2T_all[:, :, cols], in_=pT[:, :, 0:P]), reads=["pT"], writes=["h2T_all"])
            pr, prk = nextbank()
            for k in range(8):
                S.op("pe", lambda e, k=k, pr=pr: e.matmul(pr[0:P, 0:64], lhsT=h2T_all[:, k, cols], rhs=wr[:, k, :],
                                                         start=(k == 0), stop=(k == 7)), reads=["h2T_all", "wr"], writes=[prk])
            S.op("act", lambda e, pr=pr: e.activation(out=c_sc[0:P, :], in_=pr[0:P, 0:64], func=AF.Sigmoid), reads=[prk], writes=["c_sc"])
            S.op("dve", lambda e: e.tensor_tensor(out=c_sbias[0:P, :], in0=c_sc[0:P, :], in1=brb[0:P, :], op=ALU.add),
                 reads=["c_sc", "brb"], writes=["c_sbias"])
            S.op("dve", lambda e: e.max(out=c_top[0:P, :], in_=c_sbias[0:P, :]), reads=["c_sbias"], writes=["c_top"])
            S.op("dve", lambda e: e.tensor_scalar(out=c_sbias[0:P, :], in0=c_sbias[0:P, :], scalar1=c_top[0:P, 5:6], scalar2=None,
                                                  op0=ALU.is_ge), reads=["c_sbias", "c_top"], writes=["c_sbias"])
            S.op("dve", lambda e: e.tensor_tensor(out=c_sc[0:P, :], in0=c_sc[0:P, :], in1=c_sbias[0:P, :], op=ALU.mult),
                 reads=["c_sc", "c_sbias"], writes=["c_sc"])
            S.op("dve", lambda e: e.tensor_reduce(out=c_den[0:P, :], in_=c_sc[0:P, :], axis=AX.X, op=ALU.add),
                 reads=["c_sc"], writes=["c_den"])
            S.op("dve", lambda e: e.reciprocal(out=c_den[0:P, :], in_=c_den[0:P, :]), reads=["c_den"], writes=["c_den"])
            S.op("dve", lambda e: e.tensor_scalar(out=gates_all[0:P, t, 0:64], in0=c_sc[0:P, :], scalar1=c_den[0:P, 0:1], scalar2=2.5,
                                                  op0=ALU.mult, op1=ALU.mult), reads=["c_sc", "c_den", "gates_all"], writes=["gates_all"])

        for t in range(NT):
            passC(t)

        ar.release(m_passC)
        gt2p = tb("gt2p", [128, D], F32)
        bcast_row(gt2p, "gt2p", mod[32:33, 5 * D:6 * D], modk(5 * D, 6 * D))
        yacc = tb("yacc", [128, NT, D], F32)
        S.op("pool", lambda e: e.memset(yacc[:], 0.0), writes=["yacc"])
        wgf = [tb("wgf", [128, 8, 256], F32) for _ in range(2)]
        wdf = [tb("wdf", [128, D], F32) for _ in range(2)]
        wgb = [tb("wgb", [128, 8, 256], BF16) for _ in range(2)]
        wdb = [tb("wdb", [128, D], BF16) for _ in range(2)]
        m_sa = tb("m_sa", [128, 128], F32)
        m_act = tb("m_act", [128, 128], BF16)
        m_actT = tb("m_actT", [128, 128], BF16)
        for ex in range(65):
            b_ = ex % 2
            gsrc = (w_exp_gu[ex] if ex < 64 else w_sh_gu).rearrange("(k p) f -> p k f", p=128)
            dsrc = w_exp_down[ex] if ex < 64 else w_sh_down
            S.dma("sp", "ld_wgf%d" % b_, wgf[b_][:], gsrc, writes=["wgf%d" % b_])
            S.dma("sp", "ld_wdf%d" % b_, wdf[b_][:], dsrc[:, :], writes=["wdf%d" % b_])
            S.op("pool", lambda e, b_=b_: e.tensor_copy(out=wgb[b_][:], in_=wgf[b_][:]), reads=["wgf%d" % b_], writes=["wgb%d" % b_])
            S.op("pool", lambda e, b_=b_: e.tensor_copy(out=wdb[b_][:], in_=wdf[b_][:]), reads=["wdf%d" % b_], writes=["wdb%d" % b_])
            for t in range(NT):
                P = SB_ if t == NQB else 128
                cols = slice(t * 128, t * 128 + P)
                pgu, pguk = banks[t % 4]
                for k in range(8):
                    S.op("pe", lambda e, k=k, pgu=pgu, cols=cols, P=P, b_=b_: e.matmul(pgu[0:P, 0:256], lhsT=h2T_all[:, k, cols], rhs=wgb[b_][:, k, :],
                                                                                      start=(k == 0), stop=(k == 7)),
                         reads=["h2T_all", "wgb%d" % b_], writes=[pguk])
                S.op("act", lambda e, pgu=pgu, P=P: e.activation(out=m_sa[0:P, :], in_=pgu[0:P, 0:128], func=AF.Silu), reads=[pguk], writes=["m_sa"])
                S.op("dve", lambda e, pgu=pgu, P=P, t=t, ex=ex: e.scalar_tensor_tensor(out=m_act[0:P, :], in0=m_sa[0:P, :],
                                                                                      scalar=gates_all[0:P, t, ex:ex + 1], in1=pgu[0:P, 128:256],
                                                                                      op0=ALU.mult, op1=ALU.mult),
                     reads=["m_sa", pguk, "gates_all"], writes=["m_act"])
                S.op("pe", lambda e, P=P: e.transpose(out=pT[:, 0, 0:P], in_=m_act[0:P, :], identity=ident[0:P, 0:P]),
                     reads=["m_act", "ident"], writes=["pT"])
                S.op("act", lambda e, P=P: e.copy(out=m_actT[:, 0:P], in_=pT[:, 0, 0:P]), reads=["pT"], writes=["m_actT"])
                for hf in range(2):
                    S.op("pe", lambda e, hf=hf, P=P, b_=b_: e.matmul(pAB[0:P, hf * 512:(hf + 1) * 512], lhsT=m_actT[:, 0:P], rhs=wdb[b_][:, hf * 512:(hf + 1) * 512],
                                                                    start=True, stop=True), reads=["m_actT", "wdb%d" % b_], writes=["pz0", "pz1"])
                S.op("dve", lambda e, P=P, t=t: e.tensor_tensor(out=yacc[0:P, t, :], in0=yacc[0:P, t, :], in1=pAB[0:P, :], op=ALU.add),
                     reads=["pz0", "pz1", "yacc%d" % t], writes=["yacc%d" % t])
        f_x1 = [tb("f_x1", [128, D], F32) for _ in range(2)]
        for t in range(NT):
            smp = (t == NQB)
            P = SB_ if smp else 128
            b_ = t % 2
            gt2_ap = mod[0:P, 5 * D:6 * D] if smp else gt2p[:, :]
            akeys = modk(5 * D, 6 * D) if smp else ["gt2p"]
            oy = o_ys[:, :] if smp else o_yp[t, :, :]
            S.dma("sp", "ld_fx1_%d" % b_, f_x1[b_][0:P, :], oy, reads=["oy%d" % t], writes=["f_x1_%d" % b_])
            S.op("dve", lambda e, t=t, P=P, gt2_ap=gt2_ap: e.tensor_tensor(out=yacc[0:P, t, :], in0=yacc[0:P, t, :], in1=gt2_ap, op=ALU.mult),
                 reads=["yacc%d" % t, "yacc"] + akeys, writes=["yacc%d" % t])
            S.op("pool", lambda e, t=t, P=P, b_=b_: e.tensor_tensor(out=f_x1[b_][0:P, :], in0=f_x1[b_][0:P, :], in1=yacc[0:P, t, :], op=ALU.add),
                 reads=["yacc%d" % t, "f_x1_%d" % b_], writes=["f_x1_%d" % b_])
            S.dma("pool", "st_y%d" % b_, oy, f_x1[b_][0:P, :], reads=["f_x1_%d" % b_], writes=["oy%d" % t])

        S.finalize()
    return nc


def kernel(**inp):
    f = lambda k: np.ascontiguousarray(np.asarray(inp[k]))
    x_prompt = f("x_prompt")[0]
    x_sample = f("x_sample")[:, 0]
    c_prompt = f("c_prompt")
    c_sample = f("c_sample")
    cache_win = f("cache_win_kv")[0].reshape(32, 512, 256)
    state_conv = f("state_conv")[0]
    xq = x_prompt.reshape(T // 128, 128, D)
    NBLK = T // 128 + 7
    import ml_dtypes
    bf = ml_dtypes.bfloat16
    LLOC = NBLK * 128
    NCB = 8 * NBLK
    NCT = (NCB + 127) // 128
    NSBK = 2 * NBLK
    tpos = np.arange(LLOC)
    kaug_pos = np.stack([np.ones(LLOC), np.ones(LLOC), tpos % 128, tpos - tpos % 128, np.zeros(LLOC)]).astype(np.float32).astype(bf)
    nn = np.arange(NCT * 128)
    kaug_cmp = np.stack([np.ones_like(nn), np.ones_like(nn), 16 * (nn % 128), 2048 * (nn // 128), np.ones_like(nn)]).astype(np.float32).astype(bf)
    slopes = 2.0 ** -(np.arange(8) + 1.0)
    qi = np.arange(128)
    qaug_all = np.zeros((T // 128 + 7, 5, 8, 128), np.float32)
    for J in range(T // 128 + 7):
        qaug_all[J, 0] = -slopes[:, None] * qi[None, :]
        qaug_all[J, 1] = -slopes[:, None] * (128.0 * J)
        qaug_all[J, 2] = slopes[:, None]
        qaug_all[J, 3] = slopes[:, None]
        qaug_all[J, 4] = 31.0 * slopes[:, None]
    qaug_own = np.ascontiguousarray(qaug_all[7::8][:16]).astype(bf)
    bb = np.arange(NSBK)
    band = ((nn[:, None] >= 4 * bb[None, :] - 1) & (nn[:, None] <= 4 * bb[None, :] + 3)).astype(np.float32).astype(bf)
    nc = build_nc()
    qaug_samp = np.ascontiguousarray(qaug_all[128]).astype(bf)
    page_table = f("page_table").astype(np.int32)
    cache_rows = f("cache_nsa_kv")[0].reshape(5120 * 128, 512)
    in_maps = []
    for c in range(NC):
        call = np.zeros((33, D), np.float32)
        call[0:SB_] = c_sample[SB_ * c:SB_ * (c + 1)]
        call[32] = c_prompt[0]
        in_maps.append({
            "xp": np.concatenate([np.zeros((7 - c, 128, D), np.float32), xq, np.zeros((c, 128, D), np.float32)], 0)[:NBLK],
            "vmask": np.ascontiguousarray(np.broadcast_to((np.arange(8) >= 7 - c).astype(np.float32)[None, :], (128, 8))),
            "xs": np.ascontiguousarray(x_sample[SB_ * c:SB_ * (c + 1)]),
            "call": call,
            "w_ada": f("w_ada")[0], "b_ada": f("b_ada"), "norm1_g": f("norm1_g"), "w_in": f("w_in")[0],
            "norm2_g": f("norm2_g"), "w_proj_a": f("w_proj_a")[0], "w_proj_b": f("w_proj_b")[0], "w_out": f("w_out")[0],
            "w_router": f("w_router")[0], "b_router": f("b_router"), "w_exp_gu": f("w_exp_gu")[0],
            "w_exp_down": f("w_exp_down")[0], "w_sh_gu": f("w_sh_gu")[0], "w_sh_down": f("w_sh_down")[0],
            "k_norm_g": f("k_norm_g")[0], "q_norm_g": f("q_norm_g"), "w_cmp": f("w_cmp")[0], "pe_cmp": f("pe_cmp")[0],
            "kaug_pos": kaug_pos, "kaug_cmp": kaug_cmp, "qaug": qaug_own, "band_tab": band,
            "availneg": np.ascontiguousarray(np.broadcast_to(np.where(bb < 2 * (7 - c), -1e30, 0.0).astype(np.float32)[None, :], (128, NSBK))),
            "forced0": np.ascontiguousarray(np.broadcast_to(np.where(bb == 2 * (7 - c), 1e30, -3e38).astype(np.float32)[None, :], (128, NSBK))),
            "kvalid": np.ascontiguousarray((np.arange(NCT * 128).reshape(NCT, 128).T >= 8 * (7 - c)).astype(np.float32)),
            "qaug_s": qaug_samp, "piota": np.arange(128, dtype=np.float32).reshape(128, 1),
            "ptab": np.ascontiguousarray(page_table[SB_ * c:SB_ * (c + 1)]),
            "cache": cache_rows,
            "cwin": np.ascontiguousarray(cache_win[SB_ * c:SB_ * (c + 1)]),
            "sconv": np.ascontiguousarray(state_conv[SB_ * c:SB_ * (c + 1)]),
            "sdelta": np.ascontiguousarray(f("state_delta")[0][SB_ * c:SB_ * (c + 1)]),
            "conv_w": f("conv_w")[0], "a_log": f("a_log"), "dt_bias": f("dt_bias"), "o_norm_g": f("o_norm_g"),
        })
    res = run_bass_kernel_spmd(nc, in_maps, core_ids=list(range(NC))).results
    kernel.last_res = res
    yq = np.zeros((T // 128, 128, D), np.float32)
    for c in range(NC):
        r_ = res[c]["o_yp"]
        yq[c::NC][:r_.shape[0]] = r_
    y_prompt = yq.reshape(1, T, D)
    y_sample = np.concatenate([res[c]["o_ys"] for c in range(NC)], 0).reshape(32, 1, D)
    kvp = np.zeros((T // 128, 128, 512), np.float32)
    for c in range(NC):
        r_ = res[c]["o_kvp"]
        kvp[c::NC][:r_.shape[0]] = r_
    kv_rows_prompt = kvp.reshape(1, 1, T, 4, 2, 64)
    win_prompt = np.concatenate([res[c]["o_winp"] for c in range(4, 8)], 0).reshape(1, 1, 512, 2, 2, 64)
    conv_prompt = res[7]["o_convp"].reshape(1, 1, 3, 1536)
    delta_prompt = res[0]["o_dp"].reshape(1, 1, 4, 128, 128)
    kv_rows_sample = np.concatenate([res[c]["o_kvs"] for c in range(NC)], 0).reshape(1, 32, 1, 4, 2, 64)
    win_sample = np.concatenate([res[c]["o_wins"] for c in range(NC)], 0).reshape(1, 32, 512, 2, 2, 64)
    conv_sample = np.concatenate([res[c]["o_convs"] for c in range(NC)], 0).reshape(1, 32, 3, 1536)
    delta_sample = np.concatenate([res[c]["o_ds"] for c in range(NC)], 0).reshape(1, 32, 4, 128, 128)
    return (y_prompt, y_sample, kv_rows_prompt, win_prompt, conv_prompt, delta_prompt,
            kv_rows_sample, win_sample, conv_sample, delta_sample)
```
